# Optimizing a Trainium2 kernel written in Bass

```python
import jax
import jax.numpy as jnp
from jax import lax
import numpy as np

D_MODEL = 1024
BATCH = 8
SEQ = 2048
DEPTH = 2

N_MIXERS = 2
GRID_W = 64
N_MEM = 256
MEM_HEADS = 4
MEM_DH = 64
MEM_W = MEM_HEADS * MEM_DH
NA_HEADS = 12
NA_DH = 64
NA_W = NA_HEADS * NA_DH
WIN_H_MAX = 8
WIN_W = 16
ML_HEADS = 4
ML_DH = 192
ML_W = ML_HEADS * ML_DH
CONV_K = 5
CHUNK = 128
MIX_W = NA_W + MEM_W
N_EXPERTS = 16
N_GROUPS = 4
EXPERTS_PER_GROUP = N_EXPERTS // N_GROUPS
TOP_K = 2
D_EXPERT = 512
MOE_BLOCK = 128
ALPHA = (2 * DEPTH) ** 0.25
BETA = (8 * DEPTH) ** -0.25
LN_EPS = 1e-5

kernel_name = 'hybrid_natten_mlstm_moe_encoder'


def layer_norm(x, g, b):
    xf = x.astype(jnp.float32)
    mu = jnp.mean(xf, axis=-1, keepdims=True)
    var = jnp.mean(jnp.square(xf - mu), axis=-1, keepdims=True)
    return ((xf - mu) * lax.rsqrt(var + LN_EPS) * g + b).astype(x.dtype)


def memory_attention(q, mem_k, mem_v):
    B, S, _ = q.shape
    q = q.reshape(B, S, MEM_HEADS, MEM_DH) * (MEM_DH ** -0.5)
    s = jnp.einsum('bshd,bmhd->bhsm', q, mem_k, preferred_element_type=jnp.float32)
    p = jax.nn.softmax(s, axis=-1).astype(mem_v.dtype)
    return jnp.einsum('bhsm,bmhd->bshd', p, mem_v).reshape(B, S, MEM_W)


def neighbourhood_attention(q, k, v, rpb):
    B, R, W, H, Dh = q.shape
    win_h = min(WIN_H_MAX, R)
    n_cb = W // WIN_W
    span = 2 * WIN_W
    q_cols = np.arange(W).reshape(n_cb, WIN_W)
    cb_start = np.clip(np.arange(n_cb) * WIN_W - WIN_W // 2, 0, W - span)
    key_cols = cb_start[:, None] + np.arange(span)[None, :]
    col_start = np.clip(q_cols - WIN_W // 2, 0, W - WIN_W)
    kc = key_cols[:, None, :]
    col_in = (kc >= col_start[..., None]) & (kc < col_start[..., None] + WIN_W)
    dc_idx = np.clip(kc - q_cols[..., None] + WIN_W - 1, 0, 2 * WIN_W - 2)
    rpb_c = jnp.transpose(rpb[:, :, dc_idx], (0, 2, 3, 1, 4))

    def row_block(r):
        rs = jnp.clip(r - win_h // 2, 0, R - win_h)
        k_rows = lax.dynamic_slice_in_dim(k, rs, win_h, axis=1)
        v_rows = lax.dynamic_slice_in_dim(v, rs, win_h, axis=1)
        k_blk = k_rows[:, :, key_cols]
        v_blk = v_rows[:, :, key_cols]
        q_blk = lax.dynamic_index_in_dim(q, r, axis=1, keepdims=False).reshape(B, n_cb, WIN_W, H, Dh)
        s = jnp.einsum('bjqhd,brjkhd->bhjqrk', q_blk, k_blk, preferred_element_type=jnp.float32)
        dr = rs + jnp.arange(win_h) - r + WIN_H_MAX - 1
        s = s + jnp.take(rpb_c, dr, axis=3)[None].astype(jnp.float32)
        s = jnp.where(col_in[:, :, None, :], s, -jnp.inf)
        p = jax.nn.softmax(s.reshape(B, H, n_cb, WIN_W, win_h * span), axis=-1)
        p = p.reshape(s.shape).astype(v.dtype)
        o = jnp.einsum('bhjqrk,brjkhd->bjqhd', p, v_blk)
        return o.reshape(B, W, H, Dh)

    out = lax.map(row_block, jnp.arange(R))
    return jnp.moveaxis(out, 0, 1)


def na_mixer(x, w_in, rpb, mem_k, mem_v):
    B, S, _ = x.shape
    rows = S // GRID_W
    h = x @ w_in
    q, k, v, q_mem = jnp.split(h, [NA_W, 2 * NA_W, 3 * NA_W], axis=-1)
    grid = (B, rows, GRID_W, NA_HEADS, NA_DH)
    y = neighbourhood_attention(q.reshape(grid) * (NA_DH ** -0.5), k.reshape(grid), v.reshape(grid), rpb)
    return jnp.concatenate([y.reshape(B, S, NA_W), memory_attention(q_mem, mem_k, mem_v)], axis=-1)


def mlstm_chunkwise(q, k, v, i_pre, f_pre):
    q, k, v = q.astype(jnp.float32), k.astype(jnp.float32), v.astype(jnp.float32)
    B, H, S, Dk = q.shape
    Dv = v.shape[-1]
    nc = S // CHUNK
    log_f = jax.nn.log_sigmoid(f_pre.astype(jnp.float32))
    i_pre = i_pre.astype(jnp.float32)

    def to_chunks(a):
        return jnp.moveaxis(a.reshape(a.shape[:2] + (nc, CHUNK) + a.shape[3:]), 2, 0)

    causal = np.tril(np.ones((CHUNK, CHUNK), dtype=bool))

    def step(carry, inp):
        C, n, m = carry
        qb, kb, vb, ib, fb = inp
        b = jnp.cumsum(fb, axis=-1)
        d = jnp.where(causal, b[..., :, None] - b[..., None, :] + ib[..., None, :], -jnp.inf)
        inter = b + m[..., None]
        m_t = jnp.maximum(jnp.max(d, axis=-1), inter)
        dexp = jnp.exp(d - m_t[..., None])
        iexp = jnp.exp(inter - m_t)
        s = jnp.einsum('bhtd,bhsd->bhts', qb, kb) * dexp
        num = jnp.einsum('bhts,bhse->bhte', s, vb) + iexp[..., None] * jnp.einsum('bhtd,bhde->bhte', qb, C)
        den = jnp.sum(s, axis=-1) + iexp * jnp.einsum('bhtd,bhd->bht', qb, n)
        h = num / jnp.maximum(jnp.abs(den), jnp.exp(-m_t))[..., None]
        b_last = b[..., -1]
        w_s = b_last[..., None] - b + ib
        m_new = jnp.maximum(b_last + m, jnp.max(w_s, axis=-1))
        wexp = jnp.exp(w_s - m_new[..., None])
        cexp = jnp.exp(b_last + m - m_new)
        C_new = cexp[..., None, None] * C + jnp.einsum('bhs,bhsd,bhse->bhde', wexp, kb, vb)
        n_new = cexp[..., None] * n + jnp.einsum('bhs,bhsd->bhd', wexp, kb)
        return (C_new, n_new, m_new), h

    init = (jnp.zeros((B, H, Dk, Dv), jnp.float32), jnp.zeros((B, H, Dk), jnp.float32),
            jnp.zeros((B, H), jnp.float32))
    _, hs = lax.scan(step, init, (to_chunks(q), to_chunks(k), to_chunks(v), to_chunks(i_pre), to_chunks(log_f)))
    return jnp.moveaxis(hs, 0, 2).reshape(B, H, S, Dv)


def mlstm_mixer(x, w_in, conv_w, conv_b, w_qkv, gate_b, norm_g, skip, mem_k, mem_v):
    B, S, _ = x.shape
    h = x @ w_in
    xm, z, gates, q_mem = jnp.split(h, [ML_W, 2 * ML_W, 2 * ML_W + 4 * ML_HEADS], axis=-1)
    xc = lax.conv_general_dilated(xm, conv_w[:, None, :], window_strides=(1,), padding='SAME',
                                  dimension_numbers=('NWC', 'WIO', 'NWC'), feature_group_count=ML_W)
    xc = jax.nn.silu(xc + conv_b)
    xc_h = xc.reshape(B, S, ML_HEADS, ML_DH)
    xm_h = xm.reshape(B, S, ML_HEADS, ML_DH)
    q = jnp.einsum('bshd,hde->bhse', xc_h, w_qkv[0])
    k = jnp.einsum('bshd,hde->bhse', xc_h, w_qkv[1]) * (ML_DH ** -0.5)
    v = jnp.einsum('bshd,hde->bhse', xm_h, w_qkv[2])
    g = (gates.reshape(B, S, 4, ML_HEADS) + gate_b).astype(jnp.float32)
    g = jnp.transpose(g, (2, 0, 3, 1))
    h_f = mlstm_chunkwise(q, k, v, g[0], g[1])
    h_b = jnp.flip(mlstm_chunkwise(jnp.flip(q, 2), jnp.flip(k, 2), jnp.flip(v, 2),
                                   jnp.flip(g[2], -1), jnp.flip(g[3], -1)), 2)
    hs = h_f + h_b
    mu = jnp.mean(hs, axis=-1, keepdims=True)
    var = jnp.mean(jnp.square(hs - mu), axis=-1, keepdims=True)
    hs = (hs - mu) * lax.rsqrt(var + LN_EPS)
    hs = jnp.transpose(hs, (0, 2, 1, 3)).reshape(B, S, ML_W) * norm_g
    y = ((hs + skip * xc) * jax.nn.silu(z)).astype(x.dtype)
    return jnp.concatenate([y, memory_attention(q_mem, mem_k, mem_v)], axis=-1)


def moe(x, router_w, router_b, w_gate, w_up, w_down):
    B, S, D = x.shape
    N = B * S
    xf = x.reshape(N, D)
    scores = jax.nn.sigmoid(jnp.matmul(xf, router_w, preferred_element_type=jnp.float32))
    biased = scores + router_b.astype(jnp.float32)
    grp_score = jnp.sum(lax.top_k(biased.reshape(N, N_GROUPS, EXPERTS_PER_GROUP), 2)[0], axis=-1)
    g_sel = jnp.argmax(grp_score, axis=-1)
    in_group = (jnp.arange(N_EXPERTS) // EXPERTS_PER_GROUP)[None, :] == g_sel[:, None]
    _, idx = lax.top_k(jnp.where(in_group, biased, -jnp.inf), TOP_K)
    w = jnp.take_along_axis(scores, idx, axis=1)
    w = w / jnp.sum(w, axis=-1, keepdims=True)
    NK = N * TOP_K
    flat_e = idx.reshape(NK)
    order = jnp.argsort(flat_e)
    e_sorted = flat_e[order]
    tok_sorted = order // TOP_K
    counts = jnp.bincount(flat_e, length=N_EXPERTS)
    padded = (counts + MOE_BLOCK - 1) // MOE_BLOCK * MOE_BLOCK
    pad_end = jnp.cumsum(padded)
    pad_start = pad_end - padded
    start = jnp.cumsum(counts) - counts
    dest = pad_start[e_sorted] + jnp.arange(NK) - start[e_sorted]
    n_blocks = -(-NK // MOE_BLOCK) + N_EXPERTS
    buf = jnp.zeros((n_blocks * MOE_BLOCK, D), x.dtype).at[dest].set(xf[tok_sorted])
    block_e = jnp.minimum(jnp.searchsorted(pad_end, jnp.arange(n_blocks) * MOE_BLOCK, side='right'),
                          N_EXPERTS - 1)

    def expert_block(args):
        xb, e = args
        hb = jax.nn.silu(xb @ w_gate[e]) * (xb @ w_up[e])
        return hb @ w_down[e]

    y = lax.map(expert_block, (buf.reshape(n_blocks, MOE_BLOCK, D), block_e)).reshape(n_blocks * MOE_BLOCK, D)
    w_sorted = w.reshape(NK)[order].astype(x.dtype)
    out = jnp.zeros((N, D), x.dtype).at[tok_sorted].add(y[dest] * w_sorted[:, None])
    return out.reshape(B, S, D)


def setup_inputs(seed: int = 0) -> dict:
    key = jax.random.key(seed)
    ks = jax.random.split(key, 26)
    n_a = (DEPTH + 1) // 2
    n_b = DEPTH // 2

    def nrm(k, shape, scale):
        return jax.random.normal(k, shape, jnp.float32) * scale

    x = nrm(ks[0], (BATCH, SEQ, D_MODEL), 1.0)
    mem = nrm(ks[1], (BATCH, N_MEM, D_MODEL), 1.0)
    mem_ln_g = 1.0 + nrm(ks[2], (D_MODEL,), 0.05)
    mem_ln_b = nrm(ks[3], (D_MODEL,), 0.02)
    w_mem_kv = nrm(ks[4], (D_MODEL, 2 * MEM_W), D_MODEL ** -0.5).at[:, MEM_W:].multiply(BETA)
    router_w = nrm(ks[5], (D_MODEL, N_EXPERTS), D_MODEL ** -0.5)
    router_b = nrm(ks[6], (N_EXPERTS,), 0.01)
    na_w_in = nrm(ks[7], (n_a, D_MODEL, 3 * NA_W + MEM_W), D_MODEL ** -0.5)
    na_w_in = na_w_in.at[:, :, 2 * NA_W:3 * NA_W].multiply(BETA)
    na_rpb = nrm(ks[8], (n_a, NA_HEADS, 2 * WIN_H_MAX - 1, 2 * WIN_W - 1), 0.1)
    ml_w_in = nrm(ks[9], (n_b, D_MODEL, 2 * ML_W + 4 * ML_HEADS + MEM_W), D_MODEL ** -0.5)
    ml_conv_w = nrm(ks[10], (n_b, CONV_K, ML_W), CONV_K ** -0.5)
    ml_conv_b = nrm(ks[11], (n_b, ML_W), 0.02)
    ml_w_qkv = nrm(ks[12], (n_b, 3, ML_HEADS, ML_DH, ML_DH), ML_DH ** -0.5).at[:, 2].multiply(BETA)
    i_bias = nrm(ks[13], (n_b, 2, ML_HEADS), 0.1)
    f_bias = jnp.linspace(3.0, 6.0, ML_HEADS)[None, None, :] + nrm(ks[14], (n_b, 2, ML_HEADS), 0.1)
    ml_gate_b = jnp.stack([i_bias[:, 0], f_bias[:, 0], i_bias[:, 1], f_bias[:, 1]], axis=1)
    ml_norm_g = 1.0 + nrm(ks[15], (n_b, ML_W), 0.05)
    ml_skip = 1.0 + nrm(ks[16], (n_b, ML_W), 0.05)
    w_out = nrm(ks[17], (DEPTH, MIX_W, D_MODEL), MIX_W ** -0.5 * BETA)
    ln_g = 1.0 + nrm(ks[18], (DEPTH, 2, D_MODEL), 0.05)
    ln_b = nrm(ks[19], (DEPTH, 2, D_MODEL), 0.02)
    exp_w_gate = nrm(ks[20], (DEPTH, N_EXPERTS, D_MODEL, D_EXPERT), D_MODEL ** -0.5)
    exp_w_up = nrm(ks[21], (DEPTH, N_EXPERTS, D_MODEL, D_EXPERT), D_MODEL ** -0.5)
    exp_w_down = nrm(ks[22], (DEPTH, N_EXPERTS, D_EXPERT, D_MODEL), D_EXPERT ** -0.5 * BETA)
    return {'x': x, 'mem': mem, 'mem_ln_g': mem_ln_g, 'mem_ln_b': mem_ln_b, 'w_mem_kv': w_mem_kv,
            'router_w': router_w, 'router_b': router_b, 'na_w_in': na_w_in, 'na_rpb': na_rpb,
            'ml_w_in': ml_w_in, 'ml_conv_w': ml_conv_w, 'ml_conv_b': ml_conv_b, 'ml_w_qkv': ml_w_qkv,
            'ml_gate_b': ml_gate_b, 'ml_norm_g': ml_norm_g, 'ml_skip': ml_skip, 'w_out': w_out,
            'ln_g': ln_g, 'ln_b': ln_b, 'exp_w_gate': exp_w_gate, 'exp_w_up': exp_w_up,
            'exp_w_down': exp_w_down}


def reference(x, mem, mem_ln_g, mem_ln_b, w_mem_kv, router_w, router_b, na_w_in, na_rpb,
              ml_w_in, ml_conv_w, ml_conv_b, ml_w_qkv, ml_gate_b, ml_norm_g, ml_skip, w_out,
              ln_g, ln_b, exp_w_gate, exp_w_up, exp_w_down):
    B, M, _ = mem.shape
    mem_k, mem_v = jnp.split(layer_norm(mem, mem_ln_g, mem_ln_b) @ w_mem_kv, 2, axis=-1)
    mem_k = mem_k.reshape(B, M, MEM_HEADS, MEM_DH)
    mem_v = mem_v.reshape(B, M, MEM_HEADS, MEM_DH)
    for i in range(DEPTH):
        j = i // N_MIXERS
        if i % N_MIXERS == 0:
            mixed = na_mixer(x, na_w_in[j], na_rpb[j], mem_k, mem_v)
        else:
            mixed = mlstm_mixer(x, ml_w_in[j], ml_conv_w[j], ml_conv_b[j], ml_w_qkv[j], ml_gate_b[j],
                                ml_norm_g[j], ml_skip[j], mem_k, mem_v)
        x = layer_norm(ALPHA * x + mixed @ w_out[i], ln_g[i, 0], ln_b[i, 0])
        x = layer_norm(ALPHA * x + moe(x, router_w, router_b, exp_w_gate[i], exp_w_up[i], exp_w_down[i]),
                       ln_g[i, 1], ln_b[i, 1])
    return x
```

```python
import numpy as np
import concourse.bass as bass
import concourse.mybir as mybir
from concourse.bass_utils import run_bass_kernel_spmd
from contextlib import ExitStack

F32 = mybir.dt.float32
BF16 = mybir.dt.bfloat16
AF = mybir.ActivationFunctionType
ALU = mybir.AluOpType
AX = mybir.AxisListType


class Prog:
    CE = ['pe', 'act', 'dve', 'pool']

    def __init__(self, nc, st):
        self.nc = nc
        self.st = st
        self.engs = {'pe': nc.tensor, 'act': nc.scalar, 'dve': nc.vector, 'pool': nc.gpsimd, 'sp': nc.sync}
        self.sems = {e: st.enter_context(nc.semaphore("s_" + e)) for e in self.CE}
        self.cnt = {e: 0 for e in self.CE}
        self.seen = {e: {} for e in self.CE + ['sp']}
        self.last_w = {}
        self.readers = {}
        self.out_sems = []

    def _sem(self, name):
        if name not in self.sems:
            self.sems[name] = self.st.enter_context(self.nc.semaphore("d_" + name))
            self.cnt[name] = 0
        return self.sems[name]

    def _deps(self, engine, reads, writes, skip_waw_sem=None):
        deps = {}

        def add(ev):
            sk, val, eng = ev
            if eng == 'pe' and engine == 'pe':
                return
            if deps.get(sk, 0) < val:
                deps[sk] = val
        for k in reads:
            if k in self.last_w:
                add(self.last_w[k])
            if k.startswith('ps'):
                for sk, (val, eng) in self.readers.get(k, {}).items():
                    if eng != engine:
                        add((sk, val, eng))
        for k in writes:
            if k in self.last_w:
                ev = self.last_w[k]
                if not (skip_waw_sem is not None and ev[0] == skip_waw_sem and not self.readers.get(k)):
                    add(ev)
            for sk, (val, eng) in self.readers.get(k, {}).items():
                add((sk, val, eng))
        waits = []
        for sk, val in deps.items():
            if self.seen[engine].get(sk, 0) >= val:
                continue
            self.seen[engine][sk] = val
            waits.append((sk, val))
        return waits

    def _record(self, ev, reads, writes):
        for k in writes:
            self.last_w[k] = ev
            self.readers[k] = {}
        for k in reads:
            r = self.readers.setdefault(k, {})
            if r.get(ev[0], (0, None))[0] < ev[1]:
                r[ev[0]] = (ev[1], ev[2])

    def op(self, engine, fn, reads=(), writes=()):
        waits = self._deps(engine, reads, writes)
        self.cnt[engine] += 1
        ev = (engine, self.cnt[engine], engine)
        self._record(ev, reads, writes)
        self._emit(engine, waits, fn, (engine, 1))

    def _emit(self, engine, waits, fn, inc):
        eng = self.engs[engine]
        for sk, val in waits:
            eng.wait_ge(self.sems[sk], val)
        if fn is not None:
            ins = fn(eng)
            ins.then_inc(self.sems[inc[0]], inc[1])

    def dma(self, queue, out_ap, in_ap, reads=(), writes=(), sem=None, out=False, **kw):
        self._sem(sem)
        waits = self._deps(queue, reads, writes, skip_waw_sem=sem)
        self.cnt[sem] += 16
        ev = (sem, self.cnt[sem], 'dma')
        self._record(ev, reads, writes)
        self._emit(queue, waits, lambda e: e.dma_start(out=out_ap, in_=in_ap, **kw), (sem, 16))
        if out and sem not in self.out_sems:
            self.out_sems.append(sem)

    def idma(self, out_ap, in_ap, idx_ap, scatter, reads=(), writes=(), sem=None, bounds=None):
        self._sem(sem)
        waits = self._deps('pool', reads, writes, skip_waw_sem=sem)
        self.cnt[sem] += 16
        ev = (sem, self.cnt[sem], 'dma')
        self._record(ev, reads, writes)
        off = bass.IndirectOffsetOnAxis(ap=idx_ap, axis=0)
        if scatter:
            fn = lambda e: e.indirect_dma_start(out=out_ap, out_offset=off, in_=in_ap, in_offset=None)
        else:
            if bounds is None:
                fn = lambda e: e.indirect_dma_start(out=out_ap, out_offset=None, in_=in_ap, in_offset=off)
            else:
                if getattr(self, 'bnd_reg', None) is None:
                    self.bnd_reg = self.nc.gpsimd.alloc_register('bnd')
                    self.nc.gpsimd.reg_mov(self.bnd_reg, bounds)
                reg = self.bnd_reg
                fn = lambda e: e.indirect_dma_start(out=out_ap, out_offset=None, in_=in_ap, in_offset=off,
                                                    bounds_check=reg, oob_is_err=False)
        self._emit('pool', waits, fn, (sem, 16))

    def barrier(self):
        for e in self.CE + ['sp']:
            waits = []
            for sk, c in self.cnt.items():
                if c > 0 and self.seen[e].get(sk, 0) < c and not (sk == e):
                    self.seen[e][sk] = c
                    waits.append((sk, c))
            if waits:
                self._emit(e, waits, None, None)
        for e in self.CE:
            if self.cnt[e] > 0 and self.seen[e].get(e, 0) < self.cnt[e]:
                self.seen[e][e] = self.cnt[e]
                self._emit(e, [(e, self.cnt[e])], None, None)

    def finish(self):
        self.barrier()


S = 2048
D = 1024
NT = 16
ALPHA = (2 * 2) ** 0.25
LN_EPS = 1e-5
NEG = -30000.0
BS = 384
QT = BS // 128
NB = 27
I32 = mybir.dt.int32


def _na_key_tiles(i):
    if i < 2:
        return [0, 1, 2, 3], (0 if i == 0 else 4)
    if i > 13:
        return [12, 13, 14, 15], (13 if i == 14 else 17)
    return [i - 2, i - 1, i, i + 1, i + 2], 8


class Builder:
    def __init__(self, stage):
        self.stage = stage
        self.nc = nc = bass.Bass("TRN2", target_bir_lowering=False)

        def din(name, shape):
            return nc.dram_tensor(name, list(shape), F32, kind="ExternalInput").ap()
        self.x = din("x", [S, D])
        self.mem = din("mem", [256, D])
        self.lnp = din("lnp", [5, 128, 2 * D])
        self.wmkv = din("w_mem_kv", [D, 512])
        self.rw = din("router_w", [D, 16])
        self.rb = din("router_b", [128, 16])
        self.na_w = din("na_w_in", [D, 2560])
        self.nab = din("na_bias", [12, 128, 21 * 128])
        self.ml_w = din("ml_w_in", [D, 1808])
        self.cwb = din("ml_cwb", [128, 64])
        self.wqkv = din("ml_w_qkv", [3, 4, 192, 192])
        self.gb = din("ml_gate_b", [128, 16])
        self.ngs = din("ml_ngs", [128, 2 * 768])
        self.wout = din("w_out", [2, D, D])
        self.wg = din("exp_w_gate", [4 * 2048, 2048])
        self.wu = din("exp_w_up", [4 * 2048, 2048])
        self.wd = din("exp_w_down", [4 * 2048, 2048])
        self.consts = din("consts", [128, 6 * 128])
        self.consts2 = din("consts2", [128, NB * 16 + 1])
        self.y = nc.dram_tensor("y", [S, D], F32, kind="ExternalOutput").ap()
        self.xs = nc.dram_tensor("xs", [S, D], F32, kind="Internal").ap()
        self.xsorted = nc.dram_tensor("xsorted", [NB * BS, D], BF16, kind="Internal").ap()
        self.ysorted = nc.dram_tensor("ysorted", [NB * BS, D], F32, kind="Internal").ap()

    def xkb(self, tb):
        return ['xT%d' % t for t in range(tb * 4, tb * 4 + 4)]

    def sb(self, st, name, shape, dt):
        self._uid = getattr(self, '_uid', 0) + 1
        return st.enter_context(self.nc.sbuf_tensor("%s_%d" % (name, self._uid), list(shape), dt))

    def mm(self, out, lhsT, rhs, start, stop, reads, writes):
        self.P.op('pe', lambda e: e.matmul(out, lhsT, rhs, start=start, stop=stop), reads, writes)

    def tr(self, out, in_, ident, reads, writes):
        self.P.op('pe', lambda e: e.transpose(out, in_, ident), reads, writes)

    def act(self, out, in_, func, reads, writes, bias=0.0, scale=1.0, eng='act'):
        self.P.op('act', lambda e: e.activation(out, in_, func, bias=bias, scale=scale), reads, writes)

    def copy(self, eng, out, in_, reads, writes):
        if eng == 'act':
            self.P.op('act', lambda e: e.copy(out, in_), reads, writes)
        else:
            self.P.op(eng, lambda e: e.tensor_copy(out, in_), reads, writes)

    def tt(self, eng, out, a, b, op, reads, writes):
        self.P.op(eng, lambda e: e.tensor_tensor(out, a, b, op), reads, writes)

    def ts(self, eng, out, a, s1, s2, op0, op1, reads, writes):
        if op1 is None:
            self.P.op(eng, lambda e: e.tensor_scalar(out, a, s1, None, op0), reads, writes)
        else:
            self.P.op(eng, lambda e: e.tensor_scalar(out, a, s1, s2, op0, op1), reads, writes)

    def stt(self, out, a, s, b, op0, op1, reads, writes):
        self.P.op('dve', lambda e: e.scalar_tensor_tensor(out, a, s, b, op0, op1), reads, writes)

    def load_cast(self, dst_ap, src_ap, dst_key, nparts, shape):
        self.P.dma('pool', dst_ap, src_ap, writes=[dst_key], sem='lc_' + dst_key)

    def ln_stages(self, src, dst, src_key, dst_key, lnp_key, slot):
        P = self.P
        st6 = self.ln_st[:, slot * 12:(slot + 1) * 12]
        mv = self.ln_mv[:, slot * 8:(slot + 1) * 8]
        kst, kmv = 'ln_st%d' % slot, 'ln_mv%d' % slot

        def sA():
            for hlf in range(2):
                P.op('dve', lambda e, hlf=hlf: e.bn_stats(st6[:, hlf * 6:(hlf + 1) * 6], src[:, hlf * 512:(hlf + 1) * 512]),
                     [src_key], [kst])
            P.op('dve', lambda e: e.bn_aggr(mv[:, 0:2], st6[:, 0:12]), [kst], [kmv])

        def sB():
            self.act(mv[:, 2:3], mv[:, 1:2], AF.Sqrt, [kmv], [kmv], bias=self.eps_ap[:, 0:1])

        def sC():
            P.op('dve', lambda e: e.reciprocal(mv[:, 3:4], mv[:, 2:3]), [kmv], [kmv])
            self.ts('dve', mv[:, 4:5], mv[:, 0:1], mv[:, 3:4], -1.0, ALU.mult, ALU.mult, [kmv], [kmv])

        def sD():
            P.op('act', lambda e: e.activation(src, src, AF.Identity, bias=mv[:, 4:5], scale=mv[:, 3:4]), [src_key, kmv], [src_key])

        def sE():
            self.tt('dve', src, src, self.lnp_sb[:, 0:D], ALU.mult, [src_key, lnp_key], [src_key])
            self.tt('dve', dst, src, self.lnp_sb[:, D:2 * D], ALU.add, [src_key, lnp_key], [dst_key])
        return [sA, sB, sC, sD, sE]

    def ln_multi(self, n, tile_fn, lnp_key, pre=None, post=None):
        stages = {}
        for i in range(n + 4):
            if i < n:
                if pre is not None:
                    pre(i)
                stages[i] = self.ln_stages(*tile_fn(i), lnp_key, i % 4)
            for k in range(5):
                t = i - k
                if 0 <= t < n:
                    stages[t][k]()
                    if k == 4 and post is not None:
                        post(t)

    def transpose_tile(self, src_bf, src_key, dstT, dst_key, tt, bank):
        pst = self.psb[bank]
        for c in range(8):
            self.tr(pst[:, c * 128:(c + 1) * 128], src_bf[:, c * 128:(c + 1) * 128], self.ident_bf[:],
                    [src_key, 'ident_bf'], ['ps%d' % bank])
        eng = 'act' if (tt % 2 == 0) else 'dve'
        self.copy(eng, dstT[:, :, tt * 128:(tt + 1) * 128], pst[:, 0:1024].rearrange("p (c n) -> p c n", n=128),
                  ['ps%d' % bank], ['%s%d' % (dst_key, tt)])

    def mem_attention(self, w_ap_cols):
        with ExitStack() as t2:
            self.wqm = self.sb(t2, "wqm", [128, 8, 256], BF16)
            self.qmT = self.sb(t2, "qmT", [64, S], BF16)
            self.pmem = [self.sb(t2, "pmem%d" % i, [128, 2, 512], BF16) for i in range(2)]
            self.rd4 = [self.sb(t2, "rd4%d" % i, [128, 4, 1], F32) for i in range(2)]
            self._mem_attention(w_ap_cols)
            self.P.barrier()

    def _mem_attention(self, w_ap_cols):
        P = self.P
        wqm = self.wqm
        self.load_cast(wqm[:], w_ap_cols.rearrange("(c p) n -> p c n", p=128), 'wqm', 128, (8, 256))
        for h in range(4):
            for tb in range(4):
                bank = 6 + (tb % 2)
                for c in range(8):
                    self.mm(self.ps[bank][0:64, :], wqm[:, c, h * 64:(h + 1) * 64], self.xT[:, c, tb * 512:(tb + 1) * 512],
                            c == 0, c == 7, ['wqm'] + self.xkb(tb), ['ps%d' % bank])
                self.copy('act' if tb % 2 == 0 else 'dve', self.qmT[0:64, tb * 512:(tb + 1) * 512], self.ps[bank][0:64, :],
                          ['ps%d' % bank], ['qmT'])
            for tb in range(4):
                pt = self.pmem[tb % 2]
                ptk = 'pmem%d' % (tb % 2)
                for mt in range(2):
                    bank = 0 + mt
                    self.mm(self.ps[bank][:, :], self.mkT[0:64, h, mt * 128:(mt + 1) * 128],
                            self.qmT[0:64, tb * 512:(tb + 1) * 512], True, True, ['mkT', 'qmT'], ['ps%d' % bank])
                    self.act(pt[:, mt, :], self.ps[bank][:, :], AF.Exp, ['ps%d' % bank], [ptk], scale=0.125)
                bank = 4 + (tb % 2)
                for q in range(4):
                    for mt in range(2):
                        self.mm(self.ps[bank][:, q * 65:q * 65 + 65], pt[:, mt, q * 128:(q + 1) * 128], self.mv[:, mt, h, :],
                                mt == 0, mt == 1, [ptk, 'mv'], ['ps%d' % bank])
                pv = self.ps[bank][:, 0:260].rearrange("p (q e) -> p q e", e=65)
                rd4 = self.rd4[tb % 2]
                P.op('dve', lambda e, pv=pv, rd4=rd4: e.reciprocal(rd4[:], pv[:, :, 64:65]), ['ps%d' % bank], ['rd4%d' % (tb % 2)])
                self.tt('dve', self.mixed[:, tb * 4:(tb + 1) * 4, 768 + h * 64:768 + (h + 1) * 64], pv[:, :, 0:64],
                        rd4[:].to_broadcast([128, 4, 64]), ALU.mult, ['ps%d' % bank, 'rd4%d' % (tb % 2)], ['mixed'])

    def out_proj_ln(self, li, res_src, post=None):
        P = self.P
        for hlf in range(2):
            for kh in range(2):
                self.load_cast(self.wo[:, kh * 4:(kh + 1) * 4, hlf * 512:(hlf + 1) * 512],
                               self.wout[li][kh * 512:(kh + 1) * 512, hlf * 512:(hlf + 1) * 512].rearrange("(c p) n -> p c n", p=128),
                               'wo', 128, (4, 512))
        P.dma('sp', self.lnp_sb[:], self.lnp[1 + 2 * li], writes=['lnp'], sem='lnp')

        def pre(tt):
            xr = self.xres[tt % 2]
            xrk = 'xres%d' % (tt % 2)
            P.dma('sp', xr[:], res_src[tt * 128:(tt + 1) * 128, :], writes=[xrk], sem=xrk)
            for hlf in range(2):
                bank = hlf + 2 * (tt % 2)
                for c in range(8):
                    self.mm(self.ps[bank][:, :], self.xT[:, c, tt * 128:(tt + 1) * 128], self.wo[:, c, hlf * 512:(hlf + 1) * 512],
                            c == 0, c == 7, ['xT%d' % tt, 'wo'], ['ps%d' % bank])
                self.stt(self.X[:, tt, hlf * 512:(hlf + 1) * 512], xr[:, hlf * 512:(hlf + 1) * 512], ALPHA,
                         self.ps[bank][:, :], ALU.mult, ALU.add, [xrk, 'ps%d' % bank], ['X%d' % tt])
        self.ln_multi(NT, lambda tt: (self.X[:, tt, :], self.X[:, tt, :], 'X%d' % tt, 'X%d' % tt), 'lnp', pre=pre, post=post)

    def route_prep(self):
        P = self.P
        P.dma('sp', self.rw_sb[:], self.rw.rearrange("(c p) n -> p c n", p=128), writes=['rw'], sem='rw')
        P.dma('sp', self.rb_sb[:], self.rb, writes=['rb'], sem='rb')

    def route_tile(self, tt):
        pp = tt % 2
        for c in range(8):
            bank = 4 + 2 * pp + (0 if c < 4 else 1)
            self.mm(self.ps[bank][:, (c % 4) * 128:(c % 4 + 1) * 128], self.X[:, tt, c * 128:(c + 1) * 128],
                    self.ident_f, True, True, ['X%d' % tt, 'ident_f'], ['ps%d' % bank])
        xtf = self.xtf[pp]
        for b_ in range(2):
            bank = 4 + 2 * pp + b_
            self.copy('act' if b_ == 0 else 'dve', xtf[:, b_ * 4:(b_ + 1) * 4, :],
                      self.ps[bank][:, :].rearrange("p (c n) -> p c n", n=128), ['ps%d' % bank], ['xtf%d' % pp])
        for c in range(8):
            self.mm(self.ps[4 + 2 * pp][:, 0:16], xtf[:, c, :], self.rw_sb[:, c, :], c == 0, c == 7, ['xtf%d' % pp, 'rw'], ['ps%d' % (4 + 2 * pp)])
        self.copy('act', self.lg[:, tt, :], self.ps[4 + 2 * pp][:, 0:16], ['ps%d' % (4 + 2 * pp)], ['lg'])

    def moe_route(self, li):
        P = self.P
        lg = self.lg
        sc, bi, t1, t2, t3 = self.r_sc, self.r_bi, self.r_t1, self.r_t2, self.r_t3
        self.act(sc[:], lg[:], AF.Sigmoid, ['lg'], ['r_sc'])
        self.tt('dve', bi[:], sc[:], self.rb_sb[:].unsqueeze(1).to_broadcast([128, NT, 16]), ALU.add, ['r_sc', 'rb'], ['r_bi'])
        g4 = lambda t: t[:].rearrange("p t (g k) -> p (t g) k", k=4)
        m1, m2, gs = self.r_m1, self.r_m2, self.r_gs
        P.op('dve', lambda e: e.tensor_reduce(m1[:], g4(bi), AX.X, ALU.max), ['r_bi'], ['r_m1'])
        self.tt('dve', g4(t1), g4(bi), m1[:].to_broadcast([128, 64, 4]), ALU.is_equal, ['r_bi', 'r_m1'], ['r_t1'])
        self.stt(g4(t2), g4(t1), NEG, g4(bi), ALU.mult, ALU.add, ['r_t1', 'r_bi'], ['r_t2'])
        P.op('dve', lambda e: e.tensor_reduce(m2[:], g4(t2), AX.X, ALU.max), ['r_t2'], ['r_m2'])
        self.tt('dve', g4(t3), g4(t2), m2[:].to_broadcast([128, 64, 4]), ALU.is_equal, ['r_t2', 'r_m2'], ['r_t3'])
        self.tt('dve', gs[:], m1[:], m2[:], ALU.add, ['r_m1', 'r_m2'], ['r_gs'])
        gsv = gs[:].rearrange("p (t g) o -> p t (g o)", g=4)
        P.op('dve', lambda e: e.tensor_reduce(self.r_gm[:], gsv, AX.X, ALU.max), ['r_gs'], ['r_gm'])
        self.tt('dve', gsv, gsv, self.r_gm[:].to_broadcast([128, NT, 4]), ALU.is_equal, ['r_gs', 'r_gm'], ['r_gs'])
        self.tt('dve', g4(t1), g4(t1), gs[:].to_broadcast([128, 64, 4]), ALU.mult, ['r_t1', 'r_gs'], ['r_t1'])
        self.tt('dve', g4(t3), g4(t3), gs[:].to_broadcast([128, 64, 4]), ALU.mult, ['r_t3', 'r_gs'], ['r_t3'])
        wk = self.wk
        for k, sel in enumerate((t1, t3)):
            self.tt('dve', t2[:], sel[:], sc[:], ALU.mult, ['r_t1', 'r_t3', 'r_sc'], ['r_t2'])
            P.op('dve', lambda e, k=k: e.tensor_reduce(wk[:, :, k:k + 1], t2[:], AX.X, ALU.add), ['r_t2'], ['wk'])
        P.op('dve', lambda e: e.tensor_reduce(self.r_gm[:], wk[:], AX.X, ALU.add), ['wk'], ['r_gm'])
        P.op('dve', lambda e: e.reciprocal(self.r_gm[:], self.r_gm[:]), ['r_gm'], ['r_gm'])
        self.tt('dve', wk[:], wk[:], self.r_gm[:].to_broadcast([128, NT, 2]), ALU.mult, ['wk', 'r_gm'], ['wk'])
        sel = t2
        self.tt('dve', sel[:], t1[:], t3[:], ALU.add, ['r_t1', 'r_t3'], ['r_t2'])
        sel2d = sel[:].rearrange("p t e -> p (t e)")
        wi, to, cx = self.r_wi, self.r_to, self.r_cx
        self.tt('dve', self.Lst[:], self.Uf, self.ident_f, ALU.subtract, ['cst'], ['Lst'])
        self.mm(self.ps[5][:, 0:256], self.Lst[:], sel2d, True, True, ['Lst', 'r_t2'], ['ps5'])
        self.mm(self.ps[6][:, 0:256], self.ones_f, sel2d, True, True, ['cst', 'r_t2'], ['ps6'])
        self.copy('dve', wi[:].rearrange("p t e -> p (t e)"), self.ps[5][:, 0:256], ['ps5'], ['r_wi'])
        self.copy('dve', to[:].rearrange("p t e -> p (t e)"), self.ps[6][:, 0:256], ['ps6'], ['r_to'])
        P.op('dve', lambda e: e.memset(cx[:, 0, :], 0.0), [], ['r_cx'])
        for tt in range(1, NT):
            self.tt('dve', cx[:, tt, :], cx[:, tt - 1, :], to[:, tt - 1, :], ALU.add, ['r_cx', 'r_to'], ['r_cx'])
        cnt, nbk, pend = self.r_cnt, self.r_nbk, self.r_pend
        self.tt('dve', cnt[:], cx[:, NT - 1, :], to[:, NT - 1, :], ALU.add, ['r_cx', 'r_to'], ['r_cnt'])
        self.ts('dve', nbk[:], cnt[:], 0.0, None, ALU.is_gt, None, ['r_cnt'], ['r_nbk'])
        for k in range(1, -(-S // BS)):
            self.stt(nbk[:], cnt[:], float(BS * k), nbk[:], ALU.is_gt, ALU.add, ['r_cnt', 'r_nbk'], ['r_nbk'])
        self.copy('dve', pend[:, 0:1], nbk[:, 0:1], ['r_nbk'], ['r_pend'])
        for e_ in range(1, 16):
            self.tt('dve', pend[:, e_:e_ + 1], pend[:, e_ - 1:e_], nbk[:, e_:e_ + 1], ALU.add, ['r_pend', 'r_nbk'], ['r_pend'])
        self.tt('dve', cnt[:], pend[:], nbk[:], ALU.subtract, ['r_pend', 'r_nbk'], ['r_cnt'])
        self.ts('dve', cnt[:], cnt[:], float(BS), None, ALU.mult, None, ['r_cnt'], ['r_cnt'])
        self.tt('dve', wi[:], wi[:], cx[:], ALU.add, ['r_wi', 'r_cx'], ['r_wi'])
        self.tt('dve', wi[:], wi[:], cnt[:].unsqueeze(1).to_broadcast([128, NT, 16]), ALU.add, ['r_wi', 'r_cnt'], ['r_wi'])
        posf = self.r_posf
        for k, sl in enumerate((t1, t3)):
            self.tt('dve', sl[:], sl[:], wi[:], ALU.mult, ['r_t1', 'r_t3', 'r_wi'], ['r_t1', 'r_t3'])
            P.op('dve', lambda e, k=k, sl=sl: e.tensor_reduce(posf[:, :, k:k + 1], sl[:], AX.X, ALU.add), ['r_t1', 'r_t3'], ['r_posf'])
        self.copy('dve', self.pos_i[:], posf[:], ['r_posf'], ['pos_i'])
        bg = self.bgrid
        self.tt('dve', bg[:, :, :], pend[:].unsqueeze(1).to_broadcast([128, NB, 16]), self.c2[:, 0:NB * 16].rearrange("p (b e) -> p b e", e=16),
                ALU.is_le, ['r_pend', 'c2'], ['bgrid'])
        P.op('dve', lambda e: e.tensor_reduce(self.r_be[:], bg[:], AX.X, ALU.add), ['bgrid'], ['r_be'])
        self.ts('dve', self.r_be2[:], self.r_be[:], 16.0, 1.0e6, ALU.is_ge, ALU.mult, ['r_be'], ['r_be2'])
        self.ts('dve', self.r_be[:], self.r_be[:], 128.0, self.c2[:, NB * 16:NB * 16 + 1], ALU.mult, ALU.add, ['r_be', 'c2'], ['r_be'])
        self.tt('dve', self.r_be[:], self.r_be[:], self.r_be2[:], ALU.add, ['r_be', 'r_be2'], ['r_be'])
        for hlf in range(2):
            self.ts('dve', self.r_be2[:], self.r_be[:], float((li * 2 + hlf) * 2048), None, ALU.add, None, ['r_be'], ['r_be2'])
            self.copy('dve', self.widx_i[:, hlf, :], self.r_be2[:].rearrange("p b o -> p (b o)"), ['r_be2'], ['widx_i'])
        for tt in range(NT):
            xb = self.xbf[tt % 2]
            xbk = 'xbf%d' % (tt % 2)
            self.copy('act', xb[:], self.X[:, tt, :], ['X%d' % tt], [xbk])
            for k in range(2):
                P.idma(self.xsorted, xb[:, :], self.pos_i[:, tt, k:k + 1], True, reads=[xbk, 'pos_i', 'xsorted'],
                       writes=['xsorted%d' % (tt % 2)], sem='xsc%d' % (tt % 2))
            self.P.op('act', lambda e, tt=tt: e.mul(self.X[:, tt, :], self.X[:, tt, :], ALPHA), ['X%d' % tt], ['X%d' % tt])

    def moe_experts(self, li):
        P = self.P
        wsrc = (self.wg, self.wu, self.wd)
        nb = NB if self.stage != 'moe_small' else 2
        def st_W(b):
            s = b % 2
            for m, (dst, key) in enumerate(((self.wgb[s], 'wg%d' % s), (self.wub[s], 'wu%d' % s), (self.wdb[s], 'wd%d' % s))):
                for hlf in range(2):
                    if m < 2:
                        dv = dst[:, hlf * 4:(hlf + 1) * 4, :].rearrange("p c n -> p (c n)")
                    else:
                        dv = dst[:, hlf * 2:(hlf + 1) * 2, :].rearrange("p c n -> p (c n)")
                    P.idma(dv, wsrc[m], self.widx_i[:, hlf, b:b + 1], False, reads=['widx_i'], writes=[key], sem='s' + key,
                           bounds=(4 * 2048 - 1) if b >= 2 else None)

        def st_X(b):
            s = b % 2
            xblk = self.xT[:, QT * s:QT * s + QT, 1024:2048]
            xTb = self.xT[:, :, s * BS:(s + 1) * BS]
            P.dma('sp', xblk, self.xsorted[b * BS:(b + 1) * BS, :].rearrange("(q p) n -> p q n", p=128), reads=['xsorted0', 'xsorted1'],
                  writes=['xblk%d' % s], sem='xblk%d' % s)
            for q in range(QT):
                bank = 6 + (q % 2)
                for c in range(8):
                    self.tr(self.psb[bank][:, c * 128:(c + 1) * 128], xblk[:, q, c * 128:(c + 1) * 128], self.ident_bf[:],
                            ['xblk%d' % s, 'ident_bf'], ['ps%d' % bank])
                self.copy('act' if q % 2 == 0 else 'dve', xTb[:, :, q * 128:(q + 1) * 128],
                          self.psb[bank][:, 0:1024].rearrange("p (c n) -> p c n", n=128), ['ps%d' % bank], ['xTb%d' % s])

        def st_C(b):
            s = b % 2
            wgb, wub, wdb = self.wgb[s], self.wub[s], self.wdb[s]
            xTb = self.xT[:, :, s * BS:(s + 1) * BS]
            hT = self.hT[s]
            hk = 'hT%d' % s
            for efc in range(4):
                bg = 0 + (efc % 2)
                bu = 2 + (efc % 2)
                for c in range(8):
                    self.mm(self.ps[bg][:, 0:BS], wgb[:, c, efc * 128:(efc + 1) * 128], xTb[:, c, :],
                            c == 0, c == 7, ['wg%d' % s, 'xTb%d' % s], ['ps%d' % bg])
                for c in range(8):
                    self.mm(self.ps[bu][:, 0:BS], wub[:, c, efc * 128:(efc + 1) * 128], xTb[:, c, :],
                            c == 0, c == 7, ['wu%d' % s, 'xTb%d' % s], ['ps%d' % bu])
                sg = self.sg[efc % 2]
                sgk = 'sg%d' % (efc % 2)
                self.act(sg[:, 0:BS], self.ps[bg][:, 0:BS], AF.Silu, ['ps%d' % bg], [sgk])
                self.tt('dve', hT[:, efc, 0:BS], sg[:, 0:BS], self.ps[bu][:, 0:BS], ALU.mult, [sgk, 'ps%d' % bu], [hk])
            for q in range(QT):
                ysb = self.ysb[q]
                yk = 'ysb%d' % q
                for hlf in range(2):
                    by = 4 + (q % 2) * 2 + hlf
                    for kc in range(4):
                        self.mm(self.ps[by][:, :], hT[:, kc, q * 128:(q + 1) * 128], wdb[:, kc, hlf * 512:(hlf + 1) * 512],
                                kc == 0, kc == 3, [hk, 'wd%d' % s], ['ps%d' % by])
                    self.copy('act' if hlf == 0 else 'dve', ysb[:, hlf * 512:(hlf + 1) * 512], self.ps[by][:, :], ['ps%d' % by], [yk])
                r0 = b * BS + q * 128
                P.dma('act', self.ysorted[r0:r0 + 128, :], ysb, reads=[yk, 'ysorted'], writes=['ysorted%d' % q], sem='yst%d' % q)

        st_X(0)
        st_W(0)
        for b in range(nb):
            if b + 1 < nb:
                st_X(b + 1)
                st_W(b + 1)
            st_C(b)

    def combine_tile(self, tt, yks):
        P = self.P
        for k in range(2):
            j = (tt % 4) * 2 + k
            yb = yks[j]
            ykk = 'yk%d' % j
            P.idma(yb[:, :], self.ysorted, self.pos_i[:, tt, k:k + 1], False,
                   reads=['ysorted0', 'ysorted1', 'ysorted2', 'ysorted3', 'pos_i'], writes=[ykk], sem=ykk)
            self.stt(self.X[:, tt, :], yb[:], self.wk[:, tt, k:k + 1], self.X[:, tt, :], ALU.mult, ALU.add,
                     [ykk, 'wk', 'X%d' % tt], ['X%d' % tt])

    def ln2(self, li, yks):
        P = self.P
        P.dma('sp', self.lnp_sb[:], self.lnp[2 + 2 * li], writes=['lnp'], sem='lnp')

        def post(tt):
            if li == 0:
                xb = self.xbf[tt % 2]
                xbk = 'xbf%d' % (tt % 2)
                self.copy('act', xb[:], self.X[:, tt, :], ['X%d' % tt], [xbk])
                P.dma('sp', self.xs[tt * 128:(tt + 1) * 128, :], self.X[:, tt, :], reads=['X%d' % tt], writes=['xs%d' % tt],
                      sem='xs_st%d' % (tt % 2))
                self.transpose_tile(xb[:], xbk, self.xT, 'xT', tt, 6 + (tt % 2))
            else:
                P.dma('sp', self.y[tt * 128:(tt + 1) * 128, :], self.X[:, tt, :], reads=['X%d' % tt], sem='y_st%d' % (tt % 2),
                      out=True)
        self.ln_multi(NT, lambda tt: (self.X[:, tt, :], self.X[:, tt, :], 'X%d' % tt, 'X%d' % tt), 'lnp',
                      pre=lambda tt: self.combine_tile(tt, yks), post=post)

    def build(self):
        nc = self.nc
        with ExitStack() as st:
            self.P = P = Prog(nc, st)
            sb = lambda n, s, d: self.sb(st, n, s, d)
            self.ps = [st.enter_context(nc.psum_tensor("ps%d" % i, [128, 512], F32)) for i in range(8)]
            self.psb = [p[:].bitcast(BF16) for p in self.ps]
            cst = sb("cst", [128, 6, 128], F32)
            self.ident_f = cst[:, 0, :]
            self.Uf, self.Ub, self.mnf, self.mnb, self.ones_f = (cst[:, i, :] for i in range(1, 6))
            self.ident_bf = sb("ident_bf", [128, 128], BF16)
            self.eps_ap = sb("eps", [128, 1], F32)
            self.ln_st = sb("ln_st", [128, 4 * 12], F32)
            self.ln_mv = sb("ln_mv", [128, 4 * 8], F32)
            self.lnp_sb = sb("lnp_sb", [128, 2 * D], F32)
            self.rden = sb("rden", [128, 1], F32)
            self.xT = sb("xT", [128, 8, S], BF16)
            self.mkT = sb("mkT", [64, 4, 256], BF16)
            self.mv = sb("mv", [128, 2, 4, 65], BF16)
            P.dma('sp', cst[:], self.consts.rearrange("p (k n) -> p k n", n=128), writes=['cst'], sem='cst')
            self.copy('dve', self.ident_bf[:], cst[:, 0, :], ['cst'], ['ident_bf'])
            P.op('dve', lambda e: e.memset(self.eps_ap[:], LN_EPS), [], ['eps'])
            self.last_w_alias()
            self.mem_kv()
            self.layer0_mixer()
            P.barrier()
            if self.post(0):
                return
            self.layer1_mixer()
            P.barrier()
            self.post(1)
            P.finish()

    def post(self, li):
        P = self.P
        nc = self.nc
        with ExitStack() as ph:
            sb = lambda n, s_, d: self.sb(ph, n, s_, d)
            self.X = sb("X", [128, NT, D], F32)
            self.xbf = [sb("xbf%d" % i, [128, D], BF16) for i in range(2)]
            self.wk = sb("wk", [128, NT, 2], F32)
            self.pos_i = sb("pos_i", [128, NT, 2], I32)
            self.widx_i = sb("widx_i", [128, 2, NB], I32)
            with ExitStack() as pa:
                sb = lambda n, s_, d: self.sb(pa, n, s_, d)
                self.wo = sb("wo", [128, 8, D], BF16)
                self.xres = [sb("xres%d" % i, [128, D], F32) for i in range(2)]
                self.rw_sb = sb("rw_sb", [128, 8, 16], F32)
                self.rb_sb = sb("rb_sb", [128, 16], F32)
                self.xtf = [sb("xtf%d" % i, [128, 8, 128], F32) for i in range(2)]
                self.lg = sb("lg", [128, NT, 16], F32)
                for n in ['r_sc', 'r_bi', 'r_t1', 'r_t2', 'r_t3']:
                    setattr(self, n, sb(n, [128, NT, 16], F32))
                for n in ['r_m1', 'r_m2', 'r_gs']:
                    setattr(self, n, sb(n, [128, NT * 4, 1], F32))
                self.r_gm = sb("r_gm", [128, NT, 1], F32)
                for n in ['r_wi', 'r_to', 'r_cx']:
                    setattr(self, n, sb(n, [128, NT, 16], F32))
                for n in ['r_cnt', 'r_nbk', 'r_pend']:
                    setattr(self, n, sb(n, [128, 16], F32))
                self.r_posf = sb("r_posf", [128, NT, 2], F32)
                self.Lst = sb("Lst", [128, 128], F32)
                self.bgrid = sb("bgrid", [128, NB, 16], F32)
                self.r_be = sb("r_be", [128, NB, 1], F32)
                self.r_be2 = sb("r_be2", [128, NB, 1], F32)
                self.c2 = sb("c2", [128, NB * 16 + 1], F32)
                P.dma('sp', self.c2[:], self.consts2, writes=['c2'], sem='c2')
                self.route_prep()
                self.out_proj_ln(li, self.x if li == 0 else self.xs, post=None if self.stage == 'l0_ln1' else self.route_tile)
                if self.stage == 'l0_ln1' or (self.stage in ('l1_ln1', 'ml_small') and li == 1):
                    self.dump_X()
                    return True
                self.moe_route(li)
                if self.stage in ('l0_route',):
                    self.dump_X()
                    return True
                P.barrier()
            with ExitStack() as pb:
                sb = lambda n, s_, d: self.sb(pb, n, s_, d)
                self.wgb = [sb("wgb%d" % i, [128, 8, 512], BF16) for i in range(2)]
                self.wub = [sb("wub%d" % i, [128, 8, 512], BF16) for i in range(2)]
                self.wdb = [sb("wdb%d" % i, [128, 4, 1024], BF16) for i in range(2)]
                self.hT = [sb("hT%d" % i, [128, 4, 512], BF16) for i in range(2)]
                self.sg = [sb("sg%d" % i, [128, 512], F32) for i in range(2)]
                self.ysb = [sb("ysb%d" % i, [128, D], F32)[:] for i in range(4)]
                self.moe_experts(li)
                P.barrier()
            with ExitStack() as pc:
                yks = [self.sb(pc, "yk%d" % i, [128, D], F32) for i in range(8)]
                if self.stage in ('l0', 'moe_small'):
                    for tt in range(NT):
                        self.combine_tile(tt, yks)
                    self.dump_X()
                    return True
                self.ln2(li, yks)
                P.barrier()
        return False

    def last_w_alias(self):
        for k in ['ident_f']:
            self.P.last_w[k] = self.P.last_w['cst']

    def dump_X(self):
        P = self.P
        for tt in range(NT):
            P.dma('sp', self.y[tt * 128:(tt + 1) * 128, :], self.X[:, tt, :], reads=['X%d' % tt], sem='y_st%d' % (tt % 2), out=True)
        P.finish()

    def mem_kv(self):
        P = self.P
        with ExitStack() as ph:
            memsb = self.sb(ph, "memsb", [128, 2, D], F32)
            membf = self.sb(ph, "membf", [128, 2, D], BF16)
            memT = self.sb(ph, "memT", [128, 8, 256], BF16)
            wkv = self.sb(ph, "wkv", [128, 8, 512], BF16)
            zt = self.sb(ph, "zt", [128, 8, D], BF16)
            P.op('pool', lambda e: e.memset(zt[:], 0.0), [], ['zt'])
            nrow = NB * BS
            for r0 in range(0, nrow, 1024):
                nq = min(1024, nrow - r0) // 128
                P.dma('act', self.xsorted[r0:r0 + nq * 128, :].rearrange("(q p) n -> p q n", p=128), zt[:, 0:nq, :], reads=['zt'],
                      writes=['xsorted'], sem='xsz')
            P.dma('sp', self.lnp_sb[:], self.lnp[0], writes=['lnp'], sem='lnp')
            for mt in range(2):
                P.dma('sp', memsb[:, mt, :], self.mem[mt * 128:(mt + 1) * 128, :], writes=['memsb%d' % mt], sem='memsb%d' % mt)
                for f in self.ln_stages(memsb[:, mt, :], membf[:, mt, :], 'memsb%d' % mt, 'membf%d' % mt, 'lnp', mt):
                    f()
                self.transpose_tile(membf[:, mt, :], 'membf%d' % mt, memT, 'memT', mt, 6 + mt)
            for hlf in range(2):
                self.load_cast(wkv[:, hlf * 4:(hlf + 1) * 4, :],
                               self.wmkv[hlf * 512:(hlf + 1) * 512, :].rearrange("(c p) n -> p c n", p=128), 'wkv', 128, (4, 512))
            for h in range(4):
                bank = h % 2
                for c in range(8):
                    self.mm(self.ps[bank][0:64, 0:256], wkv[:, c, h * 64:(h + 1) * 64], memT[:, c, :], c == 0, c == 7,
                            ['wkv', 'memT0', 'memT1'], ['ps%d' % bank])
                self.copy('act', self.mkT[:, h, :], self.ps[bank][0:64, 0:256], ['ps%d' % bank], ['mkT'])
            P.op('pool', lambda e: e.memset(self.mv[:, :, :, 64:65], 1.0), [], ['mv'])
            for mt in range(2):
                bank = 2 + mt
                for c in range(8):
                    self.mm(self.ps[bank][:, 0:256], memT[:, c, mt * 128:(mt + 1) * 128], wkv[:, c, 256:512], c == 0, c == 7,
                            ['wkv', 'memT%d' % mt], ['ps%d' % bank])
                self.copy('dve', self.mv[:, mt, :, 0:64], self.ps[bank][:, 0:256].rearrange("p (h d) -> p h d", d=64),
                          ['ps%d' % bank], ['mv'])
            P.barrier()

    def layer0_mixer(self):
        P = self.P
        with ExitStack() as ph:
            sb = lambda n, s_, d: self.sb(ph, n, s_, d)
            self.mixed = sb("mixed", [128, NT, D], BF16)
            vaug = sb("vaug", [128, NT, 12, 65], BF16)
            wqk = sb("wqk", [128, 8, 1536], BF16)
            qT = [sb("qT%d" % i, [128, S], BF16) for i in range(2)]
            kT = [sb("kT%d" % i, [128, S], BF16) for i in range(2)]
            s_sb = [sb("s_sb%d" % i, [128, 640], F32) for i in range(2)]
            p_bf = [sb("p_bf%d" % i, [128, 640], BF16) for i in range(2)]
            with ExitStack() as t1:
                sb1 = lambda n, s_, d: self.sb(t1, n, s_, d)
                xin = [sb1("xin%d" % i, [128, D], F32) for i in range(2)]
                xinb = [sb1("xinb%d" % i, [128, D], BF16) for i in range(2)]
                wv = sb1("wv", [128, 8, 768], BF16)
                for tt in range(NT):
                    i = tt % 2
                    P.dma('sp', xin[i][:], self.x[tt * 128:(tt + 1) * 128, :], writes=['xin%d' % i], sem='xin%d' % i)
                    self.copy('act' if tt % 2 == 0 else 'dve', xinb[i][:], xin[i][:], ['xin%d' % i], ['xinb%d' % i])
                    self.transpose_tile(xinb[i][:], 'xinb%d' % i, self.xT, 'xT', tt, 6 + i)
                for blk in range(3):
                    for hlf in range(2):
                        self.load_cast(wqk[:, hlf * 4:(hlf + 1) * 4, blk * 512:(blk + 1) * 512],
                                       self.na_w[hlf * 512:(hlf + 1) * 512, blk * 512:(blk + 1) * 512].rearrange("(c p) n -> p c n", p=128),
                                       'wqk', 128, (4, 512))
                for c0, n in ((0, 512), (512, 256)):
                    for hlf in range(2):
                        self.load_cast(wv[:, hlf * 4:(hlf + 1) * 4, c0:c0 + n],
                                       self.na_w[hlf * 512:(hlf + 1) * 512, 1536 + c0:1536 + c0 + n].rearrange("(c p) n -> p c n", p=128),
                                       'wv', 128, (4, n))
                P.op('pool', lambda e: e.memset(vaug[:, :, :, 64:65], 1.0), [], ['vaug'])
                for tt in range(NT):
                    for bi, (c0, n) in enumerate(((0, 512), (512, 256))):
                        bank = bi + 2 * (tt % 2)
                        for c in range(8):
                            self.mm(self.ps[bank][:, 0:n], self.xT[:, c, tt * 128:(tt + 1) * 128], wv[:, c, c0:c0 + n], c == 0, c == 7,
                                    ['xT%d' % tt, 'wv'], ['ps%d' % bank])
                        h0 = c0 // 64
                        self.copy('act' if bi == 0 else 'dve', vaug[:, tt, h0:h0 + n // 64, 0:64],
                                  self.ps[bank][:, 0:n].rearrange("p (h d) -> p h d", d=64), ['ps%d' % bank], ['vaug'])
                P.barrier()
            self.mem_attention(self.na_w[:, 2304:2560])
            with ExitStack() as t3:
                bias = [self.sb(t3, "bias%d" % i, [128, 21, 128], BF16) for i in range(2)]
                rdn = [self.sb(t3, "rdn%d" % i, [128, 1], F32) for i in range(2)]
                nh = 12 if self.stage != 'na_small' else 1
                for h in range(nh):
                    hb = h % 2
                    P.dma('pool', bias[hb][:], self.nab[h].rearrange("p (k n) -> p k n", n=128), writes=['bias%d' % hb], sem='bias%d' % hb)
                    pp = (h // 2) % 2
                    psl = slice(64 * (h % 2), 64 * (h % 2) + 64)
                    if h % 2 == 0:
                        for which, dst, col0, scale in (('q', qT[pp], h * 64, 0.125), ('k', kT[pp], 768 + h * 64, 1.0)):
                            for tb in range(4):
                                bank = 6 + (tb % 2)
                                for c in range(8):
                                    self.mm(self.ps[bank][:, :], wqk[:, c, col0:col0 + 128], self.xT[:, c, tb * 512:(tb + 1) * 512],
                                            c == 0, c == 7, ['wqk'] + self.xkb(tb), ['ps%d' % bank])
                                self.act(dst[:, tb * 512:(tb + 1) * 512], self.ps[bank][:, :], AF.Copy, ['ps%d' % bank],
                                         ['%sT%d' % (which, pp)], scale=scale)
                    def st_S(i):
                        kts, slot = _na_key_tiles(i)
                        pb = i % 2
                        ba, bb = 2 * pb, 2 * pb + 1
                        nk = len(kts)
                        n4 = min(nk, 4)
                        self.mm(self.ps[ba][:, 0:n4 * 128], self.ident_bf[:], bias[hb][:, slot:slot + n4, :].rearrange("p k n -> p (k n)"),
                                True, False, ['ident_bf', 'bias%d' % hb], ['ps%d' % ba])
                        if nk == 5:
                            self.mm(self.ps[bb][:, 0:128], self.ident_bf[:], bias[hb][:, slot + 4, :],
                                    True, False, ['ident_bf', 'bias%d' % hb], ['ps%d' % bb])
                        for j, kt in enumerate(kts):
                            bank, off = (ba, j * 128) if j < 4 else (bb, 0)
                            self.mm(self.ps[bank][:, off:off + 128], kT[pp][psl, kt * 128:(kt + 1) * 128], qT[pp][psl, i * 128:(i + 1) * 128],
                                    False, (j == n4 - 1) or (j == 4), ['kT%d' % pp, 'qT%d' % pp], ['ps%d' % bank])

                    def st_mid(i):
                        kts, slot = _na_key_tiles(i)
                        nk = len(kts)
                        pb = i % 2
                        ba, bb = 2 * pb, 2 * pb + 1
                        n4 = min(nk, 4)
                        self.act(p_bf[pb][:, 0:n4 * 128], self.ps[ba][:, 0:n4 * 128], AF.Exp, ['ps%d' % ba], ['p_bf%d' % pb])
                        if nk == 5:
                            self.act(p_bf[pb][:, 512:640], self.ps[bb][:, 0:128], AF.Exp, ['ps%d' % bb], ['p_bf%d' % pb])

                    def st_PV(i):
                        kts, slot = _na_key_tiles(i)
                        nk = len(kts)
                        pb = i % 2
                        bo = 4 + pb
                        for j, kt in enumerate(kts):
                            self.mm(self.ps[bo][:, 0:65], p_bf[pb][:, j * 128:(j + 1) * 128], vaug[:, kt, h, :], j == 0, j == nk - 1,
                                    ['p_bf%d' % pb, 'vaug'], ['ps%d' % bo])
                        rd = rdn[pb]
                        P.op('dve', lambda e, bo=bo, rd=rd: e.reciprocal(rd[:, 0:1], self.ps[bo][:, 64:65]), ['ps%d' % bo], ['rdn%d' % pb])
                        self.ts('dve', self.mixed[:, i, h * 64:(h + 1) * 64], self.ps[bo][:, 0:64], rd[:, 0:1], None,
                                ALU.mult, None, ['ps%d' % bo, 'rdn%d' % pb], ['mixed'])

                    st_S(0)
                    st_S(1)
                    st_mid(0)
                    for i in range(NT):
                        if i + 2 < NT:
                            st_S(i + 2)
                        if i + 1 < NT:
                            st_mid(i + 1)
                        st_PV(i)
                P.barrier()
            self.mixed_to_xT()
            P.barrier()

    def mixed_to_xT(self):
        for tt in range(NT):
            self.transpose_tile(self.mixed[:, tt, :], 'mixed', self.xT, 'xT', tt, 6 + (tt % 2))

    def layer1_mixer(self):
        P = self.P
        KS = 192 ** -0.5
        CH = ((0, 128), (128, 64))
        with ExitStack() as ph:
            sb = lambda n, s_, d: self.sb(ph, n, s_, d)
            self.mixed = sb("mixed", [128, NT, D], BF16)
            gts = sb("gts", [128, NT, 16], F32)
            lf, ig, bcol, colb, eb, blast, wexp, ebl, colf = (sb(n, [128, NT, 2, 4], F32) for n in
                                                               ("lf", "ig", "bcol", "colb", "eb", "blast", "wexp", "ebl", "colf"))
            wqkv = sb("wqkv", [128, 4, 3, 2, 256], BF16)
            cwb = sb("cwb", [128, 4, 2, 8], F32)
            ngs = sb("ngs", [128, 2, 768], F32)
            gbs = sb("gbs", [128, 16], F32)
            wgt = sb("wgt", [128, 8, 16], BF16)
            st6 = sb("hst6", [128, 6], F32)
            hmv = sb("hmv", [128, 4], F32)
            P.dma('sp', cwb[:], self.cwb.rearrange("p (h c k) -> p h c k", h=4, c=2), writes=['cwb'], sem='cwb')
            P.dma('sp', ngs[:], self.ngs.rearrange("p (a n) -> p a n", a=2), writes=['ngs'], sem='ngs')
            P.dma('sp', gbs[:], self.gb, writes=['gbs'], sem='gbs')
            P.op('pool', lambda e: e.memset(wqkv[:], 0.0), [], ['wqkv'])
            for a in range(3):
                for kc, (o, n) in enumerate(CH):
                    self.load_cast(wqkv[0:n, :, a, kc, 0:192], self.wqkv[a][:, o:o + n, :].rearrange("h p n -> p h n"),
                                   'wqkv', n, (4, 192))
            self.load_cast(wgt[:], self.ml_w[:, 1536:1552].rearrange("(c p) n -> p c n", p=128), 'wgt', 128, (8, 16))
            for tt in range(NT):
                bank = tt % 2
                for c in range(8):
                    self.mm(self.ps[bank][:, 0:16], self.xT[:, c, tt * 128:(tt + 1) * 128], wgt[:, c, :], c == 0, c == 7,
                            ['xT%d' % tt, 'wgt'], ['ps%d' % bank])
                self.tt('dve', gts[:, tt, :], self.ps[bank][:, 0:16], gbs[:], ALU.add, ['ps%d' % bank, 'gbs'], ['gts'])
            gv = gts[:].rearrange("p t (g h) -> p t g h", h=4)
            for d in range(2):
                self.copy('pool', ig[:, :, d, :], gv[:, :, 2 * d, :], ['gts'], ['ig'])
                self.act(lf[:, :, d, :], gv[:, :, 2 * d + 1, :], AF.Exp, ['gts'], ['lf'], scale=-1.0)
            self.act(lf[:], lf[:], AF.Ln, ['lf'], ['lf'], bias=1.0)
            self.ts('dve', lf[:], lf[:], -1.0, None, ALU.mult, None, ['lf'], ['lf'])
            for tt in range(NT):
                bank = 2 + (tt % 2)
                self.mm(self.ps[bank][:, 0:4], self.Uf, lf[:, tt, 0, :], True, True, ['lf', 'cst'], ['ps%d' % bank])
                self.mm(self.ps[bank][:, 4:8], self.Ub, lf[:, tt, 1, :], True, True, ['lf', 'cst'], ['ps%d' % bank])
                self.mm(self.ps[bank][:, 8:16], self.ones_f, lf[:, tt, :, :].rearrange("p d h -> p (d h)"), True, True,
                        ['lf', 'cst'], ['ps%d' % bank])
                self.copy('dve', bcol[:, tt, :, :].rearrange("p d h -> p (d h)"), self.ps[bank][:, 0:8], ['ps%d' % bank], ['bcol'])
                self.copy('act', blast[:, tt, :, :].rearrange("p d h -> p (d h)"), self.ps[bank][:, 8:16], ['ps%d' % bank], ['blast'])
            self.tt('dve', colb[:], ig[:], bcol[:], ALU.subtract, ['ig', 'bcol'], ['colb'])
            self.act(eb[:], bcol[:], AF.Exp, ['bcol'], ['eb'])
            self.act(colf[:], colb[:], AF.Exp, ['colb'], ['colf'])
            self.tt('dve', wexp[:], blast[:], colb[:], ALU.add, ['blast', 'colb'], ['wexp'])
            self.act(wexp[:], wexp[:], AF.Exp, ['wexp'], ['wexp'])
            self.act(ebl[:], blast[:], AF.Exp, ['blast'], ['ebl'])
            self.mem_attention(self.ml_w[:, 1552:1808])
            nh = 4 if self.stage != 'ml_small' else 1
            wxzs = [sb("wxz%d" % i, [128, 8, 2, 256], BF16) for i in range(2)]
            for i in range(2):
                P.op('pool', lambda e, i=i: e.memset(wxzs[i][:], 0.0), [], ['wxz%d' % i])

            def load_wxz(hh):
                for a, c0 in ((0, hh * 192), (1, 768 + hh * 192)):
                    self.load_cast(wxzs[hh % 2][:, :, a, 0:192], self.ml_w[:, c0:c0 + 192].rearrange("(c p) n -> p c n", p=128),
                                   'wxz%d' % (hh % 2), 128, (8, 192))
            load_wxz(0)
            for h in range(nh):
                with ExitStack() as hd:
                    sbh = lambda n, s_, d: self.sb(hd, n, s_, d)
                    qT = sbh("mqT", [128, 2, S], BF16)
                    kT = sbh("mkT_", [128, 2, S], BF16)
                    kw = sbh("kw", [128, NT, 2, 256], BF16)
                    P.op('pool', lambda e: e.memset(kw[:, :, :, 192:256], 0.0), [], ['kw'])
                    vaug = sbh("mvaug", [128, NT, 193], BF16)
                    xct = sbh("xct", [128, NT, 192], BF16)
                    zs = sbh("zs", [128, NT, 192], BF16)
                    with ExitStack() as t1:
                        sb1 = lambda n, s_, d: self.sb(t1, n, s_, d)
                        wxz = wxzs[h % 2]
                        wk = 'wxz%d' % (h % 2)
                        xmT = sb1("xmT", [128, 2, S + 4], BF16)
                        xcT = sb1("xcT", [128, 2, S], BF16)
                        dg = sb1("dg", [128, 2, 5, 128], BF16)
                        if h + 1 < nh:
                            load_wxz(h + 1)
                        P.op('pool', lambda e: e.memset(xmT[:, :, 0:2], 0.0), [], ['xmT'])
                        P.op('pool', lambda e: e.memset(xmT[:, :, S + 2:S + 4], 0.0), [], ['xmT'])
                        for ch, (o, n) in enumerate(CH):
                            for j in range(5):
                                self.ts('dve', dg[:, ch, j, :], self.ident_f, cwb[:, h, ch, j:j + 1], None, ALU.mult, None,
                                        ['cst', 'cwb'], ['dg'])
                        for ch, (o, n) in enumerate(CH):
                            for tb in range(4):
                                bank = 2 + (tb % 2)
                                for c in range(8):
                                    self.mm(self.ps[bank][:, :], wxz[:, c, 0, o:o + 128], self.xT[:, c, tb * 512:(tb + 1) * 512],
                                            c == 0, c == 7, [wk] + self.xkb(tb), ['ps%d' % bank])
                                self.copy('act' if tb % 2 == 0 else 'dve', xmT[:, ch, 2 + tb * 512:2 + (tb + 1) * 512], self.ps[bank][:, :],
                                          ['ps%d' % bank], ['xmT'])
                        for tt in range(NT):
                            bank = tt % 2
                            for c in range(8):
                                self.mm(self.ps[bank][:, 0:192], self.xT[:, c, tt * 128:(tt + 1) * 128], wxz[:, c, 1, 0:192], c == 0, c == 7,
                                        ['xT%d' % tt, wk], ['ps%d' % bank])
                            self.act(zs[:, tt, :], self.ps[bank][:, 0:192], AF.Silu, ['ps%d' % bank], ['zs'])
                        for ch, (o, n) in enumerate(CH):
                            for tb in range(4):
                                bank = 4 + (tb % 2)
                                for j in range(5):
                                    self.mm(self.ps[bank][:, :], dg[:, ch, j, :], xmT[:, ch, tb * 512 + j:tb * 512 + j + 512],
                                            j == 0, j == 4, ['dg', 'xmT'], ['ps%d' % bank])
                                self.act(xcT[:, ch, tb * 512:(tb + 1) * 512], self.ps[bank][:, :], AF.Silu, ['ps%d' % bank, 'cwb'], ['xcT'],
                                         bias=cwb[:, h, ch, 5:6])
                        P.op('pool', lambda e: e.memset(vaug[:, :, 192:193], 1.0), [], ['mvaug'])
                        for tt in range(NT):
                            bank = 4 + (tt % 2)
                            for kc, (o, n) in enumerate(CH):
                                self.mm(self.ps[bank][:, 0:192], xmT[:, kc, 2 + tt * 128:2 + (tt + 1) * 128], wqkv[:, h, 2, kc, 0:192], kc == 0, kc == 1,
                                        ['xmT', 'wqkv'], ['ps%d' % bank])
                            self.copy('act' if tt % 2 == 0 else 'dve', vaug[:, tt, 0:192], self.ps[bank][:, 0:192], ['ps%d' % bank], ['mvaug'])
                        for a_, dst, key, scale in ((0, qT, 'mqT', 1.0), (1, kT, 'mkT_', KS)):
                            for oc, (oo, on) in enumerate(CH):
                                for tb in range(4):
                                    bank = 6 + (tb % 2)
                                    for kc, (o, n) in enumerate(CH):
                                        self.mm(self.ps[bank][:, :], wqkv[:, h, a_, kc, oo:oo + 128], xcT[:, kc, tb * 512:(tb + 1) * 512],
                                                kc == 0, kc == 1, ['xcT', 'wqkv'], ['ps%d' % bank])
                                    self.act(dst[:, oc, tb * 512:(tb + 1) * 512], self.ps[bank][:, :], AF.Copy, ['ps%d' % bank], [key],
                                             scale=scale)
                        for tt in range(NT):
                            bank = tt % 2
                            for kc, (o, n) in enumerate(CH):
                                self.mm(self.ps[bank][:, 0:192], xcT[:, kc, tt * 128:(tt + 1) * 128], wqkv[:, h, 1, kc, 0:192], kc == 0, kc == 1,
                                        ['xcT', 'wqkv'], ['ps%d' % bank])
                            for d in range(2):
                                self.ts('dve', kw[:, tt, d, 0:192], self.ps[bank][:, 0:192], wexp[:, tt, d, h:h + 1], KS, ALU.mult, ALU.mult,
                                        ['ps%d' % bank, 'wexp'], ['kw'])
                        for tt in range(NT):
                            bank = 2 + (tt % 2)
                            for kc, (o, n) in enumerate(CH):
                                self.tr(self.psb[bank][:, o:o + 128], xcT[:, kc, tt * 128:(tt + 1) * 128], self.ident_bf[:],
                                        ['xcT', 'ident_bf'], ['ps%d' % bank])
                            self.copy('act' if tt % 2 == 0 else 'dve', xct[:, tt, :], self.psb[bank][:, 0:192], ['ps%d' % bank], ['xct'])
                        P.barrier()
                    with ExitStack() as t2:
                        sb2 = lambda n, s_, d: self.sb(t2, n, s_, d)
                        hraw = sb2("hraw", [128, 2, NT, 193], F32)
                        Pm = [[sb2("Pm%d%d" % (i, d), [128, 128], BF16) for d in range(2)] for i in range(2)]
                        C = sb2("Cst", [128, 2, 2, 193], F32)
                        Cbf = [sb2("Cbf%d" % i, [128, 2, 2, 193], BF16) for i in range(2)]
                        dn = sb2("dn", [128, 2 * NT, 1], F32)
                        sm = sb2("sm", [128, NT, 1], F32)
                        sq = sb2("sq", [128, NT, 1], F32)
                        msk = (self.Uf, self.Ub)

                        def tt_of(c, d):
                            return c if d == 0 else NT - 1 - c

                        def st_A(c):
                            cp = c % 2
                            for d in range(2):
                                tt = tt_of(c, d)
                                tsl = slice(tt * 128, (tt + 1) * 128)
                                self.mm(self.ps[cp][:, d * 128:(d + 1) * 128], kT[0:128, 0, tsl], qT[0:128, 0, tsl], True, False,
                                        ['mkT_', 'mqT'], ['ps%d' % cp])
                                self.mm(self.ps[cp][:, d * 128:(d + 1) * 128], kT[:, 1, tsl], qT[:, 1, tsl], False, True,
                                        ['mkT_', 'mqT'], ['ps%d' % cp])
                            if c < NT - 1:
                                for d in range(2):
                                    tt = tt_of(c, d)
                                    b2 = 2 + cp * 2 + d
                                    for kc, (o, n) in enumerate(CH):
                                        self.mm(self.ps[b2][:, kc * 256:kc * 256 + 193], kw[:, tt, d, o:o + 128], vaug[:, tt, :], True, True,
                                                ['kw', 'mvaug'], ['ps%d' % b2])
                            for d in range(2):
                                tt = tt_of(c, d)
                                self.stt(Pm[cp][d][:], self.ps[cp][:, d * 128:(d + 1) * 128], colf[:, tt, d, h:h + 1], msk[d],
                                         ALU.mult, ALU.mult, ['ps%d' % cp, 'colf', 'cst'], ['Pm%d%d' % (cp, d)])

                        def st_B(c):
                            cp = c % 2
                            bh = 6 + cp
                            for d in range(2):
                                tt = tt_of(c, d)
                                tsl = slice(tt * 128, (tt + 1) * 128)
                                o_ = self.ps[bh][:, d * 256:d * 256 + 193]
                                self.mm(o_, Pm[cp][d][:], vaug[:, tt, :], True, c == 0, ['Pm%d%d' % (cp, d), 'mvaug'], ['ps%d' % bh])
                                if c > 0:
                                    pk = 'Cbf%d%d' % (1 - cp, d)
                                    self.mm(o_, qT[0:128, 0, tsl], Cbf[1 - cp][0:128, d, 0, :], False, False, ['mqT', pk], ['ps%d' % bh])
                                    self.mm(o_, qT[:, 1, tsl], Cbf[1 - cp][:, d, 1, :], False, True, ['mqT', pk], ['ps%d' % bh])
                            if c < NT - 1:
                                for d in range(2):
                                    tt = tt_of(c, d)
                                    b2 = 2 + cp * 2 + d
                                    psv = self.ps[b2][:, 0:512].rearrange("p (k n) -> p k n", n=256)[:, :, 0:193]
                                    if c == 0:
                                        self.copy('dve', C[:, d, :, :], psv, ['ps%d' % b2], ['C%d' % d])
                                    else:
                                        self.stt(C[:, d, :, :], C[:, d, :, :], ebl[:, tt, d, h:h + 1], psv, ALU.mult, ALU.add,
                                                 ['C%d' % d, 'ebl', 'ps%d' % b2], ['C%d' % d])
                                    self.copy('act', Cbf[cp][:, d, :, :], C[:, d, :, :], ['C%d' % d], ['Cbf%d%d' % (cp, d)])
                            for d in range(2):
                                tt = tt_of(c, d)
                                self.act(hraw[:, d, tt, :], self.ps[bh][:, d * 256:d * 256 + 193], AF.Copy, ['ps%d' % bh, 'eb'], ['hraw%d_%d' % (d, tt)],
                                         scale=eb[:, tt, d, h:h + 1])

                        st_A(0)
                        for c in range(NT):
                            if c + 1 < NT:
                                st_A(c + 1)
                            st_B(c)
                        denv = hraw[:, :, :, 192:193].rearrange("p d t o -> p (d t) o")
                        allh = ['hraw%d_%d' % (d_, t_) for d_ in range(2) for t_ in range(NT)]
                        self.stt(dn[:], denv, -1.0, denv, ALU.mult, ALU.max, allh, ['dn', 'hraw'])
                        self.ts('dve', dn[:], dn[:], 1.0, None, ALU.max, None, ['dn'], ['dn'])
                        P.op('dve', lambda e: e.reciprocal(dn[:], dn[:]), ['dn'], ['dn'])
                        for d in range(2):
                            self.tt('dve', hraw[:, d, :, 0:192], hraw[:, d, :, 0:192],
                                    dn[:, d * NT:(d + 1) * NT, :].to_broadcast([128, NT, 192]), ALU.mult, ['hraw', 'dn'], ['hraw'])
                        h0 = hraw[:, 0, :, 0:192]
                        h1 = hraw[:, 1, :, 0:192]
                        self.tt('dve', h0, h0, h1, ALU.add, ['hraw'], ['hraw'])
                        P.op('dve', lambda e: e.tensor_reduce(sm[:], h0, AX.X, ALU.add), ['hraw'], ['sm'])
                        self.ts('dve', sm[:], sm[:], 1.0 / 192, None, ALU.mult, None, ['sm'], ['sm'])
                        self.tt('dve', h0, h0, sm[:].to_broadcast([128, NT, 192]), ALU.subtract, ['hraw', 'sm'], ['hraw'])
                        self.tt('dve', h1, h0, h0, ALU.mult, ['hraw'], ['hraw'])
                        P.op('dve', lambda e: e.tensor_reduce(sq[:], h1, AX.X, ALU.add), ['hraw'], ['sq'])
                        self.act(sq[:], sq[:], AF.Sqrt, ['sq'], ['sq'], bias=self.eps_ap[:, 0:1], scale=1.0 / 192)
                        P.op('dve', lambda e: e.reciprocal(sq[:], sq[:]), ['sq'], ['sq'])
                        self.tt('dve', h0, h0, sq[:].to_broadcast([128, NT, 192]), ALU.mult, ['hraw', 'sq'], ['hraw'])
                        gview = ngs[:, 0, h * 192:(h + 1) * 192].unsqueeze(1).to_broadcast([128, NT, 192])
                        sview = ngs[:, 1, h * 192:(h + 1) * 192].unsqueeze(1).to_broadcast([128, NT, 192])
                        self.tt('dve', h0, h0, gview, ALU.mult, ['hraw', 'ngs'], ['hraw'])
                        self.tt('dve', h1, xct[:], sview, ALU.mult, ['xct', 'ngs', 'hraw'], ['hraw'])
                        self.tt('dve', h0, h0, h1, ALU.add, ['hraw'], ['hraw'])
                        self.tt('dve', self.mixed[:, :, h * 192:(h + 1) * 192], h0, zs[:], ALU.mult, ['hraw', 'zs'], ['mixed'])
                        P.barrier()
            self.mixed_to_xT()
            P.barrier()


def _na_bias_index():
    idx = np.full((21, 128, 128), 465, dtype=np.int64)
    for i in [0, 1, 2, 14, 15]:
        kts, slot = _na_key_tiles(i)
        qi = np.arange(128)
        r = 2 * i + qi // 64
        c = qi % 64
        rs = np.clip(r - 4, 0, 24)
        cs = np.clip(c - 8, 0, 48)
        for j, kt in enumerate(kts):
            ki = np.arange(128)
            kr = (2 * kt + ki // 64)[:, None]
            kc = (ki % 64)[:, None]
            valid = (kr >= rs[None]) & (kr < rs[None] + 8) & (kc >= cs[None]) & (kc < cs[None] + 16)
            flat = (kr - r[None] + 7) * 31 + (kc - c[None] + 15)
            idx[slot + j] = np.where(valid, flat, 465)
    return idx


_CACHE = {}


def _host_inputs(inp):
    f = lambda a: np.ascontiguousarray(np.asarray(a, dtype=np.float32))
    rep = lambda v: np.ascontiguousarray(np.broadcast_to(np.asarray(v, np.float32).reshape(1, -1), (128, np.asarray(v).size)))
    ln_g, ln_b = f(inp['ln_g']), f(inp['ln_b'])
    lnp = np.stack([np.concatenate([rep(inp['mem_ln_g']), rep(inp['mem_ln_b'])], 1)] +
                   [np.concatenate([rep(ln_g[l, k]), rep(ln_b[l, k])], 1) for l in range(2) for k in range(2)], 0)
    rpb = f(inp['na_rpb'])[0].reshape(12, 465)
    rpb_ext = np.concatenate([rpb, np.full((12, 1), NEG, np.float32)], 1)
    idx = _na_bias_index()
    nab = rpb_ext[:, idx]
    nab = np.ascontiguousarray(nab.transpose(0, 2, 1, 3).reshape(12, 128, 21 * 128))
    cw = f(inp['ml_conv_w'])[0]
    cb = f(inp['ml_conv_b'])[0]
    cwb = np.zeros((128, 4, 2, 8), np.float32)
    for h in range(4):
        for ch, (o, n) in enumerate(((0, 128), (128, 64))):
            f0 = h * 192 + o
            cwb[:n, h, ch, 0:5] = cw[:, f0:f0 + n].T
            cwb[:n, h, ch, 5] = cb[f0:f0 + n]
    ii = np.arange(128)
    ident = np.eye(128, dtype=np.float32)
    Uf = (ii[:, None] <= ii[None, :]).astype(np.float32)
    Ub = (ii[:, None] >= ii[None, :]).astype(np.float32)
    mnf = np.where(ii[:, None] <= ii[None, :], 0.0, NEG).astype(np.float32)
    mnb = np.where(ii[:, None] >= ii[None, :], 0.0, NEG).astype(np.float32)
    consts = np.stack([ident, Uf, Ub, mnf, mnb, np.ones((128, 128), np.float32)], 1).reshape(128, 6 * 128)
    def relay(w, nchunk):
        w = f(w)
        L, E, K, N = w.shape
        w = w.reshape(L, E, 2, nchunk // 2, 128, N).transpose(0, 2, 1, 4, 3, 5)
        return np.ascontiguousarray(w.reshape(L * 2 * E * 128, (nchunk // 2) * N))
    consts2 = np.zeros((128, NB * 16 + 1), np.float32)
    consts2[:, :NB * 16] = np.repeat(np.arange(NB, dtype=np.float32), 16)[None, :]
    consts2[:, NB * 16] = np.arange(128, dtype=np.float32)
    shared = {
        'lnp': np.ascontiguousarray(lnp), 'w_mem_kv': f(inp['w_mem_kv']), 'router_w': f(inp['router_w']),
        'router_b': rep(inp['router_b']), 'na_w_in': f(inp['na_w_in'])[0], 'na_bias': nab,
        'ml_w_in': f(inp['ml_w_in'])[0], 'ml_cwb': cwb.reshape(128, 64), 'ml_w_qkv': f(inp['ml_w_qkv'])[0],
        'ml_gate_b': rep(f(inp['ml_gate_b'])[0].reshape(-1)),
        'ml_ngs': np.concatenate([rep(f(inp['ml_norm_g'])[0]), rep(f(inp['ml_skip'])[0])], 1),
        'w_out': f(inp['w_out']), 'exp_w_gate': relay(inp['exp_w_gate'], 8), 'exp_w_up': relay(inp['exp_w_up'], 8),
        'exp_w_down': relay(inp['exp_w_down'], 4), 'consts': np.ascontiguousarray(consts), 'consts2': consts2,
    }
    return shared


def run(inputs, stage='full', cores=8):
    if stage not in _CACHE:
        b = Builder(stage)
        b.build()
        _CACHE[stage] = b.nc
    nc = _CACHE[stage]
    shared = _host_inputs(inputs)
    x = np.asarray(inputs['x'], np.float32)
    mem = np.asarray(inputs['mem'], np.float32)
    in_maps = []
    for c in range(cores):
        m = dict(shared)
        m['x'] = np.ascontiguousarray(x[c])
        m['mem'] = np.ascontiguousarray(mem[c])
        in_maps.append(m)
    res = run_bass_kernel_spmd(nc, in_maps, core_ids=list(range(cores)))
    return np.stack([np.asarray(r['y'], np.float32) for r in res.results], 0)


def kernel(**inputs):
    return run(inputs, 'full', 8)
```

```python
import numpy as np
import concourse.bass as bass
import concourse.mybir as mybir
from concourse.bass_utils import run_bass_kernel_spmd
from contextlib import ExitStack

F32 = mybir.dt.float32
BF16 = mybir.dt.bfloat16
AF = mybir.ActivationFunctionType
ALU = mybir.AluOpType
AX = mybir.AxisListType


class Prog:
    CE = ['pe', 'act', 'dve', 'pool']

    def __init__(self, nc, st):
        self.nc = nc
        self.st = st
        self.engs = {'pe': nc.tensor, 'act': nc.scalar, 'dve': nc.vector, 'pool': nc.gpsimd, 'sp': nc.sync}
        self.sems = {e: st.enter_context(nc.semaphore("s_" + e)) for e in self.CE}
        self.cnt = {e: 0 for e in self.CE}
        self.seen = {e: {} for e in self.CE + ['sp']}
        self.last_w = {}
        self.readers = {}
        self.out_sems = []

    def _sem(self, name):
        if name not in self.sems:
            self.sems[name] = self.st.enter_context(self.nc.semaphore("d_" + name))
            self.cnt[name] = 0
        return self.sems[name]

    def _deps(self, engine, reads, writes, skip_waw_sem=None):
        deps = {}

        def add(ev):
            sk, val, eng = ev
            if eng == 'pe' and engine == 'pe':
                return
            if deps.get(sk, 0) < val:
                deps[sk] = val
        for k in reads:
            if k in self.last_w:
                add(self.last_w[k])
            if k.startswith('ps'):
                for sk, (val, eng) in self.readers.get(k, {}).items():
                    if eng != engine:
                        add((sk, val, eng))
        for k in writes:
            if k in self.last_w:
                ev = self.last_w[k]
                if not (skip_waw_sem is not None and ev[0] == skip_waw_sem and not self.readers.get(k)):
                    add(ev)
            for sk, (val, eng) in self.readers.get(k, {}).items():
                add((sk, val, eng))
        waits = []
        for sk, val in deps.items():
            if self.seen[engine].get(sk, 0) >= val:
                continue
            self.seen[engine][sk] = val
            waits.append((sk, val))
        return waits

    def _record(self, ev, reads, writes):
        for k in writes:
            self.last_w[k] = ev
            self.readers[k] = {}
        for k in reads:
            r = self.readers.setdefault(k, {})
            if r.get(ev[0], (0, None))[0] < ev[1]:
                r[ev[0]] = (ev[1], ev[2])

    def op(self, engine, fn, reads=(), writes=()):
        waits = self._deps(engine, reads, writes)
        self.cnt[engine] += 1
        ev = (engine, self.cnt[engine], engine)
        self._record(ev, reads, writes)
        self._emit(engine, waits, fn, (engine, 1))

    def _emit(self, engine, waits, fn, inc):
        eng = self.engs[engine]
        for sk, val in waits:
            eng.wait_ge(self.sems[sk], val)
        if fn is not None:
            ins = fn(eng)
            ins.then_inc(self.sems[inc[0]], inc[1])

    def dma(self, queue, out_ap, in_ap, reads=(), writes=(), sem=None, out=False, **kw):
        self._sem(sem)
        waits = self._deps(queue, reads, writes, skip_waw_sem=sem)
        self.cnt[sem] += 16
        ev = (sem, self.cnt[sem], 'dma')
        self._record(ev, reads, writes)
        self._emit(queue, waits, lambda e: e.dma_start(out=out_ap, in_=in_ap, **kw), (sem, 16))
        if out and sem not in self.out_sems:
            self.out_sems.append(sem)

    def idma(self, out_ap, in_ap, idx_ap, scatter, reads=(), writes=(), sem=None, bounds=None):
        self._sem(sem)
        waits = self._deps('pool', reads, writes, skip_waw_sem=sem)
        self.cnt[sem] += 16
        ev = (sem, self.cnt[sem], 'dma')
        self._record(ev, reads, writes)
        off = bass.IndirectOffsetOnAxis(ap=idx_ap, axis=0)
        if scatter:
            fn = lambda e: e.indirect_dma_start(out=out_ap, out_offset=off, in_=in_ap, in_offset=None)
        else:
            if bounds is None:
                fn = lambda e: e.indirect_dma_start(out=out_ap, out_offset=None, in_=in_ap, in_offset=off)
            else:
                if getattr(self, 'bnd_reg', None) is None:
                    self.bnd_reg = self.nc.gpsimd.alloc_register('bnd')
                    self.nc.gpsimd.reg_mov(self.bnd_reg, bounds)
                reg = self.bnd_reg
                fn = lambda e: e.indirect_dma_start(out=out_ap, out_offset=None, in_=in_ap, in_offset=off,
                                                    bounds_check=reg, oob_is_err=False)
        self._emit('pool', waits, fn, (sem, 16))

    def barrier(self):
        for e in self.CE + ['sp']:
            waits = []
            for sk, c in self.cnt.items():
                if c > 0 and self.seen[e].get(sk, 0) < c and not (sk == e):
                    self.seen[e][sk] = c
                    waits.append((sk, c))
            if waits:
                self._emit(e, waits, None, None)
        for e in self.CE:
            if self.cnt[e] > 0 and self.seen[e].get(e, 0) < self.cnt[e]:
                self.seen[e][e] = self.cnt[e]
                self._emit(e, [(e, self.cnt[e])], None, None)

    def finish(self):
        self.barrier()


S = 2048
D = 1024
NT = 16
ALPHA = (2 * 2) ** 0.25
LN_EPS = 1e-5
NEG = -30000.0
BS = 384
QT = BS // 128
NB = 27
I32 = mybir.dt.int32


def _na_key_tiles(i):
    if i < 2:
        return [0, 1, 2, 3], (0 if i == 0 else 4)
    if i > 13:
        return [12, 13, 14, 15], (13 if i == 14 else 17)
    return [i - 2, i - 1, i, i + 1, i + 2], 8


class Builder:
    def __init__(self, stage):
        self.stage = stage
        self.nc = nc = bass.Bass("TRN2", target_bir_lowering=False)

        def din(name, shape):
            return nc.dram_tensor(name, list(shape), F32, kind="ExternalInput").ap()
        self.x = din("x", [S, D])
        self.mem = din("mem", [256, D])
        self.lnp = din("lnp", [5, 128, 2 * D])
        self.wmkv = din("w_mem_kv", [D, 512])
        self.rw = din("router_w", [D, 16])
        self.rb = din("router_b", [128, 16])
        self.na_w = din("na_w_in", [D, 2560])
        self.nab = din("na_bias", [12, 128, 21 * 128])
        self.ml_w = din("ml_w_in", [D, 1808])
        self.cwb = din("ml_cwb", [128, 64])
        self.wqkv = din("ml_w_qkv", [3, 4, 192, 192])
        self.gb = din("ml_gate_b", [128, 16])
        self.ngs = din("ml_ngs", [128, 2 * 768])
        self.wout = din("w_out", [2, D, D])
        self.wg = din("exp_w_gate", [4 * 2048, 2048])
        self.wu = din("exp_w_up", [4 * 2048, 2048])
        self.wd = din("exp_w_down", [4 * 2048, 2048])
        self.consts = din("consts", [128, 6 * 128])
        self.consts2 = din("consts2", [128, NB * 16 + 1])
        self.y = nc.dram_tensor("y", [S, D], F32, kind="ExternalOutput").ap()
        self.xs = nc.dram_tensor("xs", [S, D], F32, kind="Internal").ap()
        self.xsorted = nc.dram_tensor("xsorted", [NB * BS, D], BF16, kind="Internal").ap()
        self.ysorted = nc.dram_tensor("ysorted", [NB * BS, D], F32, kind="Internal").ap()

    def xkb(self, tb):
        return ['xT%d' % t for t in range(tb * 4, tb * 4 + 4)]

    def sb(self, st, name, shape, dt):
        self._uid = getattr(self, '_uid', 0) + 1
        return st.enter_context(self.nc.sbuf_tensor("%s_%d" % (name, self._uid), list(shape), dt))

    def mm(self, out, lhsT, rhs, start, stop, reads, writes):
        self.P.op('pe', lambda e: e.matmul(out, lhsT, rhs, start=start, stop=stop), reads, writes)

    def tr(self, out, in_, ident, reads, writes):
        self.P.op('pe', lambda e: e.transpose(out, in_, ident), reads, writes)

    def act(self, out, in_, func, reads, writes, bias=0.0, scale=1.0, eng='act'):
        self.P.op('act', lambda e: e.activation(out, in_, func, bias=bias, scale=scale), reads, writes)

    def copy(self, eng, out, in_, reads, writes):
        if eng == 'act':
            self.P.op('act', lambda e: e.copy(out, in_), reads, writes)
        else:
            self.P.op(eng, lambda e: e.tensor_copy(out, in_), reads, writes)

    def tt(self, eng, out, a, b, op, reads, writes):
        self.P.op(eng, lambda e: e.tensor_tensor(out, a, b, op), reads, writes)

    def ts(self, eng, out, a, s1, s2, op0, op1, reads, writes):
        if op1 is None:
            self.P.op(eng, lambda e: e.tensor_scalar(out, a, s1, None, op0), reads, writes)
        else:
            self.P.op(eng, lambda e: e.tensor_scalar(out, a, s1, s2, op0, op1), reads, writes)

    def stt(self, out, a, s, b, op0, op1, reads, writes):
        self.P.op('dve', lambda e: e.scalar_tensor_tensor(out, a, s, b, op0, op1), reads, writes)

    def load_cast(self, dst_ap, src_ap, dst_key, nparts, shape):
        self.P.dma('pool', dst_ap, src_ap, writes=[dst_key], sem='lc_' + dst_key)

    def ln_stages(self, src, dst, src_key, dst_key, lnp_key, slot):
        P = self.P
        st6 = self.ln_st[:, slot * 12:(slot + 1) * 12]
        mv = self.ln_mv[:, slot * 8:(slot + 1) * 8]
        kst, kmv = 'ln_st%d' % slot, 'ln_mv%d' % slot

        def sA():
            for hlf in range(2):
                P.op('dve', lambda e, hlf=hlf: e.bn_stats(st6[:, hlf * 6:(hlf + 1) * 6], src[:, hlf * 512:(hlf + 1) * 512]),
                     [src_key], [kst])
            P.op('dve', lambda e: e.bn_aggr(mv[:, 0:2], st6[:, 0:12]), [kst], [kmv])

        def sB():
            self.act(mv[:, 2:3], mv[:, 1:2], AF.Sqrt, [kmv], [kmv], bias=self.eps_ap[:, 0:1])

        def sC():
            P.op('dve', lambda e: e.reciprocal(mv[:, 3:4], mv[:, 2:3]), [kmv], [kmv])
            self.ts('dve', mv[:, 4:5], mv[:, 0:1], mv[:, 3:4], -1.0, ALU.mult, ALU.mult, [kmv], [kmv])

        def sD():
            P.op('act', lambda e: e.activation(src, src, AF.Identity, bias=mv[:, 4:5], scale=mv[:, 3:4]), [src_key, kmv], [src_key])

        def sE():
            self.tt('dve', src, src, self.lnp_sb[:, 0:D], ALU.mult, [src_key, lnp_key], [src_key])
            self.tt('pool', dst, src, self.lnp_sb[:, D:2 * D], ALU.add, [src_key, lnp_key], [dst_key])
        return [sA, sB, sC, sD, sE]

    def ln_multi(self, n, tile_fn, lnp_key, pre=None, post=None):
        stages = {}
        for i in range(n + 4):
            if i < n:
                if pre is not None:
                    pre(i)
                stages[i] = self.ln_stages(*tile_fn(i), lnp_key, i % 4)
            for k in range(5):
                t = i - k
                if 0 <= t < n:
                    stages[t][k]()
                    if k == 4 and post is not None:
                        post(t)

    def transpose_tile(self, src_bf, src_key, dstT, dst_key, tt, bank):
        pst = self.psb[bank]
        for c in range(8):
            self.tr(pst[:, c * 128:(c + 1) * 128], src_bf[:, c * 128:(c + 1) * 128], self.ident_bf[:],
                    [src_key, 'ident_bf'], ['ps%d' % bank])
        eng = 'act' if (tt % 2 == 0) else 'dve'
        self.copy(eng, dstT[:, :, tt * 128:(tt + 1) * 128], pst[:, 0:1024].rearrange("p (c n) -> p c n", n=128),
                  ['ps%d' % bank], ['%s%d' % (dst_key, tt)])

    def mem_attention(self, w_ap_cols):
        with ExitStack() as t2:
            self.wqm = self.sb(t2, "wqm", [128, 8, 256], BF16)
            self.qmT = self.sb(t2, "qmT", [64, S], BF16)
            self.pmem = [self.sb(t2, "pmem%d" % i, [128, 2, 512], BF16) for i in range(2)]
            self.rd4 = [self.sb(t2, "rd4%d" % i, [128, 4, 1], F32) for i in range(2)]
            self._mem_attention(w_ap_cols)
            self.P.barrier()

    def _mem_attention(self, w_ap_cols):
        P = self.P
        wqm = self.wqm
        self.load_cast(wqm[:], w_ap_cols.rearrange("(c p) n -> p c n", p=128), 'wqm', 128, (8, 256))
        for h in range(4):
            for tb in range(4):
                bank = 6 + (tb % 2)
                for c in range(8):
                    self.mm(self.ps[bank][0:64, :], wqm[:, c, h * 64:(h + 1) * 64], self.xT[:, c, tb * 512:(tb + 1) * 512],
                            c == 0, c == 7, ['wqm'] + self.xkb(tb), ['ps%d' % bank])
                self.copy('act' if tb % 2 == 0 else 'dve', self.qmT[0:64, tb * 512:(tb + 1) * 512], self.ps[bank][0:64, :],
                          ['ps%d' % bank], ['qmT'])
            for tb in range(4):
                pt = self.pmem[tb % 2]
                ptk = 'pmem%d' % (tb % 2)
                for mt in range(2):
                    bank = 0 + mt
                    self.mm(self.ps[bank][:, :], self.mkT[0:64, h, mt * 128:(mt + 1) * 128],
                            self.qmT[0:64, tb * 512:(tb + 1) * 512], True, True, ['mkT', 'qmT'], ['ps%d' % bank])
                    self.act(pt[:, mt, :], self.ps[bank][:, :], AF.Exp, ['ps%d' % bank], [ptk], scale=0.125)
                bank = 4 + (tb % 2)
                for q in range(4):
                    for mt in range(2):
                        self.mm(self.ps[bank][:, q * 65:q * 65 + 65], pt[:, mt, q * 128:(q + 1) * 128], self.mv[:, mt, h, :],
                                mt == 0, mt == 1, [ptk, 'mv'], ['ps%d' % bank])
                pv = self.ps[bank][:, 0:260].rearrange("p (q e) -> p q e", e=65)
                rd4 = self.rd4[tb % 2]
                P.op('dve', lambda e, pv=pv, rd4=rd4: e.reciprocal(rd4[:], pv[:, :, 64:65]), ['ps%d' % bank], ['rd4%d' % (tb % 2)])
                self.tt('dve', self.mixed[:, tb * 4:(tb + 1) * 4, 768 + h * 64:768 + (h + 1) * 64], pv[:, :, 0:64],
                        rd4[:].to_broadcast([128, 4, 64]), ALU.mult, ['ps%d' % bank, 'rd4%d' % (tb % 2)], ['mixed'])

    def out_proj_ln(self, li, res_src):
        P = self.P
        for hlf in range(2):
            for kh in range(2):
                self.load_cast(self.wo[:, kh * 4:(kh + 1) * 4, hlf * 512:(hlf + 1) * 512],
                               self.wout[li][kh * 512:(kh + 1) * 512, hlf * 512:(hlf + 1) * 512].rearrange("(c p) n -> p c n", p=128),
                               'wo', 128, (4, 512))
        P.dma('sp', self.lnp_sb[:], self.lnp[1 + 2 * li], writes=['lnp'], sem='lnp')

        def pre(tt):
            xr = self.xres[tt % 2]
            xrk = 'xres%d' % (tt % 2)
            P.dma('sp', xr[:], res_src[tt * 128:(tt + 1) * 128, :], writes=[xrk], sem=xrk)
            for hlf in range(2):
                bank = hlf + 2 * (tt % 2)
                for c in range(8):
                    self.mm(self.ps[bank][:, :], self.xT[:, c, tt * 128:(tt + 1) * 128], self.wo[:, c, hlf * 512:(hlf + 1) * 512],
                            c == 0, c == 7, ['xT%d' % tt, 'wo'], ['ps%d' % bank])
                self.stt(self.X[:, tt, hlf * 512:(hlf + 1) * 512], xr[:, hlf * 512:(hlf + 1) * 512], ALPHA,
                         self.ps[bank][:, :], ALU.mult, ALU.add, [xrk, 'ps%d' % bank], ['X%d' % tt])
        self.ln_multi(NT, lambda tt: (self.X[:, tt, :], self.X[:, tt, :], 'X%d' % tt, 'X%d' % tt), 'lnp', pre=pre)

    def moe_route(self, li):
        P = self.P
        P.dma('sp', self.rw_sb[:], self.rw.rearrange("(c p) n -> p c n", p=128), writes=['rw'], sem='rw')
        P.dma('sp', self.rb_sb[:], self.rb, writes=['rb'], sem='rb')
        def st_T(tt):
            pp = tt % 2
            for c in range(8):
                bank = 2 * pp + (0 if c < 4 else 1)
                self.mm(self.ps[bank][:, (c % 4) * 128:(c % 4 + 1) * 128], self.X[:, tt, c * 128:(c + 1) * 128],
                        self.ident_f, True, True, ['X%d' % tt, 'ident_f'], ['ps%d' % bank])

        def st_R(tt):
            pp = tt % 2
            xtf = self.xtf[pp]
            for b_ in range(2):
                bank = 2 * pp + b_
                self.copy('dve', xtf[:, b_ * 4:(b_ + 1) * 4, :],
                          self.ps[bank][:, :].rearrange("p (c n) -> p c n", n=128), ['ps%d' % bank], ['xtf%d' % pp])
            for c in range(8):
                self.mm(self.ps[4 + pp][:, 0:16], xtf[:, c, :], self.rw_sb[:, c, :], c == 0, c == 7, ['xtf%d' % pp, 'rw'], ['ps%d' % (4 + pp)])
            self.copy('dve', self.lg[:, tt, :], self.ps[4 + pp][:, 0:16], ['ps%d' % (4 + pp)], ['lg'])
        st_T(0)
        for tt in range(NT):
            if tt + 1 < NT:
                st_T(tt + 1)
            st_R(tt)
        lg = self.lg
        sc, bi, t1, t2, t3 = self.r_sc, self.r_bi, self.r_t1, self.r_t2, self.r_t3
        self.act(sc[:], lg[:], AF.Sigmoid, ['lg'], ['r_sc'])
        self.tt('dve', bi[:], sc[:], self.rb_sb[:].unsqueeze(1).to_broadcast([128, NT, 16]), ALU.add, ['r_sc', 'rb'], ['r_bi'])
        g4 = lambda t: t[:].rearrange("p t (g k) -> p (t g) k", k=4)
        m1, m2, gs = self.r_m1, self.r_m2, self.r_gs
        P.op('dve', lambda e: e.tensor_reduce(m1[:], g4(bi), AX.X, ALU.max), ['r_bi'], ['r_m1'])
        self.tt('dve', g4(t1), g4(bi), m1[:].to_broadcast([128, 64, 4]), ALU.is_equal, ['r_bi', 'r_m1'], ['r_t1'])
        self.stt(g4(t2), g4(t1), NEG, g4(bi), ALU.mult, ALU.add, ['r_t1', 'r_bi'], ['r_t2'])
        P.op('dve', lambda e: e.tensor_reduce(m2[:], g4(t2), AX.X, ALU.max), ['r_t2'], ['r_m2'])
        self.tt('dve', g4(t3), g4(t2), m2[:].to_broadcast([128, 64, 4]), ALU.is_equal, ['r_t2', 'r_m2'], ['r_t3'])
        self.tt('dve', gs[:], m1[:], m2[:], ALU.add, ['r_m1', 'r_m2'], ['r_gs'])
        gsv = gs[:].rearrange("p (t g) o -> p t (g o)", g=4)
        P.op('dve', lambda e: e.tensor_reduce(self.r_gm[:], gsv, AX.X, ALU.max), ['r_gs'], ['r_gm'])
        self.tt('dve', gsv, gsv, self.r_gm[:].to_broadcast([128, NT, 4]), ALU.is_equal, ['r_gs', 'r_gm'], ['r_gs'])
        self.tt('dve', g4(t1), g4(t1), gs[:].to_broadcast([128, 64, 4]), ALU.mult, ['r_t1', 'r_gs'], ['r_t1'])
        self.tt('dve', g4(t3), g4(t3), gs[:].to_broadcast([128, 64, 4]), ALU.mult, ['r_t3', 'r_gs'], ['r_t3'])
        wk = self.wk
        for k, sel in enumerate((t1, t3)):
            self.tt('dve', t2[:], sel[:], sc[:], ALU.mult, ['r_t1', 'r_t3', 'r_sc'], ['r_t2'])
            P.op('dve', lambda e, k=k: e.tensor_reduce(wk[:, :, k:k + 1], t2[:], AX.X, ALU.add), ['r_t2'], ['wk'])
        P.op('dve', lambda e: e.tensor_reduce(self.r_gm[:], wk[:], AX.X, ALU.add), ['wk'], ['r_gm'])
        P.op('dve', lambda e: e.reciprocal(self.r_gm[:], self.r_gm[:]), ['r_gm'], ['r_gm'])
        self.tt('dve', wk[:], wk[:], self.r_gm[:].to_broadcast([128, NT, 2]), ALU.mult, ['wk', 'r_gm'], ['wk'])
        sel = t2
        self.tt('dve', sel[:], t1[:], t3[:], ALU.add, ['r_t1', 'r_t3'], ['r_t2'])
        sel2d = sel[:].rearrange("p t e -> p (t e)")
        wi, to, cx = self.r_wi, self.r_to, self.r_cx
        self.tt('dve', self.Lst[:], self.Uf, self.ident_f, ALU.subtract, ['cst'], ['Lst'])
        self.mm(self.ps[5][:, 0:256], self.Lst[:], sel2d, True, True, ['Lst', 'r_t2'], ['ps5'])
        self.mm(self.ps[6][:, 0:256], self.ones_f, sel2d, True, True, ['cst', 'r_t2'], ['ps6'])
        self.copy('dve', wi[:].rearrange("p t e -> p (t e)"), self.ps[5][:, 0:256], ['ps5'], ['r_wi'])
        self.copy('dve', to[:].rearrange("p t e -> p (t e)"), self.ps[6][:, 0:256], ['ps6'], ['r_to'])
        P.op('dve', lambda e: e.memset(cx[:, 0, :], 0.0), [], ['r_cx'])
        for tt in range(1, NT):
            self.tt('dve', cx[:, tt, :], cx[:, tt - 1, :], to[:, tt - 1, :], ALU.add, ['r_cx', 'r_to'], ['r_cx'])
        cnt, nbk, pend = self.r_cnt, self.r_nbk, self.r_pend
        self.tt('dve', cnt[:], cx[:, NT - 1, :], to[:, NT - 1, :], ALU.add, ['r_cx', 'r_to'], ['r_cnt'])
        self.ts('dve', nbk[:], cnt[:], 0.0, None, ALU.is_gt, None, ['r_cnt'], ['r_nbk'])
        for k in range(1, -(-S // BS)):
            self.stt(nbk[:], cnt[:], float(BS * k), nbk[:], ALU.is_gt, ALU.add, ['r_cnt', 'r_nbk'], ['r_nbk'])
        self.copy('dve', pend[:, 0:1], nbk[:, 0:1], ['r_nbk'], ['r_pend'])
        for e_ in range(1, 16):
            self.tt('dve', pend[:, e_:e_ + 1], pend[:, e_ - 1:e_], nbk[:, e_:e_ + 1], ALU.add, ['r_pend', 'r_nbk'], ['r_pend'])
        self.tt('dve', cnt[:], pend[:], nbk[:], ALU.subtract, ['r_pend', 'r_nbk'], ['r_cnt'])
        self.ts('dve', cnt[:], cnt[:], float(BS), None, ALU.mult, None, ['r_cnt'], ['r_cnt'])
        self.tt('dve', wi[:], wi[:], cx[:], ALU.add, ['r_wi', 'r_cx'], ['r_wi'])
        self.tt('dve', wi[:], wi[:], cnt[:].unsqueeze(1).to_broadcast([128, NT, 16]), ALU.add, ['r_wi', 'r_cnt'], ['r_wi'])
        posf = self.r_posf
        for k, sl in enumerate((t1, t3)):
            self.tt('dve', sl[:], sl[:], wi[:], ALU.mult, ['r_t1', 'r_t3', 'r_wi'], ['r_t1', 'r_t3'])
            P.op('dve', lambda e, k=k, sl=sl: e.tensor_reduce(posf[:, :, k:k + 1], sl[:], AX.X, ALU.add), ['r_t1', 'r_t3'], ['r_posf'])
        self.copy('dve', self.pos_i[:], posf[:], ['r_posf'], ['pos_i'])
        bg = self.bgrid
        self.tt('dve', bg[:, :, :], pend[:].unsqueeze(1).to_broadcast([128, NB, 16]), self.c2[:, 0:NB * 16].rearrange("p (b e) -> p b e", e=16),
                ALU.is_le, ['r_pend', 'c2'], ['bgrid'])
        P.op('dve', lambda e: e.tensor_reduce(self.r_be[:], bg[:], AX.X, ALU.add), ['bgrid'], ['r_be'])
        self.ts('dve', self.r_be2[:], self.r_be[:], 16.0, 1.0e6, ALU.is_ge, ALU.mult, ['r_be'], ['r_be2'])
        self.ts('dve', self.r_be[:], self.r_be[:], 128.0, self.c2[:, NB * 16:NB * 16 + 1], ALU.mult, ALU.add, ['r_be', 'c2'], ['r_be'])
        self.tt('dve', self.r_be[:], self.r_be[:], self.r_be2[:], ALU.add, ['r_be', 'r_be2'], ['r_be'])
        for hlf in range(2):
            self.ts('dve', self.r_be2[:], self.r_be[:], float((li * 2 + hlf) * 2048), None, ALU.add, None, ['r_be'], ['r_be2'])
            self.copy('dve', self.widx_i[:, hlf, :], self.r_be2[:].rearrange("p b o -> p (b o)"), ['r_be2'], ['widx_i'])
        for tt in range(NT):
            xb = self.xbf[tt % 2]
            xbk = 'xbf%d' % (tt % 2)
            self.copy('act', xb[:], self.X[:, tt, :], ['X%d' % tt], [xbk])
            for k in range(2):
                P.idma(self.xsorted, xb[:, :], self.pos_i[:, tt, k:k + 1], True, reads=[xbk, 'pos_i', 'xsorted'],
                       writes=['xsorted%d' % (tt % 2)], sem='xsc%d' % (tt % 2))
            self.P.op('act', lambda e, tt=tt: e.mul(self.X[:, tt, :], self.X[:, tt, :], ALPHA), ['X%d' % tt], ['X%d' % tt])

    def moe_experts(self, li):
        P = self.P
        wsrc = (self.wg, self.wu, self.wd)
        nb = NB if self.stage != 'moe_small' else 2
        def st_W(b):
            s = b % 2
            for m, (dst, key) in enumerate(((self.wgb[s], 'wg%d' % s), (self.wub[s], 'wu%d' % s), (self.wdb[s], 'wd%d' % s))):
                for hlf in range(2):
                    if m < 2:
                        dv = dst[:, hlf * 4:(hlf + 1) * 4, :].rearrange("p c n -> p (c n)")
                    else:
                        dv = dst[:, hlf * 2:(hlf + 1) * 2, :].rearrange("p c n -> p (c n)")
                    P.idma(dv, wsrc[m], self.widx_i[:, hlf, b:b + 1], False, reads=['widx_i'], writes=[key], sem='s' + key,
                           bounds=(4 * 2048 - 1) if b >= 2 else None)

        def st_X(b):
            s = b % 2
            xblk = self.xT[:, QT * s:QT * s + QT, 1024:2048]
            xTb = self.xT[:, :, s * BS:(s + 1) * BS]
            P.dma('sp', xblk, self.xsorted[b * BS:(b + 1) * BS, :].rearrange("(q p) n -> p q n", p=128), reads=['xsorted0', 'xsorted1'],
                  writes=['xblk%d' % s], sem='xblk%d' % s)
            for q in range(QT):
                bank = 6 + (q % 2)
                for c in range(8):
                    self.tr(self.psb[bank][:, c * 128:(c + 1) * 128], xblk[:, q, c * 128:(c + 1) * 128], self.ident_bf[:],
                            ['xblk%d' % s, 'ident_bf'], ['ps%d' % bank])
                self.copy('act' if q % 2 == 0 else 'dve', xTb[:, :, q * 128:(q + 1) * 128],
                          self.psb[bank][:, 0:1024].rearrange("p (c n) -> p c n", n=128), ['ps%d' % bank], ['xTb%d' % s])

        def st_C(b):
            s = b % 2
            wgb, wub, wdb = self.wgb[s], self.wub[s], self.wdb[s]
            xTb = self.xT[:, :, s * BS:(s + 1) * BS]
            hT = self.hT[s]
            hk = 'hT%d' % s
            for efc in range(4):
                bg = 0 + (efc % 2)
                bu = 2 + (efc % 2)
                for c in range(8):
                    self.mm(self.ps[bg][:, 0:BS], wgb[:, c, efc * 128:(efc + 1) * 128], xTb[:, c, :],
                            c == 0, c == 7, ['wg%d' % s, 'xTb%d' % s], ['ps%d' % bg])
                for c in range(8):
                    self.mm(self.ps[bu][:, 0:BS], wub[:, c, efc * 128:(efc + 1) * 128], xTb[:, c, :],
                            c == 0, c == 7, ['wu%d' % s, 'xTb%d' % s], ['ps%d' % bu])
                sg = self.sg[efc % 2]
                sgk = 'sg%d' % (efc % 2)
                self.act(sg[:, 0:BS], self.ps[bg][:, 0:BS], AF.Silu, ['ps%d' % bg], [sgk])
                self.tt('dve', hT[:, efc, 0:BS], sg[:, 0:BS], self.ps[bu][:, 0:BS], ALU.mult, [sgk, 'ps%d' % bu], [hk])
            for q in range(QT):
                ysb = self.ysb[q]
                yk = 'ysb%d' % q
                for hlf in range(2):
                    by = 4 + (q % 2) * 2 + hlf
                    for kc in range(4):
                        self.mm(self.ps[by][:, :], hT[:, kc, q * 128:(q + 1) * 128], wdb[:, kc, hlf * 512:(hlf + 1) * 512],
                                kc == 0, kc == 3, [hk, 'wd%d' % s], ['ps%d' % by])
                    self.copy('act' if hlf == 0 else 'dve', ysb[:, hlf * 512:(hlf + 1) * 512], self.ps[by][:, :], ['ps%d' % by], [yk])
                r0 = b * BS + q * 128
                P.dma('act', self.ysorted[r0:r0 + 128, :], ysb, reads=[yk, 'ysorted'], writes=['ysorted%d' % q], sem='yst%d' % q)

        st_X(0)
        st_W(0)
        for b in range(nb):
            if b + 1 < nb:
                st_X(b + 1)
                st_W(b + 1)
            st_C(b)

    def combine_tile(self, tt, yks):
        P = self.P
        for k in range(2):
            j = (tt % 4) * 2 + k
            yb = yks[j]
            ykk = 'yk%d' % j
            P.idma(yb[:, :], self.ysorted, self.pos_i[:, tt, k:k + 1], False,
                   reads=['ysorted0', 'ysorted1', 'ysorted2', 'ysorted3', 'pos_i'], writes=[ykk], sem=ykk)
            self.stt(self.X[:, tt, :], yb[:], self.wk[:, tt, k:k + 1], self.X[:, tt, :], ALU.mult, ALU.add,
                     [ykk, 'wk', 'X%d' % tt], ['X%d' % tt])

    def ln2(self, li, yks):
        P = self.P
        P.dma('sp', self.lnp_sb[:], self.lnp[2 + 2 * li], writes=['lnp'], sem='lnp')

        def post(tt):
            if li == 0:
                xb = self.xbf[tt % 2]
                xbk = 'xbf%d' % (tt % 2)
                self.copy('act', xb[:], self.X[:, tt, :], ['X%d' % tt], [xbk])
                P.dma('sp', self.xs[tt * 128:(tt + 1) * 128, :], self.X[:, tt, :], reads=['X%d' % tt], writes=['xs%d' % tt],
                      sem='xs_st%d' % (tt % 2))
                self.transpose_tile(xb[:], xbk, self.xT, 'xT', tt, 6 + (tt % 2))
            else:
                P.dma('sp', self.y[tt * 128:(tt + 1) * 128, :], self.X[:, tt, :], reads=['X%d' % tt], sem='y_st%d' % (tt % 2),
                      out=True)
        self.ln_multi(NT, lambda tt: (self.X[:, tt, :], self.X[:, tt, :], 'X%d' % tt, 'X%d' % tt), 'lnp',
                      pre=lambda tt: self.combine_tile(tt, yks), post=post)

    def build(self):
        nc = self.nc
        with ExitStack() as st:
            self.P = P = Prog(nc, st)
            sb = lambda n, s, d: self.sb(st, n, s, d)
            self.ps = [st.enter_context(nc.psum_tensor("ps%d" % i, [128, 512], F32)) for i in range(8)]
            self.psb = [p[:].bitcast(BF16) for p in self.ps]
            cst = sb("cst", [128, 6, 128], F32)
            self.ident_f = cst[:, 0, :]
            self.Uf, self.Ub, self.mnf, self.mnb, self.ones_f = (cst[:, i, :] for i in range(1, 6))
            self.ident_bf = sb("ident_bf", [128, 128], BF16)
            self.eps_ap = sb("eps", [128, 1], F32)
            self.ln_st = sb("ln_st", [128, 4 * 12], F32)
            self.ln_mv = sb("ln_mv", [128, 4 * 8], F32)
            self.lnp_sb = sb("lnp_sb", [128, 2 * D], F32)
            self.rden = sb("rden", [128, 1], F32)
            self.xT = sb("xT", [128, 8, S], BF16)
            self.mkT = sb("mkT", [64, 4, 256], BF16)
            self.mv = sb("mv", [128, 2, 4, 65], BF16)
            P.dma('sp', cst[:], self.consts.rearrange("p (k n) -> p k n", n=128), writes=['cst'], sem='cst')
            self.copy('dve', self.ident_bf[:], cst[:, 0, :], ['cst'], ['ident_bf'])
            P.op('dve', lambda e: e.memset(self.eps_ap[:], LN_EPS), [], ['eps'])
            self.last_w_alias()
            self.mem_kv()
            self.layer0_mixer()
            P.barrier()
            if self.post(0):
                return
            self.layer1_mixer()
            P.barrier()
            self.post(1)
            P.finish()

    def post(self, li):
        P = self.P
        nc = self.nc
        with ExitStack() as ph:
            sb = lambda n, s_, d: self.sb(ph, n, s_, d)
            self.X = sb("X", [128, NT, D], F32)
            self.xbf = [sb("xbf%d" % i, [128, D], BF16) for i in range(2)]
            self.wk = sb("wk", [128, NT, 2], F32)
            self.pos_i = sb("pos_i", [128, NT, 2], I32)
            self.widx_i = sb("widx_i", [128, 2, NB], I32)
            with ExitStack() as pa:
                sb = lambda n, s_, d: self.sb(pa, n, s_, d)
                self.wo = sb("wo", [128, 8, D], BF16)
                self.xres = [sb("xres%d" % i, [128, D], F32) for i in range(2)]
                self.rw_sb = sb("rw_sb", [128, 8, 16], F32)
                self.rb_sb = sb("rb_sb", [128, 16], F32)
                self.xtf = [sb("xtf%d" % i, [128, 8, 128], F32) for i in range(2)]
                self.lg = sb("lg", [128, NT, 16], F32)
                for n in ['r_sc', 'r_bi', 'r_t1', 'r_t2', 'r_t3']:
                    setattr(self, n, sb(n, [128, NT, 16], F32))
                for n in ['r_m1', 'r_m2', 'r_gs']:
                    setattr(self, n, sb(n, [128, NT * 4, 1], F32))
                self.r_gm = sb("r_gm", [128, NT, 1], F32)
                for n in ['r_wi', 'r_to', 'r_cx']:
                    setattr(self, n, sb(n, [128, NT, 16], F32))
                for n in ['r_cnt', 'r_nbk', 'r_pend']:
                    setattr(self, n, sb(n, [128, 16], F32))
                self.r_posf = sb("r_posf", [128, NT, 2], F32)
                self.Lst = sb("Lst", [128, 128], F32)
                self.bgrid = sb("bgrid", [128, NB, 16], F32)
                self.r_be = sb("r_be", [128, NB, 1], F32)
                self.r_be2 = sb("r_be2", [128, NB, 1], F32)
                self.c2 = sb("c2", [128, NB * 16 + 1], F32)
                P.dma('sp', self.c2[:], self.consts2, writes=['c2'], sem='c2')
                self.out_proj_ln(li, self.x if li == 0 else self.xs)
                if self.stage == 'l0_ln1' or (self.stage in ('l1_ln1', 'ml_small') and li == 1):
                    self.dump_X()
                    return True
                self.moe_route(li)
                if self.stage in ('l0_route',):
                    self.dump_X()
                    return True
                P.barrier()
            with ExitStack() as pb:
                sb = lambda n, s_, d: self.sb(pb, n, s_, d)
                self.wgb = [sb("wgb%d" % i, [128, 8, 512], BF16) for i in range(2)]
                self.wub = [sb("wub%d" % i, [128, 8, 512], BF16) for i in range(2)]
                self.wdb = [sb("wdb%d" % i, [128, 4, 1024], BF16) for i in range(2)]
                self.hT = [sb("hT%d" % i, [128, 4, 512], BF16) for i in range(2)]
                self.sg = [sb("sg%d" % i, [128, 512], F32) for i in range(2)]
                self.ysb = [sb("ysb%d" % i, [128, D], F32)[:] for i in range(4)]
                self.moe_experts(li)
                P.barrier()
            with ExitStack() as pc:
                yks = [self.sb(pc, "yk%d" % i, [128, D], F32) for i in range(8)]
                if self.stage in ('l0', 'moe_small'):
                    for tt in range(NT):
                        self.combine_tile(tt, yks)
                    self.dump_X()
                    return True
                self.ln2(li, yks)
                P.barrier()
        return False

    def last_w_alias(self):
        for k in ['ident_f']:
            self.P.last_w[k] = self.P.last_w['cst']

    def dump_X(self):
        P = self.P
        for tt in range(NT):
            P.dma('sp', self.y[tt * 128:(tt + 1) * 128, :], self.X[:, tt, :], reads=['X%d' % tt], sem='y_st%d' % (tt % 2), out=True)
        P.finish()

    def mem_kv(self):
        P = self.P
        with ExitStack() as ph:
            memsb = self.sb(ph, "memsb", [128, 2, D], F32)
            membf = self.sb(ph, "membf", [128, 2, D], BF16)
            memT = self.sb(ph, "memT", [128, 8, 256], BF16)
            wkv = self.sb(ph, "wkv", [128, 8, 512], BF16)
            zt = self.sb(ph, "zt", [128, 8, D], BF16)
            P.op('pool', lambda e: e.memset(zt[:], 0.0), [], ['zt'])
            nrow = NB * BS
            for r0 in range(0, nrow, 1024):
                nq = min(1024, nrow - r0) // 128
                P.dma('act', self.xsorted[r0:r0 + nq * 128, :].rearrange("(q p) n -> p q n", p=128), zt[:, 0:nq, :], reads=['zt'],
                      writes=['xsorted'], sem='xsz')
            P.dma('sp', self.lnp_sb[:], self.lnp[0], writes=['lnp'], sem='lnp')
            for mt in range(2):
                P.dma('sp', memsb[:, mt, :], self.mem[mt * 128:(mt + 1) * 128, :], writes=['memsb%d' % mt], sem='memsb%d' % mt)
                for f in self.ln_stages(memsb[:, mt, :], membf[:, mt, :], 'memsb%d' % mt, 'membf%d' % mt, 'lnp', mt):
                    f()
                self.transpose_tile(membf[:, mt, :], 'membf%d' % mt, memT, 'memT', mt, 6 + mt)
            for hlf in range(2):
                self.load_cast(wkv[:, hlf * 4:(hlf + 1) * 4, :],
                               self.wmkv[hlf * 512:(hlf + 1) * 512, :].rearrange("(c p) n -> p c n", p=128), 'wkv', 128, (4, 512))
            for h in range(4):
                bank = h % 2
                for c in range(8):
                    self.mm(self.ps[bank][0:64, 0:256], wkv[:, c, h * 64:(h + 1) * 64], memT[:, c, :], c == 0, c == 7,
                            ['wkv', 'memT0', 'memT1'], ['ps%d' % bank])
                self.copy('act', self.mkT[:, h, :], self.ps[bank][0:64, 0:256], ['ps%d' % bank], ['mkT'])
            P.op('pool', lambda e: e.memset(self.mv[:, :, :, 64:65], 1.0), [], ['mv'])
            for mt in range(2):
                bank = 2 + mt
                for c in range(8):
                    self.mm(self.ps[bank][:, 0:256], memT[:, c, mt * 128:(mt + 1) * 128], wkv[:, c, 256:512], c == 0, c == 7,
                            ['wkv', 'memT%d' % mt], ['ps%d' % bank])
                self.copy('dve', self.mv[:, mt, :, 0:64], self.ps[bank][:, 0:256].rearrange("p (h d) -> p h d", d=64),
                          ['ps%d' % bank], ['mv'])
            P.barrier()

    def layer0_mixer(self):
        P = self.P
        with ExitStack() as ph:
            sb = lambda n, s_, d: self.sb(ph, n, s_, d)
            self.mixed = sb("mixed", [128, NT, D], BF16)
            vaug = sb("vaug", [128, NT, 12, 65], BF16)
            wqk = sb("wqk", [128, 8, 1536], BF16)
            qT = [sb("qT%d" % i, [128, S], BF16) for i in range(2)]
            kT = [sb("kT%d" % i, [128, S], BF16) for i in range(2)]
            s_sb = [sb("s_sb%d" % i, [128, 640], F32) for i in range(2)]
            p_bf = [sb("p_bf%d" % i, [128, 640], BF16) for i in range(2)]
            with ExitStack() as t1:
                sb1 = lambda n, s_, d: self.sb(t1, n, s_, d)
                xin = [sb1("xin%d" % i, [128, D], F32) for i in range(4)]
                xinb = [sb1("xinb%d" % i, [128, D], BF16) for i in range(4)]
                wv = sb1("wv", [128, 8, 768], BF16)
                for tt in range(NT):
                    i = tt % 4
                    P.dma('sp', xin[i][:], self.x[tt * 128:(tt + 1) * 128, :], writes=['xin%d' % i], sem='xin%d' % i)
                    self.copy('act' if tt % 2 == 0 else 'dve', xinb[i][:], xin[i][:], ['xin%d' % i], ['xinb%d' % i])
                    self.transpose_tile(xinb[i][:], 'xinb%d' % i, self.xT, 'xT', tt, 4 + i)
                for blk in range(3):
                    for hlf in range(2):
                        self.load_cast(wqk[:, hlf * 4:(hlf + 1) * 4, blk * 512:(blk + 1) * 512],
                                       self.na_w[hlf * 512:(hlf + 1) * 512, blk * 512:(blk + 1) * 512].rearrange("(c p) n -> p c n", p=128),
                                       'wqk', 128, (4, 512))
                for c0, n in ((0, 512), (512, 256)):
                    for hlf in range(2):
                        self.load_cast(wv[:, hlf * 4:(hlf + 1) * 4, c0:c0 + n],
                                       self.na_w[hlf * 512:(hlf + 1) * 512, 1536 + c0:1536 + c0 + n].rearrange("(c p) n -> p c n", p=128),
                                       'wv', 128, (4, n))
                P.op('pool', lambda e: e.memset(vaug[:, :, :, 64:65], 1.0), [], ['vaug'])
                for tt in range(NT):
                    for bi, (c0, n) in enumerate(((0, 512), (512, 256))):
                        bank = bi + 2 * (tt % 2)
                        for c in range(8):
                            self.mm(self.ps[bank][:, 0:n], self.xT[:, c, tt * 128:(tt + 1) * 128], wv[:, c, c0:c0 + n], c == 0, c == 7,
                                    ['xT%d' % tt, 'wv'], ['ps%d' % bank])
                        h0 = c0 // 64
                        self.copy('act' if bi == 0 else 'dve', vaug[:, tt, h0:h0 + n // 64, 0:64],
                                  self.ps[bank][:, 0:n].rearrange("p (h d) -> p h d", d=64), ['ps%d' % bank], ['vaug'])
                P.barrier()
            self.mem_attention(self.na_w[:, 2304:2560])
            with ExitStack() as t3:
                bias = [self.sb(t3, "bias%d" % i, [128, 21, 128], BF16) for i in range(2)]
                rdn = [self.sb(t3, "rdn%d" % i, [128, 1], F32) for i in range(2)]
                nh = 12 if self.stage != 'na_small' else 1
                for h in range(nh):
                    hb = h % 2
                    P.dma('pool', bias[hb][:], self.nab[h].rearrange("p (k n) -> p k n", n=128), writes=['bias%d' % hb], sem='bias%d' % hb)
                    pp = (h // 2) % 2
                    psl = slice(64 * (h % 2), 64 * (h % 2) + 64)
                    if h % 2 == 0:
                        for which, dst, col0, scale in (('q', qT[pp], h * 64, 0.125), ('k', kT[pp], 768 + h * 64, 1.0)):
                            for tb in range(4):
                                bank = 6 + (tb % 2)
                                for c in range(8):
                                    self.mm(self.ps[bank][:, :], wqk[:, c, col0:col0 + 128], self.xT[:, c, tb * 512:(tb + 1) * 512],
                                            c == 0, c == 7, ['wqk'] + self.xkb(tb), ['ps%d' % bank])
                                self.act(dst[:, tb * 512:(tb + 1) * 512], self.ps[bank][:, :], AF.Copy, ['ps%d' % bank],
                                         ['%sT%d' % (which, pp)], scale=scale)
                    def st_S(i):
                        kts, slot = _na_key_tiles(i)
                        pb = i % 2
                        ba, bb = 2 * pb, 2 * pb + 1
                        nk = len(kts)
                        n4 = min(nk, 4)
                        self.mm(self.ps[ba][:, 0:n4 * 128], self.ident_bf[:], bias[hb][:, slot:slot + n4, :].rearrange("p k n -> p (k n)"),
                                True, False, ['ident_bf', 'bias%d' % hb], ['ps%d' % ba])
                        if nk == 5:
                            self.mm(self.ps[bb][:, 0:128], self.ident_bf[:], bias[hb][:, slot + 4, :],
                                    True, False, ['ident_bf', 'bias%d' % hb], ['ps%d' % bb])
                        for j, kt in enumerate(kts):
                            bank, off = (ba, j * 128) if j < 4 else (bb, 0)
                            self.mm(self.ps[bank][:, off:off + 128], kT[pp][psl, kt * 128:(kt + 1) * 128], qT[pp][psl, i * 128:(i + 1) * 128],
                                    False, (j == n4 - 1) or (j == 4), ['kT%d' % pp, 'qT%d' % pp], ['ps%d' % bank])

                    def st_mid(i):
                        kts, slot = _na_key_tiles(i)
                        nk = len(kts)
                        pb = i % 2
                        ba, bb = 2 * pb, 2 * pb + 1
                        n4 = min(nk, 4)
                        self.act(p_bf[pb][:, 0:n4 * 128], self.ps[ba][:, 0:n4 * 128], AF.Exp, ['ps%d' % ba], ['p_bf%d' % pb])
                        if nk == 5:
                            self.act(p_bf[pb][:, 512:640], self.ps[bb][:, 0:128], AF.Exp, ['ps%d' % bb], ['p_bf%d' % pb])

                    def st_PV(i):
                        kts, slot = _na_key_tiles(i)
                        nk = len(kts)
                        pb = i % 2
                        bo = 4 + pb
                        for j, kt in enumerate(kts):
                            self.mm(self.ps[bo][:, 0:65], p_bf[pb][:, j * 128:(j + 1) * 128], vaug[:, kt, h, :], j == 0, j == nk - 1,
                                    ['p_bf%d' % pb, 'vaug'], ['ps%d' % bo])
                        rd = rdn[pb]
                        P.op('dve', lambda e, bo=bo, rd=rd: e.reciprocal(rd[:, 0:1], self.ps[bo][:, 64:65]), ['ps%d' % bo], ['rdn%d' % pb])
                        self.ts('dve', self.mixed[:, i, h * 64:(h + 1) * 64], self.ps[bo][:, 0:64], rd[:, 0:1], None,
                                ALU.mult, None, ['ps%d' % bo, 'rdn%d' % pb], ['mixed'])

                    st_S(0)
                    st_S(1)
                    st_mid(0)
                    for i in range(NT):
                        if i + 2 < NT:
                            st_S(i + 2)
                        if i + 1 < NT:
                            st_mid(i + 1)
                        st_PV(i)
                P.barrier()
            self.mixed_to_xT()
            P.barrier()

    def mixed_to_xT(self):
        for tt in range(NT):
            self.transpose_tile(self.mixed[:, tt, :], 'mixed', self.xT, 'xT', tt, 6 + (tt % 2))

    def layer1_mixer(self):
        P = self.P
        KS = 192 ** -0.5
        CH = ((0, 128), (128, 64))
        with ExitStack() as ph:
            sb = lambda n, s_, d: self.sb(ph, n, s_, d)
            self.mixed = sb("mixed", [128, NT, D], BF16)
            gts = sb("gts", [128, NT, 16], F32)
            lf, ig, bcol, colb, eb, blast, wexp, ebl, colf = (sb(n, [128, NT, 2, 4], F32) for n in
                                                               ("lf", "ig", "bcol", "colb", "eb", "blast", "wexp", "ebl", "colf"))
            wqkv = sb("wqkv", [128, 4, 3, 2, 256], BF16)
            cwb = sb("cwb", [128, 4, 2, 8], F32)
            ngs = sb("ngs", [128, 2, 768], F32)
            gbs = sb("gbs", [128, 16], F32)
            wgt = sb("wgt", [128, 8, 16], BF16)
            st6 = sb("hst6", [128, 6], F32)
            hmv = sb("hmv", [128, 4], F32)
            P.dma('sp', cwb[:], self.cwb.rearrange("p (h c k) -> p h c k", h=4, c=2), writes=['cwb'], sem='cwb')
            P.dma('sp', ngs[:], self.ngs.rearrange("p (a n) -> p a n", a=2), writes=['ngs'], sem='ngs')
            P.dma('sp', gbs[:], self.gb, writes=['gbs'], sem='gbs')
            P.op('pool', lambda e: e.memset(wqkv[:], 0.0), [], ['wqkv'])
            for a in range(3):
                for kc, (o, n) in enumerate(CH):
                    self.load_cast(wqkv[0:n, :, a, kc, 0:192], self.wqkv[a][:, o:o + n, :].rearrange("h p n -> p h n"),
                                   'wqkv', n, (4, 192))
            self.load_cast(wgt[:], self.ml_w[:, 1536:1552].rearrange("(c p) n -> p c n", p=128), 'wgt', 128, (8, 16))
            for tt in range(NT):
                bank = tt % 2
                for c in range(8):
                    self.mm(self.ps[bank][:, 0:16], self.xT[:, c, tt * 128:(tt + 1) * 128], wgt[:, c, :], c == 0, c == 7,
                            ['xT%d' % tt, 'wgt'], ['ps%d' % bank])
                self.tt('dve', gts[:, tt, :], self.ps[bank][:, 0:16], gbs[:], ALU.add, ['ps%d' % bank, 'gbs'], ['gts'])
            gv = gts[:].rearrange("p t (g h) -> p t g h", h=4)
            for d in range(2):
                self.copy('pool', ig[:, :, d, :], gv[:, :, 2 * d, :], ['gts'], ['ig'])
                self.act(lf[:, :, d, :], gv[:, :, 2 * d + 1, :], AF.Exp, ['gts'], ['lf'], scale=-1.0)
            self.act(lf[:], lf[:], AF.Ln, ['lf'], ['lf'], bias=1.0)
            self.ts('dve', lf[:], lf[:], -1.0, None, ALU.mult, None, ['lf'], ['lf'])
            for tt in range(NT):
                bank = 2 + (tt % 2)
                self.mm(self.ps[bank][:, 0:4], self.Uf, lf[:, tt, 0, :], True, True, ['lf', 'cst'], ['ps%d' % bank])
                self.mm(self.ps[bank][:, 4:8], self.Ub, lf[:, tt, 1, :], True, True, ['lf', 'cst'], ['ps%d' % bank])
                self.mm(self.ps[bank][:, 8:16], self.ones_f, lf[:, tt, :, :].rearrange("p d h -> p (d h)"), True, True,
                        ['lf', 'cst'], ['ps%d' % bank])
                self.copy('dve', bcol[:, tt, :, :].rearrange("p d h -> p (d h)"), self.ps[bank][:, 0:8], ['ps%d' % bank], ['bcol'])
                self.copy('act', blast[:, tt, :, :].rearrange("p d h -> p (d h)"), self.ps[bank][:, 8:16], ['ps%d' % bank], ['blast'])
            self.tt('dve', colb[:], ig[:], bcol[:], ALU.subtract, ['ig', 'bcol'], ['colb'])
            self.act(eb[:], bcol[:], AF.Exp, ['bcol'], ['eb'])
            self.act(colf[:], colb[:], AF.Exp, ['colb'], ['colf'])
            self.tt('dve', wexp[:], blast[:], colb[:], ALU.add, ['blast', 'colb'], ['wexp'])
            self.act(wexp[:], wexp[:], AF.Exp, ['wexp'], ['wexp'])
            self.act(ebl[:], blast[:], AF.Exp, ['blast'], ['ebl'])
            self.mem_attention(self.ml_w[:, 1552:1808])
            nh = 4 if self.stage != 'ml_small' else 1
            wxzs = [sb("wxz%d" % i, [128, 8, 2, 256], BF16) for i in range(2)]
            for i in range(2):
                P.op('pool', lambda e, i=i: e.memset(wxzs[i][:], 0.0), [], ['wxz%d' % i])

            def load_wxz(hh):
                for a, c0 in ((0, hh * 192), (1, 768 + hh * 192)):
                    self.load_cast(wxzs[hh % 2][:, :, a, 0:192], self.ml_w[:, c0:c0 + 192].rearrange("(c p) n -> p c n", p=128),
                                   'wxz%d' % (hh % 2), 128, (8, 192))
            load_wxz(0)
            for h in range(nh):
                with ExitStack() as hd:
                    sbh = lambda n, s_, d: self.sb(hd, n, s_, d)
                    qT = sbh("mqT", [128, 2, S], BF16)
                    kT = sbh("mkT_", [128, 2, S], BF16)
                    kw = sbh("kw", [128, NT, 2, 256], BF16)
                    P.op('pool', lambda e: e.memset(kw[:, :, :, 192:256], 0.0), [], ['kw'])
                    vaug = sbh("mvaug", [128, NT, 193], BF16)
                    xct = sbh("xct", [128, NT, 192], BF16)
                    zs = sbh("zs", [128, NT, 192], BF16)
                    with ExitStack() as t1:
                        sb1 = lambda n, s_, d: self.sb(t1, n, s_, d)
                        wxz = wxzs[h % 2]
                        wk = 'wxz%d' % (h % 2)
                        xmT = sb1("xmT", [128, 2, S + 4], BF16)
                        xcT = sb1("xcT", [128, 2, S], BF16)
                        dg = sb1("dg", [128, 2, 5, 128], BF16)
                        if h + 1 < nh:
                            load_wxz(h + 1)
                        P.op('pool', lambda e: e.memset(xmT[:, :, 0:2], 0.0), [], ['xmT'])
                        P.op('pool', lambda e: e.memset(xmT[:, :, S + 2:S + 4], 0.0), [], ['xmT'])
                        for ch, (o, n) in enumerate(CH):
                            for j in range(5):
                                self.ts('dve', dg[:, ch, j, :], self.ident_f, cwb[:, h, ch, j:j + 1], None, ALU.mult, None,
                                        ['cst', 'cwb'], ['dg'])
                        for ch, (o, n) in enumerate(CH):
                            for tb in range(4):
                                bank = 2 + (tb % 2)
                                for c in range(8):
                                    self.mm(self.ps[bank][:, :], wxz[:, c, 0, o:o + 128], self.xT[:, c, tb * 512:(tb + 1) * 512],
                                            c == 0, c == 7, [wk] + self.xkb(tb), ['ps%d' % bank])
                                self.copy('act' if tb % 2 == 0 else 'dve', xmT[:, ch, 2 + tb * 512:2 + (tb + 1) * 512], self.ps[bank][:, :],
                                          ['ps%d' % bank], ['xmT'])
                        for tt in range(NT):
                            bank = tt % 2
                            for c in range(8):
                                self.mm(self.ps[bank][:, 0:192], self.xT[:, c, tt * 128:(tt + 1) * 128], wxz[:, c, 1, 0:192], c == 0, c == 7,
                                        ['xT%d' % tt, wk], ['ps%d' % bank])
                            self.act(zs[:, tt, :], self.ps[bank][:, 0:192], AF.Silu, ['ps%d' % bank], ['zs'])
                        for ch, (o, n) in enumerate(CH):
                            for tb in range(4):
                                bank = 4 + (tb % 2)
                                for j in range(5):
                                    self.mm(self.ps[bank][:, :], dg[:, ch, j, :], xmT[:, ch, tb * 512 + j:tb * 512 + j + 512],
                                            j == 0, j == 4, ['dg', 'xmT'], ['ps%d' % bank])
                                self.act(xcT[:, ch, tb * 512:(tb + 1) * 512], self.ps[bank][:, :], AF.Silu, ['ps%d' % bank, 'cwb'], ['xcT'],
                                         bias=cwb[:, h, ch, 5:6])
                        P.op('pool', lambda e: e.memset(vaug[:, :, 192:193], 1.0), [], ['mvaug'])
                        for tt in range(NT):
                            bank = 4 + (tt % 2)
                            for kc, (o, n) in enumerate(CH):
                                self.mm(self.ps[bank][:, 0:192], xmT[:, kc, 2 + tt * 128:2 + (tt + 1) * 128], wqkv[:, h, 2, kc, 0:192], kc == 0, kc == 1,
                                        ['xmT', 'wqkv'], ['ps%d' % bank])
                            self.copy('act' if tt % 2 == 0 else 'dve', vaug[:, tt, 0:192], self.ps[bank][:, 0:192], ['ps%d' % bank], ['mvaug'])
                        for a_, dst, key, scale in ((0, qT, 'mqT', 1.0), (1, kT, 'mkT_', KS)):
                            for oc, (oo, on) in enumerate(CH):
                                for tb in range(4):
                                    bank = 6 + (tb % 2)
                                    for kc, (o, n) in enumerate(CH):
                                        self.mm(self.ps[bank][:, :], wqkv[:, h, a_, kc, oo:oo + 128], xcT[:, kc, tb * 512:(tb + 1) * 512],
                                                kc == 0, kc == 1, ['xcT', 'wqkv'], ['ps%d' % bank])
                                    self.act(dst[:, oc, tb * 512:(tb + 1) * 512], self.ps[bank][:, :], AF.Copy, ['ps%d' % bank], [key],
                                             scale=scale)
                        for tt in range(NT):
                            bank = tt % 2
                            for kc, (o, n) in enumerate(CH):
                                self.mm(self.ps[bank][:, 0:192], xcT[:, kc, tt * 128:(tt + 1) * 128], wqkv[:, h, 1, kc, 0:192], kc == 0, kc == 1,
                                        ['xcT', 'wqkv'], ['ps%d' % bank])
                            for d in range(2):
                                self.ts('dve', kw[:, tt, d, 0:192], self.ps[bank][:, 0:192], wexp[:, tt, d, h:h + 1], KS, ALU.mult, ALU.mult,
                                        ['ps%d' % bank, 'wexp'], ['kw'])
                        for tt in range(NT):
                            bank = 2 + (tt % 2)
                            for kc, (o, n) in enumerate(CH):
                                self.tr(self.psb[bank][:, o:o + 128], xcT[:, kc, tt * 128:(tt + 1) * 128], self.ident_bf[:],
                                        ['xcT', 'ident_bf'], ['ps%d' % bank])
                            self.copy('act' if tt % 2 == 0 else 'dve', xct[:, tt, :], self.psb[bank][:, 0:192], ['ps%d' % bank], ['xct'])
                        P.barrier()
                    with ExitStack() as t2:
                        sb2 = lambda n, s_, d: self.sb(t2, n, s_, d)
                        hraw = sb2("hraw", [128, 2, NT, 193], F32)
                        Pm = [[sb2("Pm%d%d" % (i, d), [128, 128], BF16) for d in range(2)] for i in range(2)]
                        C = sb2("Cst", [128, 2, 2, 193], F32)
                        Cbf = [sb2("Cbf%d" % i, [128, 2, 2, 193], BF16) for i in range(2)]
                        dn = sb2("dn", [128, 2 * NT, 1], F32)
                        sm = sb2("sm", [128, NT, 1], F32)
                        sq = sb2("sq", [128, NT, 1], F32)
                        msk = (self.Uf, self.Ub)

                        def tt_of(c, d):
                            return c if d == 0 else NT - 1 - c

                        def st_A(c):
                            cp = c % 2
                            for d in range(2):
                                tt = tt_of(c, d)
                                tsl = slice(tt * 128, (tt + 1) * 128)
                                self.mm(self.ps[cp][:, d * 128:(d + 1) * 128], kT[0:128, 0, tsl], qT[0:128, 0, tsl], True, False,
                                        ['mkT_', 'mqT'], ['ps%d' % cp])
                                self.mm(self.ps[cp][:, d * 128:(d + 1) * 128], kT[:, 1, tsl], qT[:, 1, tsl], False, True,
                                        ['mkT_', 'mqT'], ['ps%d' % cp])
                            if c < NT - 1:
                                for d in range(2):
                                    tt = tt_of(c, d)
                                    b2 = 2 + cp * 2 + d
                                    for kc, (o, n) in enumerate(CH):
                                        self.mm(self.ps[b2][:, kc * 256:kc * 256 + 193], kw[:, tt, d, o:o + 128], vaug[:, tt, :], True, True,
                                                ['kw', 'mvaug'], ['ps%d' % b2])
                            for d in range(2):
                                tt = tt_of(c, d)
                                self.stt(Pm[cp][d][:], self.ps[cp][:, d * 128:(d + 1) * 128], colf[:, tt, d, h:h + 1], msk[d],
                                         ALU.mult, ALU.mult, ['ps%d' % cp, 'colf', 'cst'], ['Pm%d%d' % (cp, d)])

                        def st_B(c):
                            cp = c % 2
                            bh = 6 + cp
                            for d in range(2):
                                tt = tt_of(c, d)
                                tsl = slice(tt * 128, (tt + 1) * 128)
                                o_ = self.ps[bh][:, d * 256:d * 256 + 193]
                                self.mm(o_, Pm[cp][d][:], vaug[:, tt, :], True, c == 0, ['Pm%d%d' % (cp, d), 'mvaug'], ['ps%d' % bh])
                                if c > 0:
                                    pk = 'Cbf%d%d' % (1 - cp, d)
                                    self.mm(o_, qT[0:128, 0, tsl], Cbf[1 - cp][0:128, d, 0, :], False, False, ['mqT', pk], ['ps%d' % bh])
                                    self.mm(o_, qT[:, 1, tsl], Cbf[1 - cp][:, d, 1, :], False, True, ['mqT', pk], ['ps%d' % bh])
                            if c < NT - 1:
                                for d in range(2):
                                    tt = tt_of(c, d)
                                    b2 = 2 + cp * 2 + d
                                    psv = self.ps[b2][:, 0:512].rearrange("p (k n) -> p k n", n=256)[:, :, 0:193]
                                    if c == 0:
                                        self.copy('dve', C[:, d, :, :], psv, ['ps%d' % b2], ['C%d' % d])
                                    else:
                                        self.stt(C[:, d, :, :], C[:, d, :, :], ebl[:, tt, d, h:h + 1], psv, ALU.mult, ALU.add,
                                                 ['C%d' % d, 'ebl', 'ps%d' % b2], ['C%d' % d])
                                    self.copy('act', Cbf[cp][:, d, :, :], C[:, d, :, :], ['C%d' % d], ['Cbf%d%d' % (cp, d)])
                            for d in range(2):
                                tt = tt_of(c, d)
                                self.act(hraw[:, d, tt, :], self.ps[bh][:, d * 256:d * 256 + 193], AF.Copy, ['ps%d' % bh, 'eb'], ['hraw%d_%d' % (d, tt)],
                                         scale=eb[:, tt, d, h:h + 1])

                        st_A(0)
                        for c in range(NT):
                            if c + 1 < NT:
                                st_A(c + 1)
                            st_B(c)
                        denv = hraw[:, :, :, 192:193].rearrange("p d t o -> p (d t) o")
                        allh = ['hraw%d_%d' % (d_, t_) for d_ in range(2) for t_ in range(NT)]
                        self.stt(dn[:], denv, -1.0, denv, ALU.mult, ALU.max, allh, ['dn', 'hraw'])
                        self.ts('dve', dn[:], dn[:], 1.0, None, ALU.max, None, ['dn'], ['dn'])
                        P.op('dve', lambda e: e.reciprocal(dn[:], dn[:]), ['dn'], ['dn'])
                        for d in range(2):
                            self.tt('dve', hraw[:, d, :, 0:192], hraw[:, d, :, 0:192],
                                    dn[:, d * NT:(d + 1) * NT, :].to_broadcast([128, NT, 192]), ALU.mult, ['hraw', 'dn'], ['hraw'])
                        h0 = hraw[:, 0, :, 0:192]
                        h1 = hraw[:, 1, :, 0:192]
                        self.tt('dve', h0, h0, h1, ALU.add, ['hraw'], ['hraw'])
                        P.op('dve', lambda e: e.tensor_reduce(sm[:], h0, AX.X, ALU.add), ['hraw'], ['sm'])
                        self.ts('dve', sm[:], sm[:], 1.0 / 192, None, ALU.mult, None, ['sm'], ['sm'])
                        self.tt('dve', h0, h0, sm[:].to_broadcast([128, NT, 192]), ALU.subtract, ['hraw', 'sm'], ['hraw'])
                        self.tt('dve', h1, h0, h0, ALU.mult, ['hraw'], ['hraw'])
                        P.op('dve', lambda e: e.tensor_reduce(sq[:], h1, AX.X, ALU.add), ['hraw'], ['sq'])
                        self.act(sq[:], sq[:], AF.Sqrt, ['sq'], ['sq'], bias=self.eps_ap[:, 0:1], scale=1.0 / 192)
                        P.op('dve', lambda e: e.reciprocal(sq[:], sq[:]), ['sq'], ['sq'])
                        self.tt('dve', h0, h0, sq[:].to_broadcast([128, NT, 192]), ALU.mult, ['hraw', 'sq'], ['hraw'])
                        gview = ngs[:, 0, h * 192:(h + 1) * 192].unsqueeze(1).to_broadcast([128, NT, 192])
                        sview = ngs[:, 1, h * 192:(h + 1) * 192].unsqueeze(1).to_broadcast([128, NT, 192])
                        self.tt('dve', h0, h0, gview, ALU.mult, ['hraw', 'ngs'], ['hraw'])
                        self.tt('dve', h1, xct[:], sview, ALU.mult, ['xct', 'ngs', 'hraw'], ['hraw'])
                        self.tt('dve', h0, h0, h1, ALU.add, ['hraw'], ['hraw'])
                        self.tt('dve', self.mixed[:, :, h * 192:(h + 1) * 192], h0, zs[:], ALU.mult, ['hraw', 'zs'], ['mixed'])
                        P.barrier()
            self.mixed_to_xT()
            P.barrier()


def _na_bias_index():
    idx = np.full((21, 128, 128), 465, dtype=np.int64)
    for i in [0, 1, 2, 14, 15]:
        kts, slot = _na_key_tiles(i)
        qi = np.arange(128)
        r = 2 * i + qi // 64
        c = qi % 64
        rs = np.clip(r - 4, 0, 24)
        cs = np.clip(c - 8, 0, 48)
        for j, kt in enumerate(kts):
            ki = np.arange(128)
            kr = (2 * kt + ki // 64)[:, None]
            kc = (ki % 64)[:, None]
            valid = (kr >= rs[None]) & (kr < rs[None] + 8) & (kc >= cs[None]) & (kc < cs[None] + 16)
            flat = (kr - r[None] + 7) * 31 + (kc - c[None] + 15)
            idx[slot + j] = np.where(valid, flat, 465)
    return idx


_CACHE = {}


def _host_inputs(inp):
    f = lambda a: np.ascontiguousarray(np.asarray(a, dtype=np.float32))
    rep = lambda v: np.ascontiguousarray(np.broadcast_to(np.asarray(v, np.float32).reshape(1, -1), (128, np.asarray(v).size)))
    ln_g, ln_b = f(inp['ln_g']), f(inp['ln_b'])
    lnp = np.stack([np.concatenate([rep(inp['mem_ln_g']), rep(inp['mem_ln_b'])], 1)] +
                   [np.concatenate([rep(ln_g[l, k]), rep(ln_b[l, k])], 1) for l in range(2) for k in range(2)], 0)
    rpb = f(inp['na_rpb'])[0].reshape(12, 465)
    rpb_ext = np.concatenate([rpb, np.full((12, 1), NEG, np.float32)], 1)
    idx = _na_bias_index()
    nab = rpb_ext[:, idx]
    nab = np.ascontiguousarray(nab.transpose(0, 2, 1, 3).reshape(12, 128, 21 * 128))
    cw = f(inp['ml_conv_w'])[0]
    cb = f(inp['ml_conv_b'])[0]
    cwb = np.zeros((128, 4, 2, 8), np.float32)
    for h in range(4):
        for ch, (o, n) in enumerate(((0, 128), (128, 64))):
            f0 = h * 192 + o
            cwb[:n, h, ch, 0:5] = cw[:, f0:f0 + n].T
            cwb[:n, h, ch, 5] = cb[f0:f0 + n]
    ii = np.arange(128)
    ident = np.eye(128, dtype=np.float32)
    Uf = (ii[:, None] <= ii[None, :]).astype(np.float32)
    Ub = (ii[:, None] >= ii[None, :]).astype(np.float32)
    mnf = np.where(ii[:, None] <= ii[None, :], 0.0, NEG).astype(np.float32)
    mnb = np.where(ii[:, None] >= ii[None, :], 0.0, NEG).astype(np.float32)
    consts = np.stack([ident, Uf, Ub, mnf, mnb, np.ones((128, 128), np.float32)], 1).reshape(128, 6 * 128)
    def relay(w, nchunk):
        w = f(w)
        L, E, K, N = w.shape
        w = w.reshape(L, E, 2, nchunk // 2, 128, N).transpose(0, 2, 1, 4, 3, 5)
        return np.ascontiguousarray(w.reshape(L * 2 * E * 128, (nchunk // 2) * N))
    consts2 = np.zeros((128, NB * 16 + 1), np.float32)
    consts2[:, :NB * 16] = np.repeat(np.arange(NB, dtype=np.float32), 16)[None, :]
    consts2[:, NB * 16] = np.arange(128, dtype=np.float32)
    shared = {
        'lnp': np.ascontiguousarray(lnp), 'w_mem_kv': f(inp['w_mem_kv']), 'router_w': f(inp['router_w']),
        'router_b': rep(inp['router_b']), 'na_w_in': f(inp['na_w_in'])[0], 'na_bias': nab,
        'ml_w_in': f(inp['ml_w_in'])[0], 'ml_cwb': cwb.reshape(128, 64), 'ml_w_qkv': f(inp['ml_w_qkv'])[0],
        'ml_gate_b': rep(f(inp['ml_gate_b'])[0].reshape(-1)),
        'ml_ngs': np.concatenate([rep(f(inp['ml_norm_g'])[0]), rep(f(inp['ml_skip'])[0])], 1),
        'w_out': f(inp['w_out']), 'exp_w_gate': relay(inp['exp_w_gate'], 8), 'exp_w_up': relay(inp['exp_w_up'], 8),
        'exp_w_down': relay(inp['exp_w_down'], 4), 'consts': np.ascontiguousarray(consts), 'consts2': consts2,
    }
    return shared


def run(inputs, stage='full', cores=8):
    if stage not in _CACHE:
        b = Builder(stage)
        b.build()
        _CACHE[stage] = b.nc
    nc = _CACHE[stage]
    shared = _host_inputs(inputs)
    x = np.asarray(inputs['x'], np.float32)
    mem = np.asarray(inputs['mem'], np.float32)
    in_maps = []
    for c in range(cores):
        m = dict(shared)
        m['x'] = np.ascontiguousarray(x[c])
        m['mem'] = np.ascontiguousarray(mem[c])
        in_maps.append(m)
    res = run_bass_kernel_spmd(nc, in_maps, core_ids=list(range(cores)))
    return np.stack([np.asarray(r['y'], np.float32) for r in res.results], 0)


def kernel(**inputs):
    return run(inputs, 'full', 8)
```

```python
import numpy as np
import concourse.bass as bass
import concourse.mybir as mybir
from concourse.bass_utils import run_bass_kernel_spmd
from contextlib import ExitStack

F32 = mybir.dt.float32
BF16 = mybir.dt.bfloat16
AF = mybir.ActivationFunctionType
ALU = mybir.AluOpType
AX = mybir.AxisListType


class Prog:
    CE = ['pe', 'act', 'dve', 'pool']

    def __init__(self, nc, st):
        self.nc = nc
        self.st = st
        self.engs = {'pe': nc.tensor, 'act': nc.scalar, 'dve': nc.vector, 'pool': nc.gpsimd, 'sp': nc.sync}
        self.sems = {e: st.enter_context(nc.semaphore("s_" + e)) for e in self.CE}
        self.cnt = {e: 0 for e in self.CE}
        self.seen = {e: {} for e in self.CE + ['sp']}
        self.last_w = {}
        self.readers = {}
        self.out_sems = []

    def _sem(self, name):
        if name not in self.sems:
            self.sems[name] = self.st.enter_context(self.nc.semaphore("d_" + name))
            self.cnt[name] = 0
        return self.sems[name]

    def _deps(self, engine, reads, writes, skip_waw_sem=None):
        deps = {}

        def add(ev):
            sk, val, eng = ev
            if eng == 'pe' and engine == 'pe':
                return
            if deps.get(sk, 0) < val:
                deps[sk] = val
        for k in reads:
            if k in self.last_w:
                add(self.last_w[k])
            if k.startswith('ps'):
                for sk, (val, eng) in self.readers.get(k, {}).items():
                    if eng != engine:
                        add((sk, val, eng))
        for k in writes:
            if k in self.last_w:
                ev = self.last_w[k]
                if not (skip_waw_sem is not None and ev[0] == skip_waw_sem and not self.readers.get(k)):
                    add(ev)
            for sk, (val, eng) in self.readers.get(k, {}).items():
                add((sk, val, eng))
        waits = []
        for sk, val in deps.items():
            if self.seen[engine].get(sk, 0) >= val:
                continue
            self.seen[engine][sk] = val
            waits.append((sk, val))
        return waits

    def _record(self, ev, reads, writes):
        for k in writes:
            self.last_w[k] = ev
            self.readers[k] = {}
        for k in reads:
            r = self.readers.setdefault(k, {})
            if r.get(ev[0], (0, None))[0] < ev[1]:
                r[ev[0]] = (ev[1], ev[2])

    def op(self, engine, fn, reads=(), writes=()):
        waits = self._deps(engine, reads, writes)
        self.cnt[engine] += 1
        ev = (engine, self.cnt[engine], engine)
        self._record(ev, reads, writes)
        self._emit(engine, waits, fn, (engine, 1))

    def _emit(self, engine, waits, fn, inc):
        eng = self.engs[engine]
        for sk, val in waits:
            eng.wait_ge(self.sems[sk], val)
        if fn is not None:
            ins = fn(eng)
            ins.then_inc(self.sems[inc[0]], inc[1])

    def dma(self, queue, out_ap, in_ap, reads=(), writes=(), sem=None, out=False, **kw):
        self._sem(sem)
        waits = self._deps(queue, reads, writes, skip_waw_sem=sem)
        self.cnt[sem] += 16
        ev = (sem, self.cnt[sem], 'dma')
        self._record(ev, reads, writes)
        self._emit(queue, waits, lambda e: e.dma_start(out=out_ap, in_=in_ap, **kw), (sem, 16))
        if out and sem not in self.out_sems:
            self.out_sems.append(sem)

    def idma(self, out_ap, in_ap, idx_ap, scatter, reads=(), writes=(), sem=None, bounds=None):
        self._sem(sem)
        waits = self._deps('pool', reads, writes, skip_waw_sem=sem)
        self.cnt[sem] += 16
        ev = (sem, self.cnt[sem], 'dma')
        self._record(ev, reads, writes)
        off = bass.IndirectOffsetOnAxis(ap=idx_ap, axis=0)
        if scatter:
            fn = lambda e: e.indirect_dma_start(out=out_ap, out_offset=off, in_=in_ap, in_offset=None)
        else:
            if bounds is None:
                fn = lambda e: e.indirect_dma_start(out=out_ap, out_offset=None, in_=in_ap, in_offset=off)
            else:
                if getattr(self, 'bnd_reg', None) is None:
                    self.bnd_reg = self.nc.gpsimd.alloc_register('bnd')
                    self.nc.gpsimd.reg_mov(self.bnd_reg, bounds)
                reg = self.bnd_reg
                fn = lambda e: e.indirect_dma_start(out=out_ap, out_offset=None, in_=in_ap, in_offset=off,
                                                    bounds_check=reg, oob_is_err=False)
        self._emit('pool', waits, fn, (sem, 16))

    def barrier(self):
        for e in self.CE + ['sp']:
            waits = []
            for sk, c in self.cnt.items():
                if c > 0 and self.seen[e].get(sk, 0) < c and not (sk == e):
                    self.seen[e][sk] = c
                    waits.append((sk, c))
            if waits:
                self._emit(e, waits, None, None)
        for e in self.CE:
            if self.cnt[e] > 0 and self.seen[e].get(e, 0) < self.cnt[e]:
                self.seen[e][e] = self.cnt[e]
                self._emit(e, [(e, self.cnt[e])], None, None)

    def finish(self):
        self.barrier()


S = 2048
D = 1024
NT = 16
ALPHA = (2 * 2) ** 0.25
LN_EPS = 1e-5
NEG = -30000.0
BS = 384
QT = BS // 128
NB = 27
I32 = mybir.dt.int32


def _na_key_tiles(i):
    if i < 2:
        return [0, 1, 2, 3], (0 if i == 0 else 4)
    if i > 13:
        return [12, 13, 14, 15], (13 if i == 14 else 17)
    return [i - 2, i - 1, i, i + 1, i + 2], 8


class Builder:
    def __init__(self, stage):
        self.stage = stage
        self.nc = nc = bass.Bass("TRN2", target_bir_lowering=False)

        def din(name, shape):
            return nc.dram_tensor(name, list(shape), F32, kind="ExternalInput").ap()
        self.x = din("x", [S, D])
        self.mem = din("mem", [256, D])
        self.lnp = din("lnp", [5, 128, 2 * D])
        self.wmkv = din("w_mem_kv", [D, 512])
        self.rw = din("router_w", [D, 16])
        self.rb = din("router_b", [128, 16])
        self.na_w = din("na_w_in", [D, 2560])
        self.nab = din("na_bias", [12, 128, 21 * 128])
        self.ml_w = din("ml_w_in", [D, 1808])
        self.cwb = din("ml_cwb", [128, 64])
        self.wqkv = din("ml_w_qkv", [3, 4, 192, 192])
        self.gb = din("ml_gate_b", [128, 16])
        self.ngs = din("ml_ngs", [128, 2 * 768])
        self.wout = din("w_out", [2, D, D])
        self.wg = din("exp_w_gate", [4 * 2048, 2048])
        self.wu = din("exp_w_up", [4 * 2048, 2048])
        self.wd = din("exp_w_down", [4 * 2048, 2048])
        self.consts = din("consts", [128, 6 * 128])
        self.consts2 = din("consts2", [128, NB * 16 + 1])
        self.y = nc.dram_tensor("y", [S, D], F32, kind="ExternalOutput").ap()
        self.xs = nc.dram_tensor("xs", [S, D], F32, kind="Internal").ap()
        self.xsorted = nc.dram_tensor("xsorted", [NB * BS, D], BF16, kind="Internal").ap()
        self.ysorted = nc.dram_tensor("ysorted", [NB * BS, D], F32, kind="Internal").ap()

    def xkb(self, tb):
        return ['xT%d' % t for t in range(tb * 4, tb * 4 + 4)]

    def sb(self, st, name, shape, dt):
        self._uid = getattr(self, '_uid', 0) + 1
        return st.enter_context(self.nc.sbuf_tensor("%s_%d" % (name, self._uid), list(shape), dt))

    def mm(self, out, lhsT, rhs, start, stop, reads, writes):
        self.P.op('pe', lambda e: e.matmul(out, lhsT, rhs, start=start, stop=stop), reads, writes)

    def tr(self, out, in_, ident, reads, writes):
        self.P.op('pe', lambda e: e.transpose(out, in_, ident), reads, writes)

    def act(self, out, in_, func, reads, writes, bias=0.0, scale=1.0, eng='act'):
        self.P.op('act', lambda e: e.activation(out, in_, func, bias=bias, scale=scale), reads, writes)

    def copy(self, eng, out, in_, reads, writes):
        if eng == 'act':
            self.P.op('act', lambda e: e.copy(out, in_), reads, writes)
        else:
            self.P.op(eng, lambda e: e.tensor_copy(out, in_), reads, writes)

    def tt(self, eng, out, a, b, op, reads, writes):
        self.P.op(eng, lambda e: e.tensor_tensor(out, a, b, op), reads, writes)

    def ts(self, eng, out, a, s1, s2, op0, op1, reads, writes):
        if op1 is None:
            self.P.op(eng, lambda e: e.tensor_scalar(out, a, s1, None, op0), reads, writes)
        else:
            self.P.op(eng, lambda e: e.tensor_scalar(out, a, s1, s2, op0, op1), reads, writes)

    def stt(self, out, a, s, b, op0, op1, reads, writes):
        self.P.op('dve', lambda e: e.scalar_tensor_tensor(out, a, s, b, op0, op1), reads, writes)

    def load_cast(self, dst_ap, src_ap, dst_key, nparts, shape):
        self.P.dma('pool', dst_ap, src_ap, writes=[dst_key], sem='lc_' + dst_key)

    def ln_stages(self, src, dst, src_key, dst_key, lnp_key, slot, bias_eng='pool'):
        P = self.P
        st6 = self.ln_st[:, slot * 12:(slot + 1) * 12]
        mv = self.ln_mv[:, slot * 8:(slot + 1) * 8]
        kst, kmv = 'ln_st%d' % slot, 'ln_mv%d' % slot

        def sA():
            for hlf in range(2):
                P.op('dve', lambda e, hlf=hlf: e.bn_stats(st6[:, hlf * 6:(hlf + 1) * 6], src[:, hlf * 512:(hlf + 1) * 512]),
                     [src_key], [kst])
            P.op('dve', lambda e: e.bn_aggr(mv[:, 0:2], st6[:, 0:12]), [kst], [kmv])

        def sB():
            self.act(mv[:, 2:3], mv[:, 1:2], AF.Sqrt, [kmv], [kmv], bias=self.eps_ap[:, 0:1])

        def sC():
            P.op('dve', lambda e: e.reciprocal(mv[:, 3:4], mv[:, 2:3]), [kmv], [kmv])
            self.ts('dve', mv[:, 4:5], mv[:, 0:1], mv[:, 3:4], -1.0, ALU.mult, ALU.mult, [kmv], [kmv])

        def sD():
            P.op('act', lambda e: e.activation(src, src, AF.Identity, bias=mv[:, 4:5], scale=mv[:, 3:4]), [src_key, kmv], [src_key])

        def sE():
            self.tt('dve', src, src, self.lnp_sb[:, 0:D], ALU.mult, [src_key, lnp_key], [src_key])
            self.tt(bias_eng, dst, src, self.lnp_sb[:, D:2 * D], ALU.add, [src_key, lnp_key], [dst_key])
        return [sA, sB, sC, sD, sE]

    def ln_multi(self, n, tile_fn, lnp_key, pre=None, post=None, bias_eng='pool'):
        stages = {}
        for i in range(n + 4):
            if i < n:
                if pre is not None:
                    pre(i)
                stages[i] = self.ln_stages(*tile_fn(i), lnp_key, i % 4, bias_eng)
            for k in range(5):
                t = i - k
                if 0 <= t < n:
                    stages[t][k]()
                    if k == 4 and post is not None:
                        post(t)

    def transpose_tile(self, src_bf, src_key, dstT, dst_key, tt, bank):
        pst = self.psb[bank]
        for c in range(8):
            self.tr(pst[:, c * 128:(c + 1) * 128], src_bf[:, c * 128:(c + 1) * 128], self.ident_bf[:],
                    [src_key, 'ident_bf'], ['ps%d' % bank])
        eng = 'act' if (tt % 2 == 0) else 'dve'
        self.copy(eng, dstT[:, :, tt * 128:(tt + 1) * 128], pst[:, 0:1024].rearrange("p (c n) -> p c n", n=128),
                  ['ps%d' % bank], ['%s%d' % (dst_key, tt)])

    def mem_attention(self, w_ap_cols):
        with ExitStack() as t2:
            self.wqm = self.sb(t2, "wqm", [128, 8, 256], BF16)
            self.qmT = self.sb(t2, "qmT", [64, S], BF16)
            self.pmem = [self.sb(t2, "pmem%d" % i, [128, 2, 512], BF16) for i in range(2)]
            self.rd4 = [self.sb(t2, "rd4%d" % i, [128, 4, 1], F32) for i in range(2)]
            self._mem_attention(w_ap_cols)
            self.P.barrier()

    def _mem_attention(self, w_ap_cols):
        P = self.P
        wqm = self.wqm
        self.load_cast(wqm[:], w_ap_cols.rearrange("(c p) n -> p c n", p=128), 'wqm', 128, (8, 256))
        for h in range(4):
            for tb in range(4):
                bank = 6 + (tb % 2)
                for c in range(8):
                    self.mm(self.ps[bank][0:64, :], wqm[:, c, h * 64:(h + 1) * 64], self.xT[:, c, tb * 512:(tb + 1) * 512],
                            c == 0, c == 7, ['wqm'] + self.xkb(tb), ['ps%d' % bank])
                self.copy('act' if tb % 2 == 0 else 'dve', self.qmT[0:64, tb * 512:(tb + 1) * 512], self.ps[bank][0:64, :],
                          ['ps%d' % bank], ['qmT'])
            for tb in range(4):
                pt = self.pmem[tb % 2]
                ptk = 'pmem%d' % (tb % 2)
                for mt in range(2):
                    bank = 0 + mt
                    self.mm(self.ps[bank][:, :], self.mkT[0:64, h, mt * 128:(mt + 1) * 128],
                            self.qmT[0:64, tb * 512:(tb + 1) * 512], True, True, ['mkT', 'qmT'], ['ps%d' % bank])
                    self.act(pt[:, mt, :], self.ps[bank][:, :], AF.Exp, ['ps%d' % bank], [ptk], scale=0.125)
                bank = 4 + (tb % 2)
                for q in range(4):
                    for mt in range(2):
                        self.mm(self.ps[bank][:, q * 65:q * 65 + 65], pt[:, mt, q * 128:(q + 1) * 128], self.mv[:, mt, h, :],
                                mt == 0, mt == 1, [ptk, 'mv'], ['ps%d' % bank])
                pv = self.ps[bank][:, 0:260].rearrange("p (q e) -> p q e", e=65)
                rd4 = self.rd4[tb % 2]
                P.op('dve', lambda e, pv=pv, rd4=rd4: e.reciprocal(rd4[:], pv[:, :, 64:65]), ['ps%d' % bank], ['rd4%d' % (tb % 2)])
                self.tt('dve', self.mixed[:, tb * 4:(tb + 1) * 4, 768 + h * 64:768 + (h + 1) * 64], pv[:, :, 0:64],
                        rd4[:].to_broadcast([128, 4, 64]), ALU.mult, ['ps%d' % bank, 'rd4%d' % (tb % 2)], ['mixed'])

    def out_proj_ln(self, li, res_src):
        P = self.P
        for hlf in range(2):
            for kh in range(2):
                self.load_cast(self.wo[:, kh * 4:(kh + 1) * 4, hlf * 512:(hlf + 1) * 512],
                               self.wout[li][kh * 512:(kh + 1) * 512, hlf * 512:(hlf + 1) * 512].rearrange("(c p) n -> p c n", p=128),
                               'wo', 128, (4, 512))
        P.dma('sp', self.lnp_sb[:], self.lnp[1 + 2 * li], writes=['lnp'], sem='lnp')

        def pre(tt):
            xr = self.xres[tt % 2]
            xrk = 'xres%d' % (tt % 2)
            P.dma('sp', xr[:], res_src[tt * 128:(tt + 1) * 128, :], writes=[xrk], sem=xrk)
            for hlf in range(2):
                bank = hlf + 2 * (tt % 2)
                for c in range(8):
                    self.mm(self.ps[bank][:, :], self.xT[:, c, tt * 128:(tt + 1) * 128], self.wo[:, c, hlf * 512:(hlf + 1) * 512],
                            c == 0, c == 7, ['xT%d' % tt, 'wo'], ['ps%d' % bank])
                self.stt(self.X[:, tt, hlf * 512:(hlf + 1) * 512], xr[:, hlf * 512:(hlf + 1) * 512], ALPHA,
                         self.ps[bank][:, :], ALU.mult, ALU.add, [xrk, 'ps%d' % bank], ['X%d' % tt])
        self.ln_multi(NT, lambda tt: (self.X[:, tt, :], self.X[:, tt, :], 'X%d' % tt, 'X%d' % tt), 'lnp', pre=pre)

    def moe_route(self, li):
        P = self.P
        P.dma('sp', self.rw_sb[:], self.rw.rearrange("(c p) n -> p c n", p=128), writes=['rw'], sem='rw')
        P.dma('sp', self.rb_sb[:], self.rb, writes=['rb'], sem='rb')
        def st_T(tt):
            pp = tt % 2
            for c in range(8):
                bank = 2 * pp + (0 if c < 4 else 1)
                self.mm(self.ps[bank][:, (c % 4) * 128:(c % 4 + 1) * 128], self.X[:, tt, c * 128:(c + 1) * 128],
                        self.ident_f, True, True, ['X%d' % tt, 'ident_f'], ['ps%d' % bank])

        def st_R(tt):
            pp = tt % 2
            xtf = self.xtf[pp]
            for b_ in range(2):
                bank = 2 * pp + b_
                self.copy('dve', xtf[:, b_ * 4:(b_ + 1) * 4, :],
                          self.ps[bank][:, :].rearrange("p (c n) -> p c n", n=128), ['ps%d' % bank], ['xtf%d' % pp])
            for c in range(8):
                self.mm(self.ps[4 + pp][:, 0:16], xtf[:, c, :], self.rw_sb[:, c, :], c == 0, c == 7, ['xtf%d' % pp, 'rw'], ['ps%d' % (4 + pp)])
            self.copy('dve', self.lg[:, tt, :], self.ps[4 + pp][:, 0:16], ['ps%d' % (4 + pp)], ['lg'])
        st_T(0)
        for tt in range(NT):
            if tt + 1 < NT:
                st_T(tt + 1)
            st_R(tt)
        lg = self.lg
        sc, bi, t1, t2, t3 = self.r_sc, self.r_bi, self.r_t1, self.r_t2, self.r_t3
        self.act(sc[:], lg[:], AF.Sigmoid, ['lg'], ['r_sc'])
        self.tt('dve', bi[:], sc[:], self.rb_sb[:].unsqueeze(1).to_broadcast([128, NT, 16]), ALU.add, ['r_sc', 'rb'], ['r_bi'])
        g4 = lambda t: t[:].rearrange("p t (g k) -> p (t g) k", k=4)
        m1, m2, gs = self.r_m1, self.r_m2, self.r_gs
        P.op('dve', lambda e: e.tensor_reduce(m1[:], g4(bi), AX.X, ALU.max), ['r_bi'], ['r_m1'])
        self.tt('dve', g4(t1), g4(bi), m1[:].to_broadcast([128, 64, 4]), ALU.is_equal, ['r_bi', 'r_m1'], ['r_t1'])
        self.stt(g4(t2), g4(t1), NEG, g4(bi), ALU.mult, ALU.add, ['r_t1', 'r_bi'], ['r_t2'])
        P.op('dve', lambda e: e.tensor_reduce(m2[:], g4(t2), AX.X, ALU.max), ['r_t2'], ['r_m2'])
        self.tt('dve', g4(t3), g4(t2), m2[:].to_broadcast([128, 64, 4]), ALU.is_equal, ['r_t2', 'r_m2'], ['r_t3'])
        self.tt('dve', gs[:], m1[:], m2[:], ALU.add, ['r_m1', 'r_m2'], ['r_gs'])
        gsv = gs[:].rearrange("p (t g) o -> p t (g o)", g=4)
        P.op('dve', lambda e: e.tensor_reduce(self.r_gm[:], gsv, AX.X, ALU.max), ['r_gs'], ['r_gm'])
        self.tt('dve', gsv, gsv, self.r_gm[:].to_broadcast([128, NT, 4]), ALU.is_equal, ['r_gs', 'r_gm'], ['r_gs'])
        self.tt('dve', g4(t1), g4(t1), gs[:].to_broadcast([128, 64, 4]), ALU.mult, ['r_t1', 'r_gs'], ['r_t1'])
        self.tt('dve', g4(t3), g4(t3), gs[:].to_broadcast([128, 64, 4]), ALU.mult, ['r_t3', 'r_gs'], ['r_t3'])
        wk = self.wk
        for k, sel in enumerate((t1, t3)):
            self.tt('dve', t2[:], sel[:], sc[:], ALU.mult, ['r_t1', 'r_t3', 'r_sc'], ['r_t2'])
            P.op('dve', lambda e, k=k: e.tensor_reduce(wk[:, :, k:k + 1], t2[:], AX.X, ALU.add), ['r_t2'], ['wk'])
        P.op('dve', lambda e: e.tensor_reduce(self.r_gm[:], wk[:], AX.X, ALU.add), ['wk'], ['r_gm'])
        P.op('dve', lambda e: e.reciprocal(self.r_gm[:], self.r_gm[:]), ['r_gm'], ['r_gm'])
        self.tt('dve', wk[:], wk[:], self.r_gm[:].to_broadcast([128, NT, 2]), ALU.mult, ['wk', 'r_gm'], ['wk'])
        sel = t2
        self.tt('dve', sel[:], t1[:], t3[:], ALU.add, ['r_t1', 'r_t3'], ['r_t2'])
        sel2d = sel[:].rearrange("p t e -> p (t e)")
        wi, to, cx = self.r_wi, self.r_to, self.r_cx
        self.tt('dve', self.Lst[:], self.Uf, self.ident_f, ALU.subtract, ['cst'], ['Lst'])
        self.mm(self.ps[5][:, 0:256], self.Lst[:], sel2d, True, True, ['Lst', 'r_t2'], ['ps5'])
        self.mm(self.ps[6][:, 0:256], self.ones_f, sel2d, True, True, ['cst', 'r_t2'], ['ps6'])
        self.copy('dve', wi[:].rearrange("p t e -> p (t e)"), self.ps[5][:, 0:256], ['ps5'], ['r_wi'])
        self.copy('dve', to[:].rearrange("p t e -> p (t e)"), self.ps[6][:, 0:256], ['ps6'], ['r_to'])
        P.op('dve', lambda e: e.memset(cx[:, 0, :], 0.0), [], ['r_cx'])
        for tt in range(1, NT):
            self.tt('dve', cx[:, tt, :], cx[:, tt - 1, :], to[:, tt - 1, :], ALU.add, ['r_cx', 'r_to'], ['r_cx'])
        cnt, nbk, pend = self.r_cnt, self.r_nbk, self.r_pend
        self.tt('dve', cnt[:], cx[:, NT - 1, :], to[:, NT - 1, :], ALU.add, ['r_cx', 'r_to'], ['r_cnt'])
        self.ts('dve', nbk[:], cnt[:], 0.0, None, ALU.is_gt, None, ['r_cnt'], ['r_nbk'])
        for k in range(1, -(-S // BS)):
            self.stt(nbk[:], cnt[:], float(BS * k), nbk[:], ALU.is_gt, ALU.add, ['r_cnt', 'r_nbk'], ['r_nbk'])
        self.copy('dve', pend[:, 0:1], nbk[:, 0:1], ['r_nbk'], ['r_pend'])
        for e_ in range(1, 16):
            self.tt('dve', pend[:, e_:e_ + 1], pend[:, e_ - 1:e_], nbk[:, e_:e_ + 1], ALU.add, ['r_pend', 'r_nbk'], ['r_pend'])
        self.tt('dve', cnt[:], pend[:], nbk[:], ALU.subtract, ['r_pend', 'r_nbk'], ['r_cnt'])
        self.ts('dve', cnt[:], cnt[:], float(BS), None, ALU.mult, None, ['r_cnt'], ['r_cnt'])
        self.tt('dve', wi[:], wi[:], cx[:], ALU.add, ['r_wi', 'r_cx'], ['r_wi'])
        self.tt('dve', wi[:], wi[:], cnt[:].unsqueeze(1).to_broadcast([128, NT, 16]), ALU.add, ['r_wi', 'r_cnt'], ['r_wi'])
        posf = self.r_posf
        for k, sl in enumerate((t1, t3)):
            self.tt('dve', sl[:], sl[:], wi[:], ALU.mult, ['r_t1', 'r_t3', 'r_wi'], ['r_t1', 'r_t3'])
            P.op('dve', lambda e, k=k, sl=sl: e.tensor_reduce(posf[:, :, k:k + 1], sl[:], AX.X, ALU.add), ['r_t1', 'r_t3'], ['r_posf'])
        self.copy('dve', self.pos_i[:], posf[:], ['r_posf'], ['pos_i'])
        bg = self.bgrid
        self.tt('dve', bg[:, :, :], pend[:].unsqueeze(1).to_broadcast([128, NB, 16]), self.c2[:, 0:NB * 16].rearrange("p (b e) -> p b e", e=16),
                ALU.is_le, ['r_pend', 'c2'], ['bgrid'])
        P.op('dve', lambda e: e.tensor_reduce(self.r_be[:], bg[:], AX.X, ALU.add), ['bgrid'], ['r_be'])
        self.ts('dve', self.r_be2[:], self.r_be[:], 16.0, 1.0e6, ALU.is_ge, ALU.mult, ['r_be'], ['r_be2'])
        self.ts('dve', self.r_be[:], self.r_be[:], 128.0, self.c2[:, NB * 16:NB * 16 + 1], ALU.mult, ALU.add, ['r_be', 'c2'], ['r_be'])
        self.tt('dve', self.r_be[:], self.r_be[:], self.r_be2[:], ALU.add, ['r_be', 'r_be2'], ['r_be'])
        for hlf in range(2):
            self.ts('dve', self.r_be2[:], self.r_be[:], float((li * 2 + hlf) * 2048), None, ALU.add, None, ['r_be'], ['r_be2'])
            self.copy('dve', self.widx_i[:, hlf, :], self.r_be2[:].rearrange("p b o -> p (b o)"), ['r_be2'], ['widx_i'])
        for tt in range(NT):
            xb = self.xbf[tt % 2]
            xbk = 'xbf%d' % (tt % 2)
            self.copy('act', xb[:], self.X[:, tt, :], ['X%d' % tt], [xbk])
            for k in range(2):
                P.idma(self.xsorted, xb[:, :], self.pos_i[:, tt, k:k + 1], True, reads=[xbk, 'pos_i', 'xsorted'],
                       writes=['xsorted%d' % (tt % 2)], sem='xsc%d' % (tt % 2))
            self.P.op('act', lambda e, tt=tt: e.mul(self.X[:, tt, :], self.X[:, tt, :], ALPHA), ['X%d' % tt], ['X%d' % tt])

    def moe_experts(self, li):
        P = self.P
        wsrc = (self.wg, self.wu, self.wd)
        nb = NB if self.stage != 'moe_small' else 2
        def st_W(b):
            s = b % 2
            for m, (dst, key) in enumerate(((self.wgb[s], 'wg%d' % s), (self.wub[s], 'wu%d' % s), (self.wdb[s], 'wd%d' % s))):
                for hlf in range(2):
                    if m < 2:
                        dv = dst[:, hlf * 4:(hlf + 1) * 4, :].rearrange("p c n -> p (c n)")
                    else:
                        dv = dst[:, hlf * 2:(hlf + 1) * 2, :].rearrange("p c n -> p (c n)")
                    P.idma(dv, wsrc[m], self.widx_i[:, hlf, b:b + 1], False, reads=['widx_i'], writes=[key], sem='s' + key,
                           bounds=(4 * 2048 - 1) if b >= 2 else None)

        def st_X(b):
            s = b % 2
            xblk = self.xT[:, QT * s:QT * s + QT, 1024:2048]
            xTb = self.xT[:, :, s * BS:(s + 1) * BS]
            P.dma('sp', xblk, self.xsorted[b * BS:(b + 1) * BS, :].rearrange("(q p) n -> p q n", p=128), reads=['xsorted0', 'xsorted1'],
                  writes=['xblk%d' % s], sem='xblk%d' % s)
            for q in range(QT):
                bank = 6 + (q % 2)
                for c in range(8):
                    self.tr(self.psb[bank][:, c * 128:(c + 1) * 128], xblk[:, q, c * 128:(c + 1) * 128], self.ident_bf[:],
                            ['xblk%d' % s, 'ident_bf'], ['ps%d' % bank])
                self.copy('act' if q % 2 == 0 else 'dve', xTb[:, :, q * 128:(q + 1) * 128],
                          self.psb[bank][:, 0:1024].rearrange("p (c n) -> p c n", n=128), ['ps%d' % bank], ['xTb%d' % s])

        def st_C(b):
            s = b % 2
            wgb, wub, wdb = self.wgb[s], self.wub[s], self.wdb[s]
            xTb = self.xT[:, :, s * BS:(s + 1) * BS]
            hT = self.hT[s]
            hk = 'hT%d' % s
            for efc in range(4):
                bg = 0 + (efc % 2)
                bu = 2 + (efc % 2)
                for c in range(8):
                    self.mm(self.ps[bg][:, 0:BS], wgb[:, c, efc * 128:(efc + 1) * 128], xTb[:, c, :],
                            c == 0, c == 7, ['wg%d' % s, 'xTb%d' % s], ['ps%d' % bg])
                for c in range(8):
                    self.mm(self.ps[bu][:, 0:BS], wub[:, c, efc * 128:(efc + 1) * 128], xTb[:, c, :],
                            c == 0, c == 7, ['wu%d' % s, 'xTb%d' % s], ['ps%d' % bu])
                sg = self.sg[efc % 2]
                sgk = 'sg%d' % (efc % 2)
                self.act(sg[:, 0:BS], self.ps[bg][:, 0:BS], AF.Silu, ['ps%d' % bg], [sgk])
                self.tt('dve', hT[:, efc, 0:BS], sg[:, 0:BS], self.ps[bu][:, 0:BS], ALU.mult, [sgk, 'ps%d' % bu], [hk])
            for q in range(QT):
                ysb = self.ysb[q]
                yk = 'ysb%d' % q
                for hlf in range(2):
                    by = 4 + (q % 2) * 2 + hlf
                    for kc in range(4):
                        self.mm(self.ps[by][:, :], hT[:, kc, q * 128:(q + 1) * 128], wdb[:, kc, hlf * 512:(hlf + 1) * 512],
                                kc == 0, kc == 3, [hk, 'wd%d' % s], ['ps%d' % by])
                    self.copy('act' if hlf == 0 else 'dve', ysb[:, hlf * 512:(hlf + 1) * 512], self.ps[by][:, :], ['ps%d' % by], [yk])
                r0 = b * BS + q * 128
                P.dma('act', self.ysorted[r0:r0 + 128, :], ysb, reads=[yk, 'ysorted'], writes=['ysorted%d' % q], sem='yst%d' % q)

        st_X(0)
        st_W(0)
        for b in range(nb):
            if b + 1 < nb:
                st_X(b + 1)
                st_W(b + 1)
            st_C(b)

    def combine_tile(self, tt, yks):
        P = self.P
        for k in range(2):
            j = (tt % 4) * 2 + k
            yb = yks[j]
            ykk = 'yk%d' % j
            P.idma(yb[:, :], self.ysorted, self.pos_i[:, tt, k:k + 1], False,
                   reads=['ysorted0', 'ysorted1', 'ysorted2', 'ysorted3', 'pos_i'], writes=[ykk], sem=ykk)
            self.stt(self.X[:, tt, :], yb[:], self.wk[:, tt, k:k + 1], self.X[:, tt, :], ALU.mult, ALU.add,
                     [ykk, 'wk', 'X%d' % tt], ['X%d' % tt])

    def ln2(self, li, yks):
        P = self.P
        P.dma('sp', self.lnp_sb[:], self.lnp[2 + 2 * li], writes=['lnp'], sem='lnp')

        def post(tt):
            if li == 0:
                xb = self.xbf[tt % 2]
                xbk = 'xbf%d' % (tt % 2)
                self.copy('act', xb[:], self.X[:, tt, :], ['X%d' % tt], [xbk])
                P.dma('sp', self.xs[tt * 128:(tt + 1) * 128, :], self.X[:, tt, :], reads=['X%d' % tt], writes=['xs%d' % tt],
                      sem='xs_st%d' % (tt % 2))
                self.transpose_tile(xb[:], xbk, self.xT, 'xT', tt, 6 + (tt % 2))
            else:
                P.dma('sp', self.y[tt * 128:(tt + 1) * 128, :], self.X[:, tt, :], reads=['X%d' % tt], sem='y_st%d' % (tt % 2),
                      out=True)
        self.ln_multi(NT, lambda tt: (self.X[:, tt, :], self.X[:, tt, :], 'X%d' % tt, 'X%d' % tt), 'lnp',
                      pre=lambda tt: self.combine_tile(tt, yks), post=post, bias_eng='dve')

    def build(self):
        nc = self.nc
        with ExitStack() as st:
            self.P = P = Prog(nc, st)
            sb = lambda n, s, d: self.sb(st, n, s, d)
            self.ps = [st.enter_context(nc.psum_tensor("ps%d" % i, [128, 512], F32)) for i in range(8)]
            self.psb = [p[:].bitcast(BF16) for p in self.ps]
            cst = sb("cst", [128, 6, 128], F32)
            self.ident_f = cst[:, 0, :]
            self.Uf, self.Ub, self.mnf, self.mnb, self.ones_f = (cst[:, i, :] for i in range(1, 6))
            self.ident_bf = sb("ident_bf", [128, 128], BF16)
            self.eps_ap = sb("eps", [128, 1], F32)
            self.ln_st = sb("ln_st", [128, 4 * 12], F32)
            self.ln_mv = sb("ln_mv", [128, 4 * 8], F32)
            self.lnp_sb = sb("lnp_sb", [128, 2 * D], F32)
            self.rden = sb("rden", [128, 1], F32)
            self.xT = sb("xT", [128, 8, S], BF16)
            self.mkT = sb("mkT", [64, 4, 256], BF16)
            self.mv = sb("mv", [128, 2, 4, 65], BF16)
            P.dma('sp', cst[:], self.consts.rearrange("p (k n) -> p k n", n=128), writes=['cst'], sem='cst')
            self.copy('dve', self.ident_bf[:], cst[:, 0, :], ['cst'], ['ident_bf'])
            P.op('dve', lambda e: e.memset(self.eps_ap[:], LN_EPS), [], ['eps'])
            self.last_w_alias()
            self.mem_kv()
            self.layer0_mixer()
            P.barrier()
            if self.post(0):
                return
            self.layer1_mixer()
            P.barrier()
            self.post(1)
            P.finish()

    def post(self, li):
        P = self.P
        nc = self.nc
        with ExitStack() as ph:
            sb = lambda n, s_, d: self.sb(ph, n, s_, d)
            self.X = sb("X", [128, NT, D], F32)
            self.xbf = [sb("xbf%d" % i, [128, D], BF16) for i in range(2)]
            self.wk = sb("wk", [128, NT, 2], F32)
            self.pos_i = sb("pos_i", [128, NT, 2], I32)
            self.widx_i = sb("widx_i", [128, 2, NB], I32)
            with ExitStack() as pa:
                sb = lambda n, s_, d: self.sb(pa, n, s_, d)
                self.wo = sb("wo", [128, 8, D], BF16)
                self.xres = [sb("xres%d" % i, [128, D], F32) for i in range(2)]
                self.rw_sb = sb("rw_sb", [128, 8, 16], F32)
                self.rb_sb = sb("rb_sb", [128, 16], F32)
                self.xtf = [sb("xtf%d" % i, [128, 8, 128], F32) for i in range(2)]
                self.lg = sb("lg", [128, NT, 16], F32)
                for n in ['r_sc', 'r_bi', 'r_t1', 'r_t2', 'r_t3']:
                    setattr(self, n, sb(n, [128, NT, 16], F32))
                for n in ['r_m1', 'r_m2', 'r_gs']:
                    setattr(self, n, sb(n, [128, NT * 4, 1], F32))
                self.r_gm = sb("r_gm", [128, NT, 1], F32)
                for n in ['r_wi', 'r_to', 'r_cx']:
                    setattr(self, n, sb(n, [128, NT, 16], F32))
                for n in ['r_cnt', 'r_nbk', 'r_pend']:
                    setattr(self, n, sb(n, [128, 16], F32))
                self.r_posf = sb("r_posf", [128, NT, 2], F32)
                self.Lst = sb("Lst", [128, 128], F32)
                self.bgrid = sb("bgrid", [128, NB, 16], F32)
                self.r_be = sb("r_be", [128, NB, 1], F32)
                self.r_be2 = sb("r_be2", [128, NB, 1], F32)
                self.c2 = sb("c2", [128, NB * 16 + 1], F32)
                P.dma('sp', self.c2[:], self.consts2, writes=['c2'], sem='c2')
                self.out_proj_ln(li, self.x if li == 0 else self.xs)
                if self.stage == 'l0_ln1' or (self.stage in ('l1_ln1', 'ml_small') and li == 1):
                    self.dump_X()
                    return True
                self.moe_route(li)
                if self.stage in ('l0_route',):
                    self.dump_X()
                    return True
                P.barrier()
            with ExitStack() as pb:
                sb = lambda n, s_, d: self.sb(pb, n, s_, d)
                self.wgb = [sb("wgb%d" % i, [128, 8, 512], BF16) for i in range(2)]
                self.wub = [sb("wub%d" % i, [128, 8, 512], BF16) for i in range(2)]
                self.wdb = [sb("wdb%d" % i, [128, 4, 1024], BF16) for i in range(2)]
                self.hT = [sb("hT%d" % i, [128, 4, 512], BF16) for i in range(2)]
                self.sg = [sb("sg%d" % i, [128, 512], F32) for i in range(2)]
                self.ysb = [sb("ysb%d" % i, [128, D], F32)[:] for i in range(4)]
                self.moe_experts(li)
                P.barrier()
            with ExitStack() as pc:
                yks = [self.sb(pc, "yk%d" % i, [128, D], F32) for i in range(8)]
                if self.stage in ('l0', 'moe_small'):
                    for tt in range(NT):
                        self.combine_tile(tt, yks)
                    self.dump_X()
                    return True
                self.ln2(li, yks)
                P.barrier()
        return False

    def last_w_alias(self):
        for k in ['ident_f']:
            self.P.last_w[k] = self.P.last_w['cst']

    def dump_X(self):
        P = self.P
        for tt in range(NT):
            P.dma('sp', self.y[tt * 128:(tt + 1) * 128, :], self.X[:, tt, :], reads=['X%d' % tt], sem='y_st%d' % (tt % 2), out=True)
        P.finish()

    def mem_kv(self):
        P = self.P
        with ExitStack() as ph:
            memsb = self.sb(ph, "memsb", [128, 2, D], F32)
            membf = self.sb(ph, "membf", [128, 2, D], BF16)
            memT = self.sb(ph, "memT", [128, 8, 256], BF16)
            wkv = self.sb(ph, "wkv", [128, 8, 512], BF16)
            zt = self.sb(ph, "zt", [128, 8, D], BF16)
            P.op('pool', lambda e: e.memset(zt[:], 0.0), [], ['zt'])
            nrow = NB * BS
            for r0 in range(0, nrow, 1024):
                nq = min(1024, nrow - r0) // 128
                P.dma('act', self.xsorted[r0:r0 + nq * 128, :].rearrange("(q p) n -> p q n", p=128), zt[:, 0:nq, :], reads=['zt'],
                      writes=['xsorted'], sem='xsz')
            P.dma('sp', self.lnp_sb[:], self.lnp[0], writes=['lnp'], sem='lnp')
            for mt in range(2):
                P.dma('sp', memsb[:, mt, :], self.mem[mt * 128:(mt + 1) * 128, :], writes=['memsb%d' % mt], sem='memsb%d' % mt)
                for f in self.ln_stages(memsb[:, mt, :], membf[:, mt, :], 'memsb%d' % mt, 'membf%d' % mt, 'lnp', mt):
                    f()
                self.transpose_tile(membf[:, mt, :], 'membf%d' % mt, memT, 'memT', mt, 6 + mt)
            for hlf in range(2):
                self.load_cast(wkv[:, hlf * 4:(hlf + 1) * 4, :],
                               self.wmkv[hlf * 512:(hlf + 1) * 512, :].rearrange("(c p) n -> p c n", p=128), 'wkv', 128, (4, 512))
            for h in range(4):
                bank = h % 2
                for c in range(8):
                    self.mm(self.ps[bank][0:64, 0:256], wkv[:, c, h * 64:(h + 1) * 64], memT[:, c, :], c == 0, c == 7,
                            ['wkv', 'memT0', 'memT1'], ['ps%d' % bank])
                self.copy('act', self.mkT[:, h, :], self.ps[bank][0:64, 0:256], ['ps%d' % bank], ['mkT'])
            P.op('pool', lambda e: e.memset(self.mv[:, :, :, 64:65], 1.0), [], ['mv'])
            for mt in range(2):
                bank = 2 + mt
                for c in range(8):
                    self.mm(self.ps[bank][:, 0:256], memT[:, c, mt * 128:(mt + 1) * 128], wkv[:, c, 256:512], c == 0, c == 7,
                            ['wkv', 'memT%d' % mt], ['ps%d' % bank])
                self.copy('dve', self.mv[:, mt, :, 0:64], self.ps[bank][:, 0:256].rearrange("p (h d) -> p h d", d=64),
                          ['ps%d' % bank], ['mv'])
            P.barrier()

    def layer0_mixer(self):
        P = self.P
        with ExitStack() as ph:
            sb = lambda n, s_, d: self.sb(ph, n, s_, d)
            self.mixed = sb("mixed", [128, NT, D], BF16)
            vaug = sb("vaug", [128, NT, 12, 65], BF16)
            wqk = sb("wqk", [128, 8, 1536], BF16)
            qT = [sb("qT%d" % i, [128, S], BF16) for i in range(2)]
            kT = [sb("kT%d" % i, [128, S], BF16) for i in range(2)]
            s_sb = [sb("s_sb%d" % i, [128, 640], F32) for i in range(2)]
            p_bf = [sb("p_bf%d" % i, [128, 640], BF16) for i in range(2)]
            with ExitStack() as t1:
                sb1 = lambda n, s_, d: self.sb(t1, n, s_, d)
                xin = [sb1("xin%d" % i, [128, D], F32) for i in range(4)]
                xinb = [sb1("xinb%d" % i, [128, D], BF16) for i in range(4)]
                wv = sb1("wv", [128, 8, 768], BF16)
                for tt in range(NT):
                    i = tt % 4
                    P.dma('sp', xin[i][:], self.x[tt * 128:(tt + 1) * 128, :], writes=['xin%d' % i], sem='xin%d' % i)
                    self.copy('act' if tt % 2 == 0 else 'dve', xinb[i][:], xin[i][:], ['xin%d' % i], ['xinb%d' % i])
                    self.transpose_tile(xinb[i][:], 'xinb%d' % i, self.xT, 'xT', tt, 4 + i)
                for blk in range(3):
                    for hlf in range(2):
                        self.load_cast(wqk[:, hlf * 4:(hlf + 1) * 4, blk * 512:(blk + 1) * 512],
                                       self.na_w[hlf * 512:(hlf + 1) * 512, blk * 512:(blk + 1) * 512].rearrange("(c p) n -> p c n", p=128),
                                       'wqk', 128, (4, 512))
                for c0, n in ((0, 512), (512, 256)):
                    for hlf in range(2):
                        self.load_cast(wv[:, hlf * 4:(hlf + 1) * 4, c0:c0 + n],
                                       self.na_w[hlf * 512:(hlf + 1) * 512, 1536 + c0:1536 + c0 + n].rearrange("(c p) n -> p c n", p=128),
                                       'wv', 128, (4, n))
                P.op('pool', lambda e: e.memset(vaug[:, :, :, 64:65], 1.0), [], ['vaug'])
                for tt in range(NT):
                    for bi, (c0, n) in enumerate(((0, 512), (512, 256))):
                        bank = bi + 2 * (tt % 2)
                        for c in range(8):
                            self.mm(self.ps[bank][:, 0:n], self.xT[:, c, tt * 128:(tt + 1) * 128], wv[:, c, c0:c0 + n], c == 0, c == 7,
                                    ['xT%d' % tt, 'wv'], ['ps%d' % bank])
                        h0 = c0 // 64
                        self.copy('act' if bi == 0 else 'dve', vaug[:, tt, h0:h0 + n // 64, 0:64],
                                  self.ps[bank][:, 0:n].rearrange("p (h d) -> p h d", d=64), ['ps%d' % bank], ['vaug'])
                P.barrier()
            self.mem_attention(self.na_w[:, 2304:2560])
            with ExitStack() as t3:
                bias = [self.sb(t3, "bias%d" % i, [128, 21, 128], BF16) for i in range(2)]
                rdn = [self.sb(t3, "rdn%d" % i, [128, 1], F32) for i in range(2)]
                nh = 12 if self.stage != 'na_small' else 1
                for h in range(nh):
                    hb = h % 2
                    P.dma('pool', bias[hb][:], self.nab[h].rearrange("p (k n) -> p k n", n=128), writes=['bias%d' % hb], sem='bias%d' % hb)
                    pp = (h // 2) % 2
                    psl = slice(64 * (h % 2), 64 * (h % 2) + 64)
                    if h % 2 == 0:
                        for which, dst, col0, scale in (('q', qT[pp], h * 64, 0.125), ('k', kT[pp], 768 + h * 64, 1.0)):
                            for tb in range(4):
                                bank = 6 + (tb % 2)
                                for c in range(8):
                                    self.mm(self.ps[bank][:, :], wqk[:, c, col0:col0 + 128], self.xT[:, c, tb * 512:(tb + 1) * 512],
                                            c == 0, c == 7, ['wqk'] + self.xkb(tb), ['ps%d' % bank])
                                self.act(dst[:, tb * 512:(tb + 1) * 512], self.ps[bank][:, :], AF.Copy, ['ps%d' % bank],
                                         ['%sT%d' % (which, pp)], scale=scale)
                    def st_S(i):
                        kts, slot = _na_key_tiles(i)
                        pb = i % 2
                        ba, bb = 2 * pb, 2 * pb + 1
                        nk = len(kts)
                        n4 = min(nk, 4)
                        self.mm(self.ps[ba][:, 0:n4 * 128], self.ident_bf[:], bias[hb][:, slot:slot + n4, :].rearrange("p k n -> p (k n)"),
                                True, False, ['ident_bf', 'bias%d' % hb], ['ps%d' % ba])
                        if nk == 5:
                            self.mm(self.ps[bb][:, 0:128], self.ident_bf[:], bias[hb][:, slot + 4, :],
                                    True, False, ['ident_bf', 'bias%d' % hb], ['ps%d' % bb])
                        for j, kt in enumerate(kts):
                            bank, off = (ba, j * 128) if j < 4 else (bb, 0)
                            self.mm(self.ps[bank][:, off:off + 128], kT[pp][psl, kt * 128:(kt + 1) * 128], qT[pp][psl, i * 128:(i + 1) * 128],
                                    False, (j == n4 - 1) or (j == 4), ['kT%d' % pp, 'qT%d' % pp], ['ps%d' % bank])

                    def st_mid(i):
                        kts, slot = _na_key_tiles(i)
                        nk = len(kts)
                        pb = i % 2
                        ba, bb = 2 * pb, 2 * pb + 1
                        n4 = min(nk, 4)
                        self.act(p_bf[pb][:, 0:n4 * 128], self.ps[ba][:, 0:n4 * 128], AF.Exp, ['ps%d' % ba], ['p_bf%d' % pb])
                        if nk == 5:
                            self.act(p_bf[pb][:, 512:640], self.ps[bb][:, 0:128], AF.Exp, ['ps%d' % bb], ['p_bf%d' % pb])

                    def st_PV(i):
                        kts, slot = _na_key_tiles(i)
                        nk = len(kts)
                        pb = i % 2
                        bo = 4 + pb
                        for j, kt in enumerate(kts):
                            self.mm(self.ps[bo][:, 0:65], p_bf[pb][:, j * 128:(j + 1) * 128], vaug[:, kt, h, :], j == 0, j == nk - 1,
                                    ['p_bf%d' % pb, 'vaug'], ['ps%d' % bo])
                        rd = rdn[pb]
                        P.op('dve', lambda e, bo=bo, rd=rd: e.reciprocal(rd[:, 0:1], self.ps[bo][:, 64:65]), ['ps%d' % bo], ['rdn%d' % pb])
                        self.ts('dve', self.mixed[:, i, h * 64:(h + 1) * 64], self.ps[bo][:, 0:64], rd[:, 0:1], None,
                                ALU.mult, None, ['ps%d' % bo, 'rdn%d' % pb], ['mixed'])

                    st_S(0)
                    st_S(1)
                    st_mid(0)
                    for i in range(NT):
                        if i + 2 < NT:
                            st_S(i + 2)
                        if i + 1 < NT:
                            st_mid(i + 1)
                        st_PV(i)
                P.barrier()
            self.mixed_to_xT()
            P.barrier()

    def mixed_to_xT(self):
        for tt in range(NT):
            self.transpose_tile(self.mixed[:, tt, :], 'mixed', self.xT, 'xT', tt, 6 + (tt % 2))

    def layer1_mixer(self):
        P = self.P
        KS = 192 ** -0.5
        CH = ((0, 128), (128, 64))
        with ExitStack() as ph:
            sb = lambda n, s_, d: self.sb(ph, n, s_, d)
            self.mixed = sb("mixed", [128, NT, D], BF16)
            gts = sb("gts", [128, NT, 16], F32)
            lf, ig, bcol, colb, eb, blast, wexp, ebl, colf = (sb(n, [128, NT, 2, 4], F32) for n in
                                                               ("lf", "ig", "bcol", "colb", "eb", "blast", "wexp", "ebl", "colf"))
            wqkv = sb("wqkv", [128, 4, 3, 2, 256], BF16)
            cwb = sb("cwb", [128, 4, 2, 8], F32)
            ngs = sb("ngs", [128, 2, 768], F32)
            gbs = sb("gbs", [128, 16], F32)
            wgt = sb("wgt", [128, 8, 16], BF16)
            st6 = sb("hst6", [128, 6], F32)
            hmv = sb("hmv", [128, 4], F32)
            P.dma('sp', cwb[:], self.cwb.rearrange("p (h c k) -> p h c k", h=4, c=2), writes=['cwb'], sem='cwb')
            P.dma('sp', ngs[:], self.ngs.rearrange("p (a n) -> p a n", a=2), writes=['ngs'], sem='ngs')
            P.dma('sp', gbs[:], self.gb, writes=['gbs'], sem='gbs')
            P.op('pool', lambda e: e.memset(wqkv[:], 0.0), [], ['wqkv'])
            for a in range(3):
                for kc, (o, n) in enumerate(CH):
                    self.load_cast(wqkv[0:n, :, a, kc, 0:192], self.wqkv[a][:, o:o + n, :].rearrange("h p n -> p h n"),
                                   'wqkv', n, (4, 192))
            self.load_cast(wgt[:], self.ml_w[:, 1536:1552].rearrange("(c p) n -> p c n", p=128), 'wgt', 128, (8, 16))
            for tt in range(NT):
                bank = tt % 2
                for c in range(8):
                    self.mm(self.ps[bank][:, 0:16], self.xT[:, c, tt * 128:(tt + 1) * 128], wgt[:, c, :], c == 0, c == 7,
                            ['xT%d' % tt, 'wgt'], ['ps%d' % bank])
                self.tt('dve', gts[:, tt, :], self.ps[bank][:, 0:16], gbs[:], ALU.add, ['ps%d' % bank, 'gbs'], ['gts'])
            gv = gts[:].rearrange("p t (g h) -> p t g h", h=4)
            for d in range(2):
                self.copy('pool', ig[:, :, d, :], gv[:, :, 2 * d, :], ['gts'], ['ig'])
                self.act(lf[:, :, d, :], gv[:, :, 2 * d + 1, :], AF.Exp, ['gts'], ['lf'], scale=-1.0)
            self.act(lf[:], lf[:], AF.Ln, ['lf'], ['lf'], bias=1.0)
            self.ts('dve', lf[:], lf[:], -1.0, None, ALU.mult, None, ['lf'], ['lf'])
            for tt in range(NT):
                bank = 2 + (tt % 2)
                self.mm(self.ps[bank][:, 0:4], self.Uf, lf[:, tt, 0, :], True, True, ['lf', 'cst'], ['ps%d' % bank])
                self.mm(self.ps[bank][:, 4:8], self.Ub, lf[:, tt, 1, :], True, True, ['lf', 'cst'], ['ps%d' % bank])
                self.mm(self.ps[bank][:, 8:16], self.ones_f, lf[:, tt, :, :].rearrange("p d h -> p (d h)"), True, True,
                        ['lf', 'cst'], ['ps%d' % bank])
                self.copy('dve', bcol[:, tt, :, :].rearrange("p d h -> p (d h)"), self.ps[bank][:, 0:8], ['ps%d' % bank], ['bcol'])
                self.copy('act', blast[:, tt, :, :].rearrange("p d h -> p (d h)"), self.ps[bank][:, 8:16], ['ps%d' % bank], ['blast'])
            self.tt('dve', colb[:], ig[:], bcol[:], ALU.subtract, ['ig', 'bcol'], ['colb'])
            self.act(eb[:], bcol[:], AF.Exp, ['bcol'], ['eb'])
            self.act(colf[:], colb[:], AF.Exp, ['colb'], ['colf'])
            self.tt('dve', wexp[:], blast[:], colb[:], ALU.add, ['blast', 'colb'], ['wexp'])
            self.act(wexp[:], wexp[:], AF.Exp, ['wexp'], ['wexp'])
            self.act(ebl[:], blast[:], AF.Exp, ['blast'], ['ebl'])
            self.mem_attention(self.ml_w[:, 1552:1808])
            nh = 4 if self.stage != 'ml_small' else 1
            wxzs = [sb("wxz%d" % i, [128, 8, 2, 256], BF16) for i in range(2)]
            for i in range(2):
                P.op('pool', lambda e, i=i: e.memset(wxzs[i][:], 0.0), [], ['wxz%d' % i])

            def load_wxz(hh):
                for a, c0 in ((0, hh * 192), (1, 768 + hh * 192)):
                    self.load_cast(wxzs[hh % 2][:, :, a, 0:192], self.ml_w[:, c0:c0 + 192].rearrange("(c p) n -> p c n", p=128),
                                   'wxz%d' % (hh % 2), 128, (8, 192))
            load_wxz(0)
            for h in range(nh):
                with ExitStack() as hd:
                    sbh = lambda n, s_, d: self.sb(hd, n, s_, d)
                    qT = sbh("mqT", [128, 2, S], BF16)
                    kT = sbh("mkT_", [128, 2, S], BF16)
                    kw = sbh("kw", [128, NT, 2, 256], BF16)
                    P.op('pool', lambda e: e.memset(kw[:, :, :, 192:256], 0.0), [], ['kw'])
                    vaug = sbh("mvaug", [128, NT, 193], BF16)
                    xct = sbh("xct", [128, NT, 192], BF16)
                    zs = sbh("zs", [128, NT, 192], BF16)
                    with ExitStack() as t1:
                        sb1 = lambda n, s_, d: self.sb(t1, n, s_, d)
                        wxz = wxzs[h % 2]
                        wk = 'wxz%d' % (h % 2)
                        xmT = sb1("xmT", [128, 2, S + 4], BF16)
                        xcT = sb1("xcT", [128, 2, S], BF16)
                        dg = sb1("dg", [128, 2, 5, 128], BF16)
                        if h + 1 < nh:
                            load_wxz(h + 1)
                        P.op('pool', lambda e: e.memset(xmT[:, :, 0:2], 0.0), [], ['xmT'])
                        P.op('pool', lambda e: e.memset(xmT[:, :, S + 2:S + 4], 0.0), [], ['xmT'])
                        for ch, (o, n) in enumerate(CH):
                            for j in range(5):
                                self.ts('dve', dg[:, ch, j, :], self.ident_f, cwb[:, h, ch, j:j + 1], None, ALU.mult, None,
                                        ['cst', 'cwb'], ['dg'])
                        for ch, (o, n) in enumerate(CH):
                            for tb in range(4):
                                bank = 2 + (tb % 2)
                                for c in range(8):
                                    self.mm(self.ps[bank][:, :], wxz[:, c, 0, o:o + 128], self.xT[:, c, tb * 512:(tb + 1) * 512],
                                            c == 0, c == 7, [wk] + self.xkb(tb), ['ps%d' % bank])
                                self.copy('act' if tb % 2 == 0 else 'dve', xmT[:, ch, 2 + tb * 512:2 + (tb + 1) * 512], self.ps[bank][:, :],
                                          ['ps%d' % bank], ['xmT'])
                        for tt in range(NT):
                            bank = tt % 2
                            for c in range(8):
                                self.mm(self.ps[bank][:, 0:192], self.xT[:, c, tt * 128:(tt + 1) * 128], wxz[:, c, 1, 0:192], c == 0, c == 7,
                                        ['xT%d' % tt, wk], ['ps%d' % bank])
                            self.act(zs[:, tt, :], self.ps[bank][:, 0:192], AF.Silu, ['ps%d' % bank], ['zs'])
                        for ch, (o, n) in enumerate(CH):
                            for tb in range(4):
                                bank = 4 + (tb % 2)
                                for j in range(5):
                                    self.mm(self.ps[bank][:, :], dg[:, ch, j, :], xmT[:, ch, tb * 512 + j:tb * 512 + j + 512],
                                            j == 0, j == 4, ['dg', 'xmT'], ['ps%d' % bank])
                                self.act(xcT[:, ch, tb * 512:(tb + 1) * 512], self.ps[bank][:, :], AF.Silu, ['ps%d' % bank, 'cwb'], ['xcT'],
                                         bias=cwb[:, h, ch, 5:6])
                        P.op('pool', lambda e: e.memset(vaug[:, :, 192:193], 1.0), [], ['mvaug'])
                        for tt in range(NT):
                            bank = 4 + (tt % 2)
                            for kc, (o, n) in enumerate(CH):
                                self.mm(self.ps[bank][:, 0:192], xmT[:, kc, 2 + tt * 128:2 + (tt + 1) * 128], wqkv[:, h, 2, kc, 0:192], kc == 0, kc == 1,
                                        ['xmT', 'wqkv'], ['ps%d' % bank])
                            self.copy('act' if tt % 2 == 0 else 'dve', vaug[:, tt, 0:192], self.ps[bank][:, 0:192], ['ps%d' % bank], ['mvaug'])
                        for a_, dst, key, scale in ((0, qT, 'mqT', 1.0), (1, kT, 'mkT_', KS)):
                            for oc, (oo, on) in enumerate(CH):
                                for tb in range(4):
                                    bank = 6 + (tb % 2)
                                    for kc, (o, n) in enumerate(CH):
                                        self.mm(self.ps[bank][:, :], wqkv[:, h, a_, kc, oo:oo + 128], xcT[:, kc, tb * 512:(tb + 1) * 512],
                                                kc == 0, kc == 1, ['xcT', 'wqkv'], ['ps%d' % bank])
                                    self.act(dst[:, oc, tb * 512:(tb + 1) * 512], self.ps[bank][:, :], AF.Copy, ['ps%d' % bank], [key],
                                             scale=scale)
                        for tt in range(NT):
                            bank = tt % 2
                            for kc, (o, n) in enumerate(CH):
                                self.mm(self.ps[bank][:, 0:192], xcT[:, kc, tt * 128:(tt + 1) * 128], wqkv[:, h, 1, kc, 0:192], kc == 0, kc == 1,
                                        ['xcT', 'wqkv'], ['ps%d' % bank])
                            for d in range(2):
                                self.ts('dve', kw[:, tt, d, 0:192], self.ps[bank][:, 0:192], wexp[:, tt, d, h:h + 1], KS, ALU.mult, ALU.mult,
                                        ['ps%d' % bank, 'wexp'], ['kw'])
                        for tt in range(NT):
                            bank = 2 + (tt % 2)
                            for kc, (o, n) in enumerate(CH):
                                self.tr(self.psb[bank][:, o:o + 128], xcT[:, kc, tt * 128:(tt + 1) * 128], self.ident_bf[:],
                                        ['xcT', 'ident_bf'], ['ps%d' % bank])
                            self.copy('act' if tt % 2 == 0 else 'dve', xct[:, tt, :], self.psb[bank][:, 0:192], ['ps%d' % bank], ['xct'])
                        P.barrier()
                    with ExitStack() as t2:
                        sb2 = lambda n, s_, d: self.sb(t2, n, s_, d)
                        hraw = sb2("hraw", [128, 2, NT, 193], F32)
                        Pm = [[sb2("Pm%d%d" % (i, d), [128, 128], BF16) for d in range(2)] for i in range(2)]
                        C = sb2("Cst", [128, 2, 2, 193], F32)
                        Cbf = [sb2("Cbf%d" % i, [128, 2, 2, 193], BF16) for i in range(2)]
                        dn = sb2("dn", [128, 2 * NT, 1], F32)
                        sm = sb2("sm", [128, NT, 1], F32)
                        sq = sb2("sq", [128, NT, 1], F32)
                        msk = (self.Uf, self.Ub)

                        def tt_of(c, d):
                            return c if d == 0 else NT - 1 - c

                        def st_A(c):
                            cp = c % 2
                            for d in range(2):
                                tt = tt_of(c, d)
                                tsl = slice(tt * 128, (tt + 1) * 128)
                                self.mm(self.ps[cp][:, d * 128:(d + 1) * 128], kT[0:128, 0, tsl], qT[0:128, 0, tsl], True, False,
                                        ['mkT_', 'mqT'], ['ps%d' % cp])
                                self.mm(self.ps[cp][:, d * 128:(d + 1) * 128], kT[:, 1, tsl], qT[:, 1, tsl], False, True,
                                        ['mkT_', 'mqT'], ['ps%d' % cp])
                            if c < NT - 1:
                                for d in range(2):
                                    tt = tt_of(c, d)
                                    b2 = 2 + cp * 2 + d
                                    for kc, (o, n) in enumerate(CH):
                                        self.mm(self.ps[b2][:, kc * 256:kc * 256 + 193], kw[:, tt, d, o:o + 128], vaug[:, tt, :], True, True,
                                                ['kw', 'mvaug'], ['ps%d' % b2])
                            for d in range(2):
                                tt = tt_of(c, d)
                                self.stt(Pm[cp][d][:], self.ps[cp][:, d * 128:(d + 1) * 128], colf[:, tt, d, h:h + 1], msk[d],
                                         ALU.mult, ALU.mult, ['ps%d' % cp, 'colf', 'cst'], ['Pm%d%d' % (cp, d)])

                        def st_B(c):
                            cp = c % 2
                            bh = 6 + cp
                            for d in range(2):
                                tt = tt_of(c, d)
                                tsl = slice(tt * 128, (tt + 1) * 128)
                                o_ = self.ps[bh][:, d * 256:d * 256 + 193]
                                self.mm(o_, Pm[cp][d][:], vaug[:, tt, :], True, c == 0, ['Pm%d%d' % (cp, d), 'mvaug'], ['ps%d' % bh])
                                if c > 0:
                                    pk = 'Cbf%d%d' % (1 - cp, d)
                                    self.mm(o_, qT[0:128, 0, tsl], Cbf[1 - cp][0:128, d, 0, :], False, False, ['mqT', pk], ['ps%d' % bh])
                                    self.mm(o_, qT[:, 1, tsl], Cbf[1 - cp][:, d, 1, :], False, True, ['mqT', pk], ['ps%d' % bh])
                            if c < NT - 1:
                                for d in range(2):
                                    tt = tt_of(c, d)
                                    b2 = 2 + cp * 2 + d
                                    psv = self.ps[b2][:, 0:512].rearrange("p (k n) -> p k n", n=256)[:, :, 0:193]
                                    if c == 0:
                                        self.copy('dve', C[:, d, :, :], psv, ['ps%d' % b2], ['C%d' % d])
                                    else:
                                        self.stt(C[:, d, :, :], C[:, d, :, :], ebl[:, tt, d, h:h + 1], psv, ALU.mult, ALU.add,
                                                 ['C%d' % d, 'ebl', 'ps%d' % b2], ['C%d' % d])
                                    self.copy('act', Cbf[cp][:, d, :, :], C[:, d, :, :], ['C%d' % d], ['Cbf%d%d' % (cp, d)])
                            for d in range(2):
                                tt = tt_of(c, d)
                                self.act(hraw[:, d, tt, :], self.ps[bh][:, d * 256:d * 256 + 193], AF.Copy, ['ps%d' % bh, 'eb'], ['hraw%d_%d' % (d, tt)],
                                         scale=eb[:, tt, d, h:h + 1])

                        st_A(0)
                        for c in range(NT):
                            if c + 1 < NT:
                                st_A(c + 1)
                            st_B(c)
                        denv = hraw[:, :, :, 192:193].rearrange("p d t o -> p (d t) o")
                        allh = ['hraw%d_%d' % (d_, t_) for d_ in range(2) for t_ in range(NT)]
                        self.stt(dn[:], denv, -1.0, denv, ALU.mult, ALU.max, allh, ['dn', 'hraw'])
                        self.ts('dve', dn[:], dn[:], 1.0, None, ALU.max, None, ['dn'], ['dn'])
                        P.op('dve', lambda e: e.reciprocal(dn[:], dn[:]), ['dn'], ['dn'])
                        for d in range(2):
                            self.tt('dve', hraw[:, d, :, 0:192], hraw[:, d, :, 0:192],
                                    dn[:, d * NT:(d + 1) * NT, :].to_broadcast([128, NT, 192]), ALU.mult, ['hraw', 'dn'], ['hraw'])
                        h0 = hraw[:, 0, :, 0:192]
                        h1 = hraw[:, 1, :, 0:192]
                        self.tt('dve', h0, h0, h1, ALU.add, ['hraw'], ['hraw'])
                        P.op('dve', lambda e: e.tensor_reduce(sm[:], h0, AX.X, ALU.add), ['hraw'], ['sm'])
                        self.ts('dve', sm[:], sm[:], 1.0 / 192, None, ALU.mult, None, ['sm'], ['sm'])
                        self.tt('dve', h0, h0, sm[:].to_broadcast([128, NT, 192]), ALU.subtract, ['hraw', 'sm'], ['hraw'])
                        self.tt('dve', h1, h0, h0, ALU.mult, ['hraw'], ['hraw'])
                        P.op('dve', lambda e: e.tensor_reduce(sq[:], h1, AX.X, ALU.add), ['hraw'], ['sq'])
                        self.act(sq[:], sq[:], AF.Sqrt, ['sq'], ['sq'], bias=self.eps_ap[:, 0:1], scale=1.0 / 192)
                        P.op('dve', lambda e: e.reciprocal(sq[:], sq[:]), ['sq'], ['sq'])
                        self.tt('dve', h0, h0, sq[:].to_broadcast([128, NT, 192]), ALU.mult, ['hraw', 'sq'], ['hraw'])
                        gview = ngs[:, 0, h * 192:(h + 1) * 192].unsqueeze(1).to_broadcast([128, NT, 192])
                        sview = ngs[:, 1, h * 192:(h + 1) * 192].unsqueeze(1).to_broadcast([128, NT, 192])
                        self.tt('dve', h0, h0, gview, ALU.mult, ['hraw', 'ngs'], ['hraw'])
                        self.tt('dve', h1, xct[:], sview, ALU.mult, ['xct', 'ngs', 'hraw'], ['hraw'])
                        self.tt('dve', h0, h0, h1, ALU.add, ['hraw'], ['hraw'])
                        self.tt('dve', self.mixed[:, :, h * 192:(h + 1) * 192], h0, zs[:], ALU.mult, ['hraw', 'zs'], ['mixed'])
                        P.barrier()
            self.mixed_to_xT()
            P.barrier()


def _na_bias_index():
    idx = np.full((21, 128, 128), 465, dtype=np.int64)
    for i in [0, 1, 2, 14, 15]:
        kts, slot = _na_key_tiles(i)
        qi = np.arange(128)
        r = 2 * i + qi // 64
        c = qi % 64
        rs = np.clip(r - 4, 0, 24)
        cs = np.clip(c - 8, 0, 48)
        for j, kt in enumerate(kts):
            ki = np.arange(128)
            kr = (2 * kt + ki // 64)[:, None]
            kc = (ki % 64)[:, None]
            valid = (kr >= rs[None]) & (kr < rs[None] + 8) & (kc >= cs[None]) & (kc < cs[None] + 16)
            flat = (kr - r[None] + 7) * 31 + (kc - c[None] + 15)
            idx[slot + j] = np.where(valid, flat, 465)
    return idx


_CACHE = {}


def _host_inputs(inp):
    f = lambda a: np.ascontiguousarray(np.asarray(a, dtype=np.float32))
    rep = lambda v: np.ascontiguousarray(np.broadcast_to(np.asarray(v, np.float32).reshape(1, -1), (128, np.asarray(v).size)))
    ln_g, ln_b = f(inp['ln_g']), f(inp['ln_b'])
    lnp = np.stack([np.concatenate([rep(inp['mem_ln_g']), rep(inp['mem_ln_b'])], 1)] +
                   [np.concatenate([rep(ln_g[l, k]), rep(ln_b[l, k])], 1) for l in range(2) for k in range(2)], 0)
    rpb = f(inp['na_rpb'])[0].reshape(12, 465)
    rpb_ext = np.concatenate([rpb, np.full((12, 1), NEG, np.float32)], 1)
    idx = _na_bias_index()
    nab = rpb_ext[:, idx]
    nab = np.ascontiguousarray(nab.transpose(0, 2, 1, 3).reshape(12, 128, 21 * 128))
    cw = f(inp['ml_conv_w'])[0]
    cb = f(inp['ml_conv_b'])[0]
    cwb = np.zeros((128, 4, 2, 8), np.float32)
    for h in range(4):
        for ch, (o, n) in enumerate(((0, 128), (128, 64))):
            f0 = h * 192 + o
            cwb[:n, h, ch, 0:5] = cw[:, f0:f0 + n].T
            cwb[:n, h, ch, 5] = cb[f0:f0 + n]
    ii = np.arange(128)
    ident = np.eye(128, dtype=np.float32)
    Uf = (ii[:, None] <= ii[None, :]).astype(np.float32)
    Ub = (ii[:, None] >= ii[None, :]).astype(np.float32)
    mnf = np.where(ii[:, None] <= ii[None, :], 0.0, NEG).astype(np.float32)
    mnb = np.where(ii[:, None] >= ii[None, :], 0.0, NEG).astype(np.float32)
    consts = np.stack([ident, Uf, Ub, mnf, mnb, np.ones((128, 128), np.float32)], 1).reshape(128, 6 * 128)
    def relay(w, nchunk):
        w = f(w)
        L, E, K, N = w.shape
        w = w.reshape(L, E, 2, nchunk // 2, 128, N).transpose(0, 2, 1, 4, 3, 5)
        return np.ascontiguousarray(w.reshape(L * 2 * E * 128, (nchunk // 2) * N))
    consts2 = np.zeros((128, NB * 16 + 1), np.float32)
    consts2[:, :NB * 16] = np.repeat(np.arange(NB, dtype=np.float32), 16)[None, :]
    consts2[:, NB * 16] = np.arange(128, dtype=np.float32)
    shared = {
        'lnp': np.ascontiguousarray(lnp), 'w_mem_kv': f(inp['w_mem_kv']), 'router_w': f(inp['router_w']),
        'router_b': rep(inp['router_b']), 'na_w_in': f(inp['na_w_in'])[0], 'na_bias': nab,
        'ml_w_in': f(inp['ml_w_in'])[0], 'ml_cwb': cwb.reshape(128, 64), 'ml_w_qkv': f(inp['ml_w_qkv'])[0],
        'ml_gate_b': rep(f(inp['ml_gate_b'])[0].reshape(-1)),
        'ml_ngs': np.concatenate([rep(f(inp['ml_norm_g'])[0]), rep(f(inp['ml_skip'])[0])], 1),
        'w_out': f(inp['w_out']), 'exp_w_gate': relay(inp['exp_w_gate'], 8), 'exp_w_up': relay(inp['exp_w_up'], 8),
        'exp_w_down': relay(inp['exp_w_down'], 4), 'consts': np.ascontiguousarray(consts), 'consts2': consts2,
    }
    return shared


def run(inputs, stage='full', cores=8):
    if stage not in _CACHE:
        b = Builder(stage)
        b.build()
        _CACHE[stage] = b.nc
    nc = _CACHE[stage]
    shared = _host_inputs(inputs)
    x = np.asarray(inputs['x'], np.float32)
    mem = np.asarray(inputs['mem'], np.float32)
    in_maps = []
    for c in range(cores):
        m = dict(shared)
        m['x'] = np.ascontiguousarray(x[c])
        m['mem'] = np.ascontiguousarray(mem[c])
        in_maps.append(m)
    res = run_bass_kernel_spmd(nc, in_maps, core_ids=list(range(cores)))
    return np.stack([np.asarray(r['y'], np.float32) for r in res.results], 0)


def kernel(**inputs):
    return run(inputs, 'full', 8)
```
